# Optimizing a Trainium2 kernel written in Bass

```python
import jax, jax.numpy as jnp
from jax import lax
import numpy as np

D_MODEL = 1024
BATCH = 8
SEQ = 4096
DEPTH = 2

GRID_W = 64
CTX_LEN = 256
ROPE_THETA = 10000.0
Q_BLOCK = 128

A_HEADS = 4
A_KV_HEADS = 2
A_DIM = 128
B_HEADS = 4
B_QK_DIM = 128
B_V_DIM = 128
RET_CHUNK = 128
MLA_HEADS = 8
MLA_Q_LORA = 384
MLA_KV_LORA = 256
MLA_NOPE = 128
MLA_ROPE = 64
MLA_V = 128
N_EXPERTS = 32
TOP_K = 4
D_FF = D_MODEL
SWIGLU_LIMIT = 7.0
SWIGLU_ALPHA = 1.702
MOE_BLOCK = 128
DN_ALPHA = (2 * DEPTH) ** 0.25
DN_BETA = (8 * DEPTH) ** -0.25
N_EVEN = (DEPTH + 1) // 2
N_ODD = DEPTH // 2
A_Q_W = A_HEADS * A_DIM
A_KV_W = A_KV_HEADS * A_DIM
B_QK_W = B_HEADS * B_QK_DIM
B_V_W = B_HEADS * B_V_DIM
AB_IN = A_Q_W + 2 * A_KV_W + 2 * B_QK_W + 2 * B_V_W
MIX_WIDTH = A_Q_W + B_V_W
MLA_IN = MLA_Q_LORA + MLA_KV_LORA + MLA_ROPE

kernel_name = 'hybrid_dit_gqa_retention_mla_moe'

F32 = jnp.float32


def rms_norm(x, g, eps=1e-6):
    xf = x.astype(F32)
    return (xf * lax.rsqrt(jnp.mean(xf * xf, axis=-1, keepdims=True) + eps)).astype(x.dtype) * g


def layer_norm(x, g, b, eps=1e-5):
    xf = x.astype(F32)
    mu = jnp.mean(xf, axis=-1, keepdims=True)
    var = jnp.mean(jnp.square(xf - mu), axis=-1, keepdims=True)
    return ((xf - mu) * lax.rsqrt(var + eps)).astype(x.dtype) * g + b


def modulate(x, shift, scale):
    return x * (1.0 + scale) + shift


def to_heads(t, n, d):
    b, s, _ = t.shape
    return t.reshape(b, s, n, d).transpose(0, 2, 1, 3)


def from_heads(t):
    b, n, s, d = t.shape
    return t.transpose(0, 2, 1, 3).reshape(b, s, n * d)


def rope_1d(x, pos):
    half = x.shape[-1] // 2
    inv = ROPE_THETA ** (-jnp.arange(half, dtype=F32) / half)
    ang = pos.astype(F32)[:, None] * inv[None, :]
    cos, sin = jnp.cos(ang), jnp.sin(ang)
    x1, x2 = x[..., :half], x[..., half:]
    return jnp.concatenate([x1 * cos - x2 * sin, x1 * sin + x2 * cos], axis=-1).astype(x.dtype)


def axial_rope(x, n_tok):
    n_rows = n_tok // GRID_W
    rows = jnp.repeat(jnp.arange(n_rows), GRID_W)
    cols = jnp.tile(jnp.arange(GRID_W), n_rows)
    d = x.shape[-1] // 2
    return jnp.concatenate([rope_1d(x[..., :d], rows), rope_1d(x[..., d:], cols)], axis=-1)


def attend(q, k, v):
    s = jnp.einsum('bkgqd,bksd->bkgqs', q, k, preferred_element_type=F32)
    p = jax.nn.softmax(s, axis=-1).astype(v.dtype)
    return jnp.einsum('bkgqs,bkse->bkgqe', p, v)


def blocked_attend(q, k, v):
    b, kh, g, t, d = q.shape
    nb = t // Q_BLOCK
    qb = jnp.moveaxis(q.reshape(b, kh, g, nb, Q_BLOCK, d), 3, 0)
    ob = lax.map(lambda qi: attend(qi, k, v), qb)
    return jnp.moveaxis(ob, 0, 3).reshape(b, kh, g, t, v.shape[-1])


def retention_scan(q, k, v, log_gamma, s0, include_diag):
    b, h, t, dk = k.shape
    dv = v.shape[-1]
    nc = t // RET_CHUNK
    pos = jnp.arange(RET_CHUNK, dtype=F32)
    lg = log_gamma[:, None]
    kc = k.reshape(b, h, nc, RET_CHUNK, dk).astype(F32)
    vc = v.reshape(b, h, nc, RET_CHUNK, dv).astype(F32)
    zeta = jnp.exp(lg * (RET_CHUNK - 1 - pos))
    kv = jnp.einsum('bhncd,bhnce->bhnde', kc * zeta[None, :, None, :, None], vc)
    chunk_decay = jnp.exp(log_gamma * RET_CHUNK)[None, :, None, None]

    def step(s, kv_n):
        return chunk_decay * s + kv_n, s

    s_last, s_prev = lax.scan(step, s0, jnp.moveaxis(kv, 2, 0))
    if q is None:
        return None, s_last
    s_prev = jnp.moveaxis(s_prev, 0, 2)
    qc = q.reshape(b, h, nc, RET_CHUNK, dk).astype(F32)
    diff = pos[:, None] - pos[None, :]
    mask = (diff >= 0) if include_diag else (diff > 0)
    d_intra = jnp.where(mask[None], jnp.exp(lg[:, :, None] * jnp.maximum(diff, 0.0)[None]), 0.0)
    xi = jnp.exp(lg * (pos + 1.0))
    sc = jnp.einsum('bhncd,bhnsd->bhncs', qc, kc) * d_intra[None, :, None]
    out = (jnp.einsum('bhncs,bhnse->bhnce', sc, vc)
           + jnp.einsum('bhncd,bhnde->bhnce', qc * xi[None, :, None, :, None], s_prev))
    return out.reshape(b, h, t, dv), s_last


def bidir_retention(q_c, k_c, v_c, q_l, k_l, v_l, log_decay):
    b, h, _, dk = k_c.shape
    dv = v_c.shape[-1]
    log_gamma = jnp.log1p(-jnp.exp(log_decay.astype(F32)))
    s0 = jnp.zeros((b, h, dk, dv), F32)
    flip = lambda t: t[:, :, ::-1]
    oc_f, s_f = retention_scan(q_c, k_c, v_c, log_gamma[0], s0, True)
    oc_b, s_b = retention_scan(None if q_c is None else flip(q_c), flip(k_c), flip(v_c), log_gamma[1], s0, False)
    ol_f, _ = retention_scan(q_l, k_l, v_l, log_gamma[0], s_f, True)
    ol_b, _ = retention_scan(flip(q_l), flip(k_l), flip(v_l), log_gamma[1], s_b, False)
    out_c = None if q_c is None else oc_f + flip(oc_b)
    return out_c, ol_f + flip(ol_b)


def head_group_norm(o, g, eps=1e-5):
    mu = jnp.mean(o, axis=-1, keepdims=True)
    var = jnp.mean(jnp.square(o - mu), axis=-1, keepdims=True)
    return from_heads(((o - mu) * lax.rsqrt(var + eps)).astype(g.dtype)) * g


def mixer_ab(hc, hl, w_in, q_norm, k_norm, log_decay, gn_g, w_out, with_ctx):
    n_ctx, n_lat = hc.shape[1], hl.shape[1]
    cuts = np.cumsum([A_Q_W, A_KV_W, A_KV_W, B_QK_W, B_QK_W, B_V_W])
    aq_c, ak_c, av_c, bq_c, bk_c, bv_c, bg_c = jnp.split(hc @ w_in, cuts, axis=-1)
    aq_l, ak_l, av_l, bq_l, bk_l, bv_l, bg_l = jnp.split(hl @ w_in, cuts, axis=-1)
    a_scale = A_DIM ** -0.5
    grp = A_HEADS // A_KV_HEADS
    ka_c = rms_norm(to_heads(ak_c, A_KV_HEADS, A_DIM), k_norm)
    va_c = to_heads(av_c, A_KV_HEADS, A_DIM)
    qa_l = axial_rope(rms_norm(to_heads(aq_l, A_HEADS, A_DIM), q_norm), n_lat) * a_scale
    ka_l = axial_rope(rms_norm(to_heads(ak_l, A_KV_HEADS, A_DIM), k_norm), n_lat)
    va_l = to_heads(av_l, A_KV_HEADS, A_DIM)
    k_all = jnp.concatenate([ka_c, ka_l], axis=2)
    v_all = jnp.concatenate([va_c, va_l], axis=2)
    b = hl.shape[0]
    oa_l = blocked_attend(qa_l.reshape(b, A_KV_HEADS, grp, n_lat, A_DIM), k_all, v_all)
    oa_l = from_heads(oa_l.reshape(b, A_HEADS, n_lat, A_DIM))
    b_scale = B_QK_DIM ** -0.5
    pos_c = jnp.arange(n_ctx)
    pos_l = n_ctx + jnp.arange(n_lat)
    kb_c = rope_1d(to_heads(bk_c, B_HEADS, B_QK_DIM), pos_c)
    vb_c = to_heads(bv_c, B_HEADS, B_V_DIM)
    qb_l = rope_1d(to_heads(bq_l, B_HEADS, B_QK_DIM), pos_l) * b_scale
    kb_l = rope_1d(to_heads(bk_l, B_HEADS, B_QK_DIM), pos_l)
    vb_l = to_heads(bv_l, B_HEADS, B_V_DIM)
    qb_c = rope_1d(to_heads(bq_c, B_HEADS, B_QK_DIM), pos_c) * b_scale if with_ctx else None
    ob_c, ob_l = bidir_retention(qb_c, kb_c, vb_c, qb_l, kb_l, vb_l, log_decay)
    yb_l = head_group_norm(ob_l, gn_g) * jax.nn.silu(bg_l)
    yl = jnp.concatenate([oa_l, yb_l], axis=-1) @ w_out
    if not with_ctx:
        return None, yl
    qa_c = rms_norm(to_heads(aq_c, A_HEADS, A_DIM), q_norm) * a_scale
    oa_c = attend(qa_c.reshape(b, A_KV_HEADS, grp, n_ctx, A_DIM), ka_c, va_c)
    oa_c = from_heads(oa_c.reshape(b, A_HEADS, n_ctx, A_DIM))
    yb_c = head_group_norm(ob_c, gn_g) * jax.nn.silu(bg_c)
    yc = jnp.concatenate([oa_c, yb_c], axis=-1) @ w_out
    return yc, yl


def mla_kv(p, kv_norm, w_ukv, use_rope):
    ckv = p[..., MLA_Q_LORA:MLA_Q_LORA + MLA_KV_LORA]
    kpe = p[..., MLA_Q_LORA + MLA_KV_LORA:][:, None]
    kv = to_heads(rms_norm(ckv, kv_norm) @ w_ukv, MLA_HEADS, MLA_NOPE + MLA_V)
    k_nope, v = kv[..., :MLA_NOPE], kv[..., MLA_NOPE:]
    if use_rope:
        kpe = axial_rope(kpe, p.shape[1])
    kpe = jnp.broadcast_to(kpe, k_nope.shape[:-1] + (MLA_ROPE,))
    return jnp.concatenate([k_nope, kpe], axis=-1), v


def mla_q(p, q_norm, w_uq, use_rope):
    q = to_heads(rms_norm(p[..., :MLA_Q_LORA], q_norm) @ w_uq, MLA_HEADS, MLA_NOPE + MLA_ROPE)
    if use_rope:
        q = jnp.concatenate([q[..., :MLA_NOPE], axial_rope(q[..., MLA_NOPE:], p.shape[1])], axis=-1)
    return q * (MLA_NOPE + MLA_ROPE) ** -0.5


def mixer_mla(hc, hl, w_in, q_norm, kv_norm, w_uq, w_ukv, w_out, with_ctx):
    pc, pl = hc @ w_in, hl @ w_in
    k_c, v_c = mla_kv(pc, kv_norm, w_ukv, False)
    k_l, v_l = mla_kv(pl, kv_norm, w_ukv, True)
    q_l = mla_q(pl, q_norm, w_uq, True)
    k_all = jnp.concatenate([k_c, k_l], axis=2)
    v_all = jnp.concatenate([v_c, v_l], axis=2)
    yl = from_heads(blocked_attend(q_l[:, :, None], k_all, v_all)[:, :, 0]) @ w_out
    if not with_ctx:
        return None, yl
    q_c = mla_q(pc, q_norm, w_uq, False)
    yc = from_heads(attend(q_c[:, :, None], k_c, v_c)[:, :, 0]) @ w_out
    return yc, yl


def clamped_swiglu(gu):
    glu = jnp.minimum(gu[..., :D_FF], SWIGLU_LIMIT)
    lin = jnp.clip(gu[..., D_FF:], -SWIGLU_LIMIT, SWIGLU_LIMIT)
    return glu * jax.nn.sigmoid(SWIGLU_ALPHA * glu) * (lin + 1.0)


def moe(h, router_w, router_b, w_up, b_up, w_down, b_down):
    t, d = h.shape
    logits = (h @ router_w + router_b).astype(F32)
    top_v, top_i = lax.top_k(logits, TOP_K)
    gates = jax.nn.softmax(top_v, axis=-1)
    n_asg = t * TOP_K
    flat_e = top_i.reshape(-1)
    order = jnp.argsort(flat_e)
    e_sorted = flat_e[order]
    tok_sorted = (order // TOP_K).astype(jnp.int32)
    counts = jnp.bincount(flat_e, length=N_EXPERTS)
    padded = (counts + MOE_BLOCK - 1) // MOE_BLOCK * MOE_BLOCK
    pad_end = jnp.cumsum(padded)
    pad_start = pad_end - padded
    start = jnp.cumsum(counts) - counts
    dest = pad_start[e_sorted] + jnp.arange(n_asg) - start[e_sorted]
    n_blocks = -(-n_asg // MOE_BLOCK) + N_EXPERTS
    n_slots = n_blocks * MOE_BLOCK
    slot_tok = jnp.full((n_slots,), t, jnp.int32).at[dest].set(tok_sorted)
    slot_gate = jnp.zeros((n_slots,), F32).at[dest].set(gates.reshape(-1)[order])
    blk_expert = jnp.minimum(jnp.searchsorted(pad_end, jnp.arange(n_blocks) * MOE_BLOCK, side='right'), N_EXPERTS - 1)
    h_pad = jnp.concatenate([h, jnp.zeros((1, d), h.dtype)], axis=0)

    def expert_block(args):
        tok, g, e = args
        xb = h_pad[tok]
        y = clamped_swiglu(xb @ w_up[e] + b_up[e]) @ w_down[e] + b_down[e]
        return (y * g[:, None]).astype(h.dtype)

    yb = lax.map(expert_block, (slot_tok.reshape(n_blocks, MOE_BLOCK), slot_gate.reshape(n_blocks, MOE_BLOCK), blk_expert))
    out = jnp.zeros((t + 1, d), h.dtype).at[slot_tok].add(yb.reshape(n_slots, d))
    return out[:t]


def setup_inputs(seed: int = 0) -> dict:
    key = jax.random.key(seed)
    ks = jax.random.split(key, 28)
    d = D_MODEL
    nrm = lambda k, shape, s: jax.random.normal(k, shape, F32) * s
    gain = lambda k, shape: 1.0 + 0.02 * jax.random.normal(k, shape, F32)
    log_decay0 = jnp.log(2.0 ** -(5.0 + jnp.arange(B_HEADS, dtype=F32)))
    return {
        'x': nrm(ks[0], (BATCH, SEQ, d), 1.0),
        'c': nrm(ks[1], (BATCH, d), 1.0),
        'ctx': nrm(ks[2], (BATCH, CTX_LEN, d), 1.0),
        'c_ctx': nrm(ks[3], (d,), 1.0),
        'ada_w': nrm(ks[4], (DEPTH, d, 6 * d), 0.5 * d ** -0.5),
        'ada_b': nrm(ks[5], (DEPTH, 6 * d), 0.02),
        'ln1_g': gain(ks[6], (DEPTH, d)),
        'ln1_b': nrm(ks[7], (DEPTH, d), 0.02),
        'ln2_g': gain(ks[8], (DEPTH, d)),
        'ln2_b': nrm(ks[9], (DEPTH, d), 0.02),
        'ab_w_in': nrm(ks[10], (N_EVEN, d, AB_IN), d ** -0.5),
        'ab_q_norm': gain(ks[11], (N_EVEN, A_DIM)),
        'ab_k_norm': gain(ks[12], (N_EVEN, A_DIM)),
        'ab_log_decay': log_decay0 + nrm(ks[13], (N_EVEN, 2, B_HEADS), 0.05),
        'ab_gn_g': gain(ks[14], (N_EVEN, B_V_W)),
        'ab_w_out': nrm(ks[15], (N_EVEN, MIX_WIDTH, d), DN_BETA * MIX_WIDTH ** -0.5),
        'mla_w_in': nrm(ks[16], (N_ODD, d, MLA_IN), d ** -0.5),
        'mla_q_norm': gain(ks[17], (N_ODD, MLA_Q_LORA)),
        'mla_kv_norm': gain(ks[18], (N_ODD, MLA_KV_LORA)),
        'mla_w_uq': nrm(ks[19], (N_ODD, MLA_Q_LORA, MLA_HEADS * (MLA_NOPE + MLA_ROPE)), MLA_Q_LORA ** -0.5),
        'mla_w_ukv': nrm(ks[20], (N_ODD, MLA_KV_LORA, MLA_HEADS * (MLA_NOPE + MLA_V)), MLA_KV_LORA ** -0.5),
        'mla_w_out': nrm(ks[21], (N_ODD, MLA_HEADS * MLA_V, d), DN_BETA * (MLA_HEADS * MLA_V) ** -0.5),
        'moe_router_w': nrm(ks[22], (DEPTH, d, N_EXPERTS), d ** -0.5),
        'moe_router_b': nrm(ks[23], (DEPTH, N_EXPERTS), 0.01),
        'moe_w_up': nrm(ks[24], (DEPTH, N_EXPERTS, d, 2 * D_FF), d ** -0.5),
        'moe_b_up': nrm(ks[25], (DEPTH, N_EXPERTS, 2 * D_FF), 0.02),
        'moe_w_down': nrm(ks[26], (DEPTH, N_EXPERTS, D_FF, d), DN_BETA * D_FF ** -0.5),
        'moe_b_down': nrm(ks[27], (DEPTH, N_EXPERTS, d), 0.02),
    }


def reference(x, c, ctx, c_ctx, ada_w, ada_b, ln1_g, ln1_b, ln2_g, ln2_b,
              ab_w_in, ab_q_norm, ab_k_norm, ab_log_decay, ab_gn_g, ab_w_out,
              mla_w_in, mla_q_norm, mla_kv_norm, mla_w_uq, mla_w_ukv, mla_w_out,
              moe_router_w, moe_router_b, moe_w_up, moe_b_up, moe_w_down, moe_b_down):
    b, n_lat, d = x.shape
    n_ctx = ctx.shape[1]
    sc = jax.nn.silu(c)
    scc = jax.nn.silu(c_ctx)
    xl, xc = x, ctx
    for i in range(DEPTH):
        last = i == DEPTH - 1
        j = i // 2
        mod_l = jnp.split((sc @ ada_w[i] + ada_b[i])[:, None, :], 6, axis=-1)
        mod_c = jnp.split(scc @ ada_w[i] + ada_b[i], 6, axis=-1)
        hl = modulate(xl, mod_l[0], mod_l[1])
        hc = modulate(xc, mod_c[0], mod_c[1])
        if i % 2 == 0:
            yc, yl = mixer_ab(hc, hl, ab_w_in[j], ab_q_norm[j], ab_k_norm[j], ab_log_decay[j],
                              ab_gn_g[j], ab_w_out[j], not last)
        else:
            yc, yl = mixer_mla(hc, hl, mla_w_in[j], mla_q_norm[j], mla_kv_norm[j], mla_w_uq[j],
                               mla_w_ukv[j], mla_w_out[j], not last)
        xl = layer_norm(DN_ALPHA * xl + mod_l[2] * yl, ln1_g[i], ln1_b[i])
        hl = modulate(xl, mod_l[3], mod_l[4])
        moe_args = (moe_router_w[i], moe_router_b[i], moe_w_up[i], moe_b_up[i], moe_w_down[i], moe_b_down[i])
        if last:
            fl = moe(hl.reshape(b * n_lat, d), *moe_args).reshape(b, n_lat, d)
        else:
            xc = layer_norm(DN_ALPHA * xc + mod_c[2] * yc, ln1_g[i], ln1_b[i])
            hc = modulate(xc, mod_c[3], mod_c[4])
            f = moe(jnp.concatenate([hc.reshape(b * n_ctx, d), hl.reshape(b * n_lat, d)], axis=0), *moe_args)
            fc = f[:b * n_ctx].reshape(b, n_ctx, d)
            fl = f[b * n_ctx:].reshape(b, n_lat, d)
            xc = layer_norm(DN_ALPHA * xc + mod_c[5] * fc, ln2_g[i], ln2_b[i])
        xl = layer_norm(DN_ALPHA * xl + mod_l[5] * fl, ln2_g[i], ln2_b[i])
    return xl
```

```python
import numpy as np
import ml_dtypes
from contextlib import ExitStack
import concourse.bass as bass
import concourse.mybir as mybir
from concourse.bass_utils import run_bass_kernel_spmd

F32 = mybir.dt.float32
BF16 = mybir.dt.bfloat16
I32 = mybir.dt.int32
AF = mybir.ActivationFunctionType
ALU = mybir.AluOpType
AX = mybir.AxisListType

D = 1024
GRID_W = 64
THETA = 10000.0
NE = 32
LIMIT = 7.0
SW_ALPHA = 1.702
DN_ALPHA = 4.0 ** 0.25


class SemSlot:
    __slots__ = ("sem", "count")

    def __init__(self):
        self.sem = None
        self.count = 0


class Res:
    __slots__ = ("name", "slot", "lastw", "readers", "lastdma")

    def __init__(self, name):
        self.name = name
        self.slot = {}
        self.lastw = None
        self.readers = []
        self.lastdma = None


class Op:
    __slots__ = ("eng", "fn", "deps", "marked", "event", "is_dma", "ei", "done")

    def __init__(self, eng, fn):
        self.eng = eng
        self.fn = fn
        self.deps = []
        self.marked = False
        self.event = None
        self.is_dma = False
        self.done = False


class T:
    __slots__ = ("ap", "r")

    def __init__(self, ap, r):
        self.ap = ap
        self.r = r

    def __getitem__(self, idx):
        return self.ap[idx]


def _rs(xs):
    return [x.r if isinstance(x, T) else x for x in xs]


class K:
    ENGS = ("pe", "act", "dve", "pool", "sp")

    def __init__(self):
        self.nc = bass.Bass("TRN2", target_bir_lowering=False)
        self.ops = []
        self.stack = ExitStack()
        self.last_on = {e: None for e in self.ENGS}
        self.owners = []
        self.free_slots = {"hw": [], "sw": []}
        self.n = 0
        self.esem = {e: self.stack.enter_context(self.nc.semaphore(f"s_{e}")) for e in self.ENGS}
        self.cnt = {e: 0 for e in self.ENGS}
        self.waited = {e: {} for e in self.ENGS}
        self.stats = dict(nops=0, nwait=0, ndrain=0, nsem=5)

    def res(self, name=None):
        self.n += 1
        return Res(f"{name or 'r'}{self.n}")

    def sbuf(self, name, shape, dt, stack=None):
        self.n += 1
        ap = (stack or self.stack).enter_context(self.nc.sbuf_tensor(f"{name}_{self.n}", list(shape), dt))
        return T(ap, self.res(name))

    def psum(self, name, shape, dt, stack=None):
        self.n += 1
        ap = (stack or self.stack).enter_context(self.nc.psum_tensor(f"{name}_{self.n}", list(shape), dt))
        return T(ap, self.res(name))

    def dram(self, name, shape, dt, kind="Internal"):
        return T(self.nc.dram_tensor(name, list(shape), dt, kind=kind).ap(), self.res(name))

    def _track(self, op, reads, writes):
        deps = op.deps
        for r in reads:
            if r.lastw is not None:
                deps.append(r.lastw)
            r.readers.append(op)
        for w in writes:
            if w.lastw is not None:
                deps.append(w.lastw)
            deps.extend(w.readers)
            w.lastw = op
            w.readers = []
        seen = set()
        out = []
        for d in deps:
            if d is op or d.done or id(d) in seen:
                continue
            seen.add(id(d))
            if d.eng == "pe" and op.eng == "pe" and not d.is_dma and not op.is_dma:
                continue
            out.append(d)
            if d.eng != op.eng or d.is_dma:
                d.marked = True
        op.deps = out
        self.ops.append(op)
        self.last_on[op.eng] = op

    def op(self, eng, fn, reads=(), writes=()):
        o = Op(eng, fn)
        self._track(o, _rs(reads), _rs(writes))
        return o

    def dma(self, eng, out, in_, owner, reads=(), writes=(), **kw):
        o = Op(eng, lambda e: e.dma_start(out=out, in_=in_, **kw))
        self._dma_common(o, owner, reads, writes)
        return o

    def dma_fn(self, eng, fn, owner, reads=(), writes=()):
        o = Op(eng, fn)
        self._dma_common(o, owner, reads, writes)
        return o

    def _dma_common(self, o, owner, reads, writes):
        owner = owner.r if isinstance(owner, T) else owner
        o.is_dma = True
        o.marked = True
        kind = "sw" if o.eng == "pool" else "hw"
        if kind not in owner.slot:
            fl = self.free_slots[kind]
            owner.slot[kind] = fl.pop() if fl else SemSlot()
            if owner not in self.owners:
                self.owners.append(owner)
        if owner.lastdma is not None and not owner.lastdma.done:
            o.deps.append(owner.lastdma)
        owner.lastdma = o
        sl = owner.slot[kind]
        sl.count += 16
        o.event = (sl, sl.count)
        self._track(o, _rs(reads), _rs(writes))

    def barrier(self):
        lasts = [self.last_on[e] for e in self.ENGS if self.last_on[e] is not None and not self.last_on[e].done]
        lasts += [o.lastdma for o in self.owners if o.lastdma is not None and not o.lastdma.done]
        for e in self.ENGS:
            o = Op(e, None)
            for d in lasts:
                if d.eng == e and not d.is_dma:
                    continue
                if d.fn is None and not d.is_dma:
                    continue
                o.deps.append(d)
                d.marked = True
            self.ops.append(o)
            self.last_on[e] = o

    def flush(self):
        self.barrier()
        nc = self.nc
        esem = self.esem
        for r in self.owners:
            for sl in r.slot.values():
                if sl.sem is None:
                    self.n += 1
                    sl.sem = self.stack.enter_context(nc.semaphore(f"d{self.n}"))
                    self.stats["nsem"] += 1
        for o in self.ops:
            if o.is_dma:
                o.event = (o.event[0].sem, o.event[1])
            elif o.marked:
                self.cnt[o.eng] += 1
                o.event = (esem[o.eng], self.cnt[o.eng])
        by_eng = {e: [o for o in self.ops if o.eng == e] for e in self.ENGS}
        for e in self.ENGS:
            for i, o in enumerate(by_eng[e]):
                o.ei = i

        def run(ename, eng):
            w = self.waited[ename]
            last_drain = -1
            for o in by_eng[ename]:
                need = {}
                drain = False
                for d in o.deps:
                    if d.eng == ename and not d.is_dma:
                        if d.fn is not None and d.ei > last_drain and o.ei - d.ei <= 8:
                            drain = True
                        continue
                    sem, val = d.event
                    key = id(sem)
                    if w.get(key, 0) >= val:
                        continue
                    if key not in need or need[key][1] < val:
                        need[key] = (sem, val)
                for key, (sem, val) in need.items():
                    eng.wait_ge(sem, val)
                    w[key] = val
                    self.stats["nwait"] += 1
                if drain or (o.fn is None and ename != "pe"):
                    eng.drain()
                    last_drain = o.ei - 1
                    self.stats["ndrain"] += 1
                if o.fn is None:
                    continue
                ins = o.fn(eng)
                if o.is_dma:
                    ins.then_inc(o.event[0], 16)
                elif o.marked:
                    ins.then_inc(esem[o.eng], 1)

        with nc.Block() as block:
            block.tensor(lambda e: run("pe", e))
            block.scalar(lambda e: run("act", e))
            block.vector(lambda e: run("dve", e))
            block.gpsimd(lambda e: run("pool", e))
            block.sync(lambda e: run("sp", e))
        self.stats["nops"] += len(self.ops)
        for o in self.ops:
            o.done = True
        self.ops = []
        for r in self.owners:
            for kind, sl in r.slot.items():
                self.free_slots[kind].append(sl)
            r.slot = {}
        self.owners = []

    def emit(self):
        self.flush()
        self.stack.close()
        return self.nc


def _rope_tables(pos, d):
    half = d // 2
    inv = (THETA ** (-np.arange(half, dtype=np.float32) / np.float32(half))).astype(np.float32)
    ang = pos.astype(np.float32)[:, None] * inv[None, :]
    c, s = np.cos(ang).astype(np.float32), np.sin(ang).astype(np.float32)
    return np.concatenate([c, c], 1), np.concatenate([-s, s], 1)


def _axial_tables(n_tok, d):
    rows = np.repeat(np.arange(n_tok // GRID_W), GRID_W)
    cols = np.tile(np.arange(GRID_W), n_tok // GRID_W)
    c1, s1 = _rope_tables(rows, d // 2)
    c2, s2 = _rope_tables(cols, d // 2)
    return np.concatenate([c1, c2], 1), np.concatenate([s1, s2], 1)


def make_tables(TL, TC):
    TA = TL + TC
    ca, sa = _axial_tables(TL, 128)
    CA = np.concatenate([np.ones((TC, 128), np.float32), ca], 0)
    SA = np.concatenate([np.zeros((TC, 128), np.float32), sa], 0)
    cb, sb = _rope_tables(np.arange(TA), 128)
    cm, sm = _axial_tables(TL, 64)
    CM = np.concatenate([np.ones((TC, 64), np.float32), cm], 0)
    SM = np.concatenate([np.zeros((TC, 64), np.float32), sm], 0)
    pm = np.zeros((128, 128), np.float32)
    for m in range(64):
        src = m + 16 if (m % 32) < 16 else m - 16
        pm[src, m] = 1.0
    rmt = np.zeros((128, 2, TA), np.float32)
    rmt[:64, 0] = CM.T
    rmt[:64, 1] = SM.T
    return dict(pmat=pm, ropeMT=rmt,
                ropeA=np.stack([CA, SA], 1).astype(np.float32),
                ropeB=np.stack([np.tile(cb, (1, 4)), np.tile(sb, (1, 4))], 1).astype(np.float32),
                ropeM=np.stack([np.tile(CM, (1, 8)), np.tile(SM, (1, 8))], 1).astype(np.float32))


class B:
    def __init__(self, TL, TC, debug=False, stop=None):
        self.k = K()
        self.TL, self.TC, self.TA = TL, TC, TL + TC
        self.NTL, self.NTC, self.NTA = TL // 128, TC // 128, (TL + TC) // 128
        self.debug = debug
        self.stop = stop
        self.GT = 4

    def mm(self, out, lhsT, rhs, start, stop, reads, writes):
        self.k.op("pe", lambda e: e.matmul(out=out, lhsT=lhsT, rhs=rhs, start=start, stop=stop), reads, writes)

    def tr(self, out, in_, ident, reads, writes):
        self.k.op("pe", lambda e: e.transpose(out=out, in_=in_, identity=ident), reads, writes)

    def act(self, out, in_, func, reads, writes, bias=0.0, scale=1.0, accum_out=None):
        if accum_out is None:
            self.k.op("act", lambda e: e.activation(out=out, in_=in_, func=func, bias=bias, scale=scale), reads, writes)
        else:
            self.k.op("act", lambda e: e.activation(out=out, in_=in_, func=func, bias=bias, scale=scale, accum_out=accum_out), reads, writes)

    def ts(self, eng, out, in0, s1, s2, op0, op1, reads, writes):
        if s2 is None:
            self.k.op(eng, lambda e: e.tensor_scalar(out=out, in0=in0, scalar1=s1, scalar2=None, op0=op0), reads, writes)
        else:
            self.k.op(eng, lambda e: e.tensor_scalar(out=out, in0=in0, scalar1=s1, scalar2=s2, op0=op0, op1=op1), reads, writes)

    def tt(self, eng, out, in0, in1, op, reads, writes):
        self.k.op(eng, lambda e: e.tensor_tensor(out=out, in0=in0, in1=in1, op=op), reads, writes)

    def stt(self, eng, out, in0, scalar, in1, op0, op1, reads, writes):
        self.k.op(eng, lambda e: e.scalar_tensor_tensor(out=out, in0=in0, scalar=scalar, in1=in1, op0=op0, op1=op1), reads, writes)

    def cp(self, eng, out, in_, reads, writes):
        if eng == "act":
            self.k.op("act", lambda e: e.copy(out=out, in_=in_), reads, writes)
        else:
            self.k.op(eng, lambda e: e.tensor_copy(out=out, in_=in_), reads, writes)

    def memset(self, eng, ap, val, writes):
        self.k.op(eng, lambda e: e.memset(ap, val), (), writes)

    def recip(self, out, in_, reads, writes):
        self.k.op("dve", lambda e: e.reciprocal(out=out, in_=in_), reads, writes)

    def dma(self, out, in_, owner, reads=(), writes=(), eng="sp", **kw):
        self.k.dma(eng, out, in_, owner, reads, writes, **kw)

    def scratch(self, name, shape, dt):
        return self.k.dram(name, shape, dt, kind="ExternalOutput" if self.debug else "Internal")

    def inp(self, name, shape, dt=F32):
        return self.k.dram(name, shape, dt, kind="ExternalInput")

    def declare(self):
        TL, TC, TA = self.TL, self.TC, self.TA
        i = self.inp
        self.x_in = i("x", [TL, D]); self.ctx_in = i("ctx", [TC, D])
        self.cv2 = i("cv2", [128, 8, 2])
        self.ada_w = i("ada_w", [2, D, 6 * D]); self.ada_bF = i("ada_bF", [2, 128, 48]); self.ada_b = i("ada_b", [2, 1, 6 * D])
        self.lnF = i("lnF", [2, 128, 4, 8]); self.ln_rows = i("ln_rows", [2, 4, D])
        self.ab_w_in = i("ab_w_in", [D, 3072]); self.ab_qk = i("ab_qk", [2, 128]); self.ab_ld = i("ab_ld", [1, 8])
        self.ab_gn = i("ab_gn", [1, 512]); self.ab_w_out = i("ab_w_out", [D, D])
        self.mla_w_in = i("mla_w_in", [D, 704]); self.mla_qn = i("mla_qn", [1, 384]); self.mla_kvn = i("mla_kvn", [1, 256])
        self.mla_w_uq = i("mla_w_uq", [384, 1536]); self.mla_w_ukv = i("mla_w_ukv", [256, 2048]); self.mla_w_out = i("mla_w_out", [D, D])
        self.r_w = i("r_w", [2, D, NE]); self.r_b = i("r_b", [2, 1, NE])
        ne = getattr(self, "ne_decl", NE)
        self.w_up = i("w_up", [2, ne, D, 2 * D]); self.b_upF = i("b_upF", [2, NE, 128, 16])
        self.w_down = i("w_down", [2, ne, D, D]); self.b_down = i("b_down", [2, NE, D])
        self.identF_in = i("identF", [128, 128])
        self.ropeA = i("ropeA", [TA, 2, 128]); self.ropeB = i("ropeB", [TA, 2, 512]); self.ropeM = i("ropeM", [TA, 2, 512])
        self.y_out = self.k.dram("y", [TL, D], F32, kind="ExternalOutput")
        s = self.scratch
        self.hT0 = s("hT0", [self.NTA, 128, 8, 128], BF16)
        self.cat = s("cat", [TA, D], BF16)
        self.x1 = s("x1", [TA, D], F32)
        self.h2T = s("h2T", [self.NTA, 128, 8, 128], BF16)
        self.x2 = s("x2", [TA, D], F32)
        self.qTB = s("qTB", [4, 128, TA], BF16); self.kTB = s("kTB", [4, 128, TA], BF16)
        self.kB = s("kB", [TA, 512], BF16); self.vB = s("vB", [TA, 512], BF16); self.gB = s("gB", [TA, 512], BF16)

    def consts(self):
        k = self.k
        self.identF = k.sbuf("identF", [128, 128], F32)
        self.dma(self.identF[:], self.identF_in[:, :], self.identF, writes=[self.identF])
        self.identB = k.sbuf("identB", [128, 128], BF16)
        self.cp("dve", self.identB[:], self.identF[:], [self.identF], [self.identB])
        self.ones = k.sbuf("ones", [128, 512], F32)
        self.memset("pool", self.ones[:], 1.0, [self.ones])

    def xsrc(self, tt):
        if tt < self.NTC:
            return self.ctx_in, self.ctx_in[tt * 128:(tt + 1) * 128, :]
        t = tt - self.NTC
        return self.x_in, self.x_in[t * 128:(t + 1) * 128, :]

    def phase0(self):
        k = self.k
        P_modF = [k.sbuf("modF", [128, 48, 2], F32) for _ in range(2)]
        self.G = [k.sbuf("G", [128, self.NTA, NE], F32) for _ in range(2)]
        self.gate_rows = self.scratch("gate_rows", [2, 2, 2, D], F32)
        P_coef = [k.sbuf("coef", [128, 4, 2, 8], F32) for _ in range(2)]
        P_lnF = [k.sbuf("lnF", [128, 4, 8], F32) for _ in range(2)]
        ps = ExitStack()
        cv = k.sbuf("cv", [128, 8, 2], F32, ps)
        self.dma(cv[:], self.cv2[:, :, :], cv, writes=[cv])
        sc2 = k.sbuf("sc2", [128, 8, 2], F32, ps)
        self.act(sc2[:], cv[:], AF.Silu, [cv], [sc2])
        scB = k.sbuf("scB", [128, 8, 2, 128], F32, ps)
        for kk in range(8):
            for s in range(2):
                self.act(scB[:, kk, s, :], self.ones[:, 0:128], AF.Copy, [self.ones, sc2], [scB], scale=sc2[:, kk, s:s + 1])
        wch = [k.sbuf("wch", [128, 8, 512], F32, ps) for _ in range(2)]
        brow = [k.sbuf("brow", [1, 512], F32, ps) for _ in range(2)]
        grow = [k.sbuf("grow", [1, 512], F32, ps) for _ in range(2)]
        abFs = [k.sbuf("abF", [128, 48], F32, ps) for _ in range(2)]
        tmp = k.sbuf("ctmp", [128, 2, 8], F32, ps)
        psm = k.psum("psm", [128, 512], F32, ps)
        psg = [k.psum("psg", [128, 512], F32, ps) for _ in range(2)]
        self.modF, self.coef = [], []
        for i in range(2):
            modF, coef, lnF = P_modF[i], P_coef[i], P_lnF[i]
            abF = abFs[i]
            self.dma(abF[:], self.ada_bF[i], abF, writes=[abF])
            for n in range(12):
                w = wch[n % 2]
                self.dma(w[:], self.ada_w[i][:, n * 512:(n + 1) * 512].rearrange("(k p) n -> p k n", p=128), w, writes=[w])
                for m in range(4):
                    mc = n * 4 + m
                    for kk in range(8):
                        self.mm(psm[:, mc * 2:mc * 2 + 2], w[:, kk, m * 128:(m + 1) * 128], sc2[:, kk, :], kk == 0, kk == 7, [w, sc2], [psm])
                j = n // 2
                if j in (2, 5):
                    br = brow[n % 2]
                    self.dma(br[:], self.ada_b[i][:, n * 512:(n + 1) * 512], br, writes=[br])
                    for s in range(2):
                        for kk in range(8):
                            self.mm(psg[s][:], scB[:, kk, s, :], w[:, kk, :], kk == 0, False, [w, scB], [psg[s]])
                        self.mm(psg[s][:], self.ones[0:1, 0:128], br[:], False, True, [self.ones, br], [psg[s]])
                        gr = grow[(n + s) % 2]
                        self.cp("act", gr[:], psg[s][0:1, :], [psg[s]], [gr])
                        self.dma(self.gate_rows[i, 0 if j == 2 else 1, s:s + 1, (n % 2) * 512:(n % 2 + 1) * 512], gr[:], gr, reads=[gr], writes=[self.gate_rows])
            for s in range(2):
                self.tt("dve", modF[:, :, s], psm[:, s:96:2], abF[:], ALU.add, [psm, abF], [modF])
            self.dma(lnF[:], self.lnF[i], lnF, writes=[lnF])
            for s in range(2):
                self.ts("dve", coef[:, 0, s, :], modF[:, 8:16, s], 1.0, None, ALU.add, None, [modF], [coef])
                self.cp("dve", coef[:, 1, s, :], modF[:, 0:8, s], [modF], [coef])
                self.ts("dve", tmp[:, s, :], modF[:, 32:40, s], 1.0, None, ALU.add, None, [modF], [tmp])
                self.tt("dve", coef[:, 2, s, :], tmp[:, s, :], lnF[:, 0, :], ALU.mult, [tmp, lnF], [coef])
                self.tt("dve", coef[:, 3, s, :], tmp[:, s, :], lnF[:, 1, :], ALU.mult, [tmp, lnF], [coef])
                self.tt("dve", coef[:, 3, s, :], coef[:, 3, s, :], modF[:, 24:32, s], ALU.add, [coef, modF], [coef])
            self.modF.append(modF); self.coef.append(coef)
        k.flush()
        ps.close()

    def dbg(self, name, t, shape, dt=F32):
        if not self.debug:
            return
        d = self.k.dram("dbg_" + name, shape, dt, kind="ExternalOutput")
        self.dma(d.ap, t[:], t, reads=[t], writes=[d])


def _fm(v):
    return np.ascontiguousarray(np.swapaxes(v.reshape(v.shape[:-1] + (8, 128)), -1, -2))


def prep_inputs(inp, TL, TC, nb):
    f = lambda a: np.ascontiguousarray(np.asarray(a, dtype=np.float32))
    tabs = make_tables(TL, TC)
    shared = dict(
        ada_w=f(inp["ada_w"]),
        ada_bF=np.ascontiguousarray(np.transpose(f(inp["ada_b"]).reshape(2, 48, 128), (0, 2, 1))),
        ada_b=f(inp["ada_b"]).reshape(2, 1, 6 * D),
        lnF=np.ascontiguousarray(np.stack([_fm(f(inp[n])) for n in ("ln1_g", "ln1_b", "ln2_g", "ln2_b")], 2)),
        ln_rows=np.ascontiguousarray(np.stack([f(inp[n]) for n in ("ln1_g", "ln1_b", "ln2_g", "ln2_b")], 1)),
        ab_w_in=f(inp["ab_w_in"])[0], ab_qk=np.concatenate([f(inp["ab_q_norm"]), f(inp["ab_k_norm"])], 0),
        ab_ld=f(inp["ab_log_decay"]).reshape(1, 8), ab_gn=f(inp["ab_gn_g"]).reshape(1, 512), ab_w_out=f(inp["ab_w_out"])[0],
        mla_w_in=f(inp["mla_w_in"])[0], mla_qn=f(inp["mla_q_norm"]).reshape(1, 384), mla_kvn=f(inp["mla_kv_norm"]).reshape(1, 256),
        mla_w_uq=f(inp["mla_w_uq"])[0], mla_w_ukv=f(inp["mla_w_ukv"])[0], mla_w_out=f(inp["mla_w_out"])[0],
        r_w=f(inp["moe_router_w"]), r_b=f(inp["moe_router_b"]).reshape(2, 1, NE),
        w_up=f(inp["moe_w_up"]),
        b_upF=np.ascontiguousarray(np.transpose(f(inp["moe_b_up"]).reshape(2, NE, 16, 128), (0, 1, 3, 2))),
        w_down=f(inp["moe_w_down"]), b_down=f(inp["moe_b_down"]),
        identF=np.eye(128, dtype=np.float32), **tabs)
    x, c, ctx, c_ctx = f(inp["x"]), f(inp["c"]), f(inp["ctx"]), f(inp["c_ctx"])
    maps = []
    for b in range(nb):
        cv2 = np.ascontiguousarray(np.stack([_fm(c[b]), _fm(c_ctx)], -1))
        maps.append(dict(x=x[b], ctx=ctx[b], cv2=cv2, **shared))
    return maps


class B2(B):
    def load_hT(self, src_t, src_ap, xt, pT, hT, coef, s, ident, a_idx=0):
        self.dma(xt[:], src_ap, xt, reads=[src_t], writes=[xt])
        for half in range(2):
            for j in range(4):
                c = half * 4 + j
                self.tr(pT[half][:, j * 128:(j + 1) * 128], xt[:, c * 128:(c + 1) * 128], ident[:], [xt, ident], [pT[half]])
            for j in range(4):
                c = half * 4 + j
                self.act(hT[:, c, :], pT[half][:, j * 128:(j + 1) * 128], AF.Identity, [pT[half], coef], [hT],
                         bias=coef[:, a_idx + 1, s, c:c + 1], scale=coef[:, a_idx, s, c:c + 1])

    def rope(self, out, out_t, xin, C, S, nblk, h, tmp1, tmp2, reads):
        n = nblk * 2 * h
        v = lambda ap: ap.rearrange("p (b two h) -> p b two h", two=2, h=h)
        self.tt("dve", tmp1[:, 0:n], xin, C, ALU.mult, reads, [tmp1])
        self.tt("pool", v(tmp2[:, 0:n])[:, :, 0, :], v(xin)[:, :, 1, :], v(S)[:, :, 0, :], ALU.mult, reads, [tmp2])
        self.tt("pool", v(tmp2[:, 0:n])[:, :, 1, :], v(xin)[:, :, 0, :], v(S)[:, :, 1, :], ALU.mult, reads, [tmp2])
        self.tt("dve", out, tmp1[:, 0:n], tmp2[:, 0:n], ALU.add, [tmp1, tmp2], [out_t])

    def l0_passA(self):
        k = self.k
        NTA, TA = self.NTA, self.TA
        self.stA = ExitStack()
        self.QT = k.sbuf("QT", [128, 4, TA], BF16, self.stA)
        self.KT = k.sbuf("KT", [128, 2, TA], BF16, self.stA)
        self.VA = k.sbuf("VA", [128, NTA, 2, 130], BF16, self.stA)
        ps = ExitStack()
        wA = k.sbuf("wA", [128, 8, 1024], BF16, ps)
        self.dma(wA[:], self.ab_w_in[:, 0:1024].rearrange("(k p) n -> p k n", p=128), wA, writes=[wA], eng="pool")
        gq = k.sbuf("gq", [128, 2, 128], F32, ps)
        self.dma(gq[:, 0, :], self.ab_qk[0:1, :].partition_broadcast(128), gq, writes=[gq])
        self.dma(gq[:, 1, :], self.ab_qk[1:2, :].partition_broadcast(128), gq, writes=[gq])
        self.k.op("act", lambda e: e.mul(out=gq[:, 0, :], in_=gq[:, 0, :], mul=128.0 ** -0.5), [gq], [gq])
        self.memset("pool", self.VA[:], 1.0, [self.VA])
        xt = [k.sbuf("xt", [128, D], F32, ps) for _ in range(2)]
        hT = [k.sbuf("hT", [128, 8, 128], BF16, ps) for _ in range(2)]
        tab = [k.sbuf("tabA", [128, 2, 128], F32, ps) for _ in range(2)]
        ss = k.sbuf("ss", [128, 8], F32, ps)
        rstd = k.sbuf("rstd", [128, 8], F32, ps)
        junk = k.sbuf("junk", [128, 128], F32, ps)
        xn = [k.sbuf("xn", [128, 128], F32, ps) for _ in range(2)]
        t1 = [k.sbuf("t1", [128, 128], F32, ps) for _ in range(2)]
        t2 = [k.sbuf("t2", [128, 128], F32, ps) for _ in range(2)]
        ob = [k.sbuf("ob", [128, 128], BF16, ps) for _ in range(2)]
        pT = [k.psum("pT", [128, 512], F32, ps) for _ in range(2)]
        pA = [k.psum("pA", [128, 1024], F32, ps) for _ in range(2)]
        pTb = k.psum("pTb", [128, 1024], BF16, ps)
        coef = self.coef[0]
        for tt in range(NTA):
            s = 1 if tt < self.NTC else 0
            src_t, src_ap = self.xsrc(tt)
            x_, h_, tb, pa = xt[tt % 2], hT[tt % 2], tab[tt % 2], pA[tt % 2]
            self.load_hT(src_t, src_ap, x_, pT, h_, coef, s, self.identF)
            self.dma(self.hT0[tt], h_[:], h_, reads=[h_], writes=[self.hT0])
            self.dma(tb[:], self.ropeA[tt * 128:(tt + 1) * 128], tb, writes=[tb])
            for n in range(2):
                for c in range(8):
                    self.mm(pa[:, n * 512:(n + 1) * 512], h_[:, c, :], wA[:, c, n * 512:(n + 1) * 512], c == 0, c == 7, [h_, wA], [pa])
            for hh in range(6):
                self.act(junk[:], pa[:, hh * 128:(hh + 1) * 128], AF.Square, [pa], [junk, ss], accum_out=ss[:, hh:hh + 1])
            self.act(rstd[:, 0:6], ss[:, 0:6], AF.Sqrt, [ss], [rstd], bias=1e-6, scale=1.0 / 128)
            self.recip(rstd[:, 0:6], rstd[:, 0:6], [rstd], [rstd])
            for hh in range(6):
                i2 = hh % 2
                g = gq[:, 0, :] if hh < 4 else gq[:, 1, :]
                self.stt("dve", xn[i2][:], pa[:, hh * 128:(hh + 1) * 128], rstd[:, hh:hh + 1], g, ALU.mult, ALU.mult, [pa, rstd, gq], [xn[i2]])
                self.rope(ob[i2][:], ob[i2], xn[i2][:], tb[:, 0, :], tb[:, 1, :], 2, 32, t1[i2], t2[i2], [xn[i2], tb])
                self.tr(pTb[:, hh * 128:(hh + 1) * 128], ob[i2][:], self.identB[:], [ob[i2], self.identB], [pTb])
            self.cp("act", self.QT[:, :, tt * 128:(tt + 1) * 128], pTb[:, 0:512].rearrange("p (h t) -> p h t", h=4), [pTb], [self.QT])
            self.cp("act", self.KT[:, :, tt * 128:(tt + 1) * 128], pTb[:, 512:768].rearrange("p (h t) -> p h t", h=2), [pTb], [self.KT])
            self.cp("dve", self.VA[:, tt, :, 0:128], pa[:, 768:1024].rearrange("p (h d) -> p h d", h=2), [pa], [self.VA])
        k.flush()
        ps.close()

    def attention(self, parts, V, vres, q_ranges, cat_col, bufs, exp_bias=None):
        pS, pO, PT, osb, rec = bufs
        np_ = len(parts)
        it = 0
        for (q0, nq, ktiles) in q_ranges:
            nqb = nq // 128
            for ki, kt in enumerate(ktiles):
                sp = pS[it % 2]
                pt = PT[it % 2]
                it += 1
                for pi, (Qap, Kap, qres, kres) in enumerate(parts):
                    self.mm(sp[:, 0:nq], Kap(kt), Qap(q0, nq), pi == 0, pi == np_ - 1, [qres, kres], [sp])
                if exp_bias is None:
                    self.act(pt[:, 0:nq], sp[:, 0:nq], AF.Exp, [sp], [pt])
                else:
                    self.act(pt[:, 0:nq], sp[:, 0:nq], AF.Exp, [sp, exp_bias[1]], [pt], bias=exp_bias[0])
                for qb in range(nqb):
                    self.mm(pO[qb][:, 0:129], pt[:, qb * 128:(qb + 1) * 128], V(kt), ki == 0, ki == len(ktiles) - 1, [pt, vres], [pO[qb]])
            ob = osb[(q0 // 512) % 2]
            for qb in range(nqb):
                self.recip(rec[:, qb:qb + 1], pO[qb][:, 128:129], [pO[qb]], [rec])
                self.act(ob[:, qb, :], pO[qb][:, 0:128], AF.Copy, [pO[qb], rec], [ob], scale=rec[:, qb:qb + 1])
            self.dma(self.cat[q0:q0 + nq, cat_col:cat_col + 128].rearrange("(qb p) c -> p qb c", p=128), ob[:, 0:nqb, :], ob,
                     reads=[ob], writes=[self.cat])

    def attn_bufs(self, ps):
        k = self.k
        pS = [k.psum("pS", [128, 512], F32, ps) for _ in range(2)]
        pO = [k.psum("pO", [128, 512], F32, ps) for _ in range(4)]
        PT = [k.sbuf("PT", [128, 512], BF16, ps) for _ in range(2)]
        osb = [k.sbuf("osb", [128, 4, 128], BF16, ps) for _ in range(2)]
        rec = k.sbuf("rec", [128, 4], F32, ps)
        return pS, pO, PT, osb, rec

    def q_chunks(self, q0, q1, ktiles):
        out = []
        q = q0
        while q < q1:
            n = min(512, q1 - q)
            out.append((q, n, ktiles))
            q += n
        return out

    def l0_attnA(self):
        ps = ExitStack()
        bufs = self.attn_bufs(ps)
        TC, TA, NTC, NTA = self.TC, self.TA, self.NTC, self.NTA
        for h in range(4):
            kvh = h // 2
            parts = [(lambda q0, n, h=h: self.QT[:, h, q0:q0 + n], lambda kt, kvh=kvh: self.KT[:, kvh, kt * 128:(kt + 1) * 128], self.QT, self.KT)]
            V = lambda kt, kvh=kvh: self.VA[:, kt, kvh, 0:129]
            ranges = self.q_chunks(0, TC, list(range(NTC))) + self.q_chunks(TC, TA, list(range(NTA)))
            self.attention(parts, V, self.VA, ranges, h * 128, bufs)
        self.k.flush()
        ps.close()
        self.stA.close()

    def l0_passB(self):
        k = self.k
        NTA = self.NTA
        ps = ExitStack()
        wB = k.sbuf("wB", [128, 8, 2048], BF16, ps)
        for hf in range(2):
            self.dma(wB[:, :, hf * 1024:(hf + 1) * 1024], self.ab_w_in[:, 1024 + hf * 1024:2048 + hf * 1024].rearrange("(k p) n -> p k n", p=128),
                     wB, writes=[wB], eng="pool")
        gnB = k.sbuf("gnB", [128, 512], F32, ps)
        self.dma(gnB[:], self.ab_gn[0:1, :].partition_broadcast(128), gnB, writes=[gnB])
        hT = [k.sbuf("hTb", [128, 8, 128], BF16, ps) for _ in range(2)]
        tab = [k.sbuf("tabB", [128, 2, 512], F32, ps) for _ in range(2)]
        xs = [k.sbuf("xsB", [128, 512], F32, ps) for _ in range(2)]
        t1 = k.sbuf("t1B", [128, 512], F32, ps)
        t2 = k.sbuf("t2B", [128, 512], F32, ps)
        qr = [k.sbuf("qrB", [128, 512], BF16, ps) for _ in range(2)]
        kr = [k.sbuf("krB", [128, 512], BF16, ps) for _ in range(2)]
        vb = [k.sbuf("vbB", [128, 512], BF16, ps) for _ in range(2)]
        gb = [k.sbuf("gbB", [128, 512], BF16, ps) for _ in range(2)]
        gs = k.sbuf("gsB", [128, 512], F32, ps)
        stg = [k.sbuf("stgB", [128, 8, 128], BF16, ps) for _ in range(2)]
        pB = k.psum("pB", [128, 2048], F32, ps)
        pTb = k.psum("pTbB", [128, 1024], BF16, ps)
        for tt in range(NTA):
            i2 = tt % 2
            h_, tb = hT[i2], tab[i2]
            rows = slice(tt * 128, (tt + 1) * 128)
            self.dma(h_[:], self.hT0[tt], h_, reads=[self.hT0], writes=[h_])
            self.dma(tb[:], self.ropeB[rows], tb, writes=[tb])
            for n in range(4):
                for c in range(8):
                    self.mm(pB[:, n * 512:(n + 1) * 512], h_[:, c, :], wB[:, c, n * 512:(n + 1) * 512], c == 0, c == 7, [h_, wB], [pB])
            self.act(xs[0][:], pB[:, 0:512], AF.Copy, [pB], [xs[0]], scale=128.0 ** -0.5)
            self.rope(qr[i2][:], qr[i2], xs[0][:], tb[:, 0, :], tb[:, 1, :], 4, 64, t1, t2, [xs[0], tb])
            self.act(xs[1][:], pB[:, 512:1024], AF.Copy, [pB], [xs[1]])
            self.rope(kr[i2][:], kr[i2], xs[1][:], tb[:, 0, :], tb[:, 1, :], 4, 64, t1, t2, [xs[1], tb])
            self.dma(self.kB[rows, :], kr[i2][:], kr[i2], reads=[kr[i2]], writes=[self.kB])
            for hh in range(4):
                self.tr(pTb[:, hh * 128:(hh + 1) * 128], qr[i2][:, hh * 128:(hh + 1) * 128], self.identB[:], [qr[i2], self.identB], [pTb])
                self.tr(pTb[:, 512 + hh * 128:512 + (hh + 1) * 128], kr[i2][:, hh * 128:(hh + 1) * 128], self.identB[:], [kr[i2], self.identB], [pTb])
            self.cp("act", stg[i2][:], pTb[:].rearrange("p (h t) -> p h t", h=8), [pTb], [stg[i2]])
            self.dma(self.qTB[:, :, rows].rearrange("h d t -> d h t"), stg[i2][:, 0:4, :], stg[i2], reads=[stg[i2]], writes=[self.qTB])
            self.dma(self.kTB[:, :, rows].rearrange("h d t -> d h t"), stg[i2][:, 4:8, :], stg[i2], reads=[stg[i2]], writes=[self.kTB])
            self.cp("dve", vb[i2][:], pB[:, 1024:1536], [pB], [vb[i2]])
            self.dma(self.vB[rows, :], vb[i2][:], vb[i2], reads=[vb[i2]], writes=[self.vB])
            self.act(gs[:], pB[:, 1536:2048], AF.Silu, [pB], [gs])
            self.tt("dve", gb[i2][:], gs[:], gnB[:], ALU.mult, [gs, gnB], [gb[i2]])
            self.dma(self.gB[rows, :], gb[i2][:], gb[i2], reads=[gb[i2]], writes=[self.gB])
        k.flush()
        ps.close()

    def l0_retention(self):
        k = self.k
        NTA, NTC, TA = self.NTA, self.NTC, self.TA
        ps = ExitStack()
        ld = k.sbuf("ld", [128, 8], F32, ps)
        self.dma(ld[:], self.ab_ld[0:1, :].partition_broadcast(128), ld, writes=[ld])
        lg = k.sbuf("lg", [128, 8], F32, ps)
        nlg = k.sbuf("nlg", [128, 8], F32, ps)
        self.act(lg[:], ld[:], AF.Exp, [ld], [lg])
        self.ts("dve", lg[:], lg[:], -1.0, 1.0, ALU.mult, ALU.add, [lg], [lg])
        self.act(lg[:], lg[:], AF.Ln, [lg], [lg])
        self.ts("dve", nlg[:], lg[:], -1.0, None, ALU.mult, None, [lg], [nlg])
        ii = k.sbuf("ii", [128, 128], I32, ps)
        dif = k.sbuf("dif", [128, 128], F32, ps)
        c1 = k.sbuf("c1", [128, 128], F32, ps)
        c128 = k.sbuf("c128", [128, 128], F32, ps)
        pcol = k.sbuf("pcol", [128, 4], F32, ps)
        k.op("pool", lambda e: e.iota(ii[:], [[1, 128]], base=0, channel_multiplier=-1), (), [ii])
        self.cp("dve", dif[:], ii[:], [ii], [dif])
        k.op("pool", lambda e: e.iota(ii[:], [[1, 128]], base=1, channel_multiplier=0), [dif], [ii])
        self.cp("dve", c1[:], ii[:], [ii], [c1])
        self.ts("dve", c128[:], c1[:], -1.0, 129.0, ALU.mult, ALU.add, [c1], [c128])
        k.op("pool", lambda e: e.iota(ii[:, 0:1], [[1, 1]], base=0, channel_multiplier=1), [c1], [ii])
        self.cp("dve", pcol[:, 0:1], ii[:, 0:1], [ii], [pcol])
        self.ts("dve", pcol[:, 1:2], pcol[:, 0:1], -1.0, 127.0, ALU.mult, ALU.add, [pcol], [pcol])
        self.memset("dve", pcol[:, 2:3], 128.0, [pcol])
        mf = k.sbuf("mf", [128, 128], F32, ps)
        mb = k.sbuf("mb", [128, 128], F32, ps)
        self.ts("dve", mf[:], dif[:], 0.0, None, ALU.is_ge, None, [dif], [mf])
        self.ts("dve", mb[:], dif[:], 0.0, None, ALU.is_lt, None, [dif], [mb])
        DT = k.sbuf("DT", [128, 128], F32, ps)
        DT2 = k.sbuf("DT2", [128, 128], F32, ps)
        xiF = k.sbuf("xiF", [128, 128], F32, ps)
        xiB = k.sbuf("xiB", [128, 128], F32, ps)
        zc = k.sbuf("zc", [128, 4], F32, ps)
        qT = k.sbuf("qTr", [128, TA], BF16, ps)
        kT = k.sbuf("kTr", [128, TA], BF16, ps)
        kk = k.sbuf("kkr", [128, NTA, 128], BF16, ps)
        vv = k.sbuf("vvr", [128, NTA, 128], BF16, ps)
        gg = k.sbuf("ggr", [128, NTA, 128], BF16, ps)
        Kzf = k.sbuf("Kzf", [128, NTA, 128], BF16, ps)
        Kzb = k.sbuf("Kzb", [128, NTA, 128], BF16, ps)
        SfP = k.sbuf("SfP", [128, NTA, 128], BF16, ps)
        SbP = k.sbuf("SbP", [128, NTA, 128], BF16, ps)
        Sf = k.sbuf("Sf", [128, 128], F32, ps)
        Sb = k.sbuf("Sb", [128, 128], F32, ps)
        Pm = [k.sbuf("Pm", [128, 128], BF16, ps) for _ in range(2)]
        qxf = [k.sbuf("qxf", [128, 128], BF16, ps) for _ in range(2)]
        qxb = [k.sbuf("qxb", [128, 128], BF16, ps) for _ in range(2)]
        st6 = k.sbuf("st6", [128, 6], F32, ps)
        mv = k.sbuf("mv", [128, 4], F32, ps)
        on = [k.sbuf("on", [128, 128], F32, ps) for _ in range(2)]
        ystg = k.sbuf("ystg", [128, NTA, 128], BF16, ps)
        pkv = [k.psum("pkv", [128, 512], F32, ps) for _ in range(2)]
        psc = [k.psum("psc", [128, 512], F32, ps) for _ in range(2)]
        pO = [k.psum("pOr", [128, 512], F32, ps) for _ in range(2)]
        order_b = list(range(NTC - 1, -1, -1)) + list(range(NTA - 1, NTC - 1, -1))
        for h in range(4):
            hs = slice(h * 128, (h + 1) * 128)
            self.dma(qT[:], self.qTB[h], qT, reads=[self.qTB], writes=[qT])
            self.dma(kT[:], self.kTB[h], kT, reads=[self.kTB], writes=[kT])
            for n0 in range(0, NTA, 8):
                n1 = min(NTA, n0 + 8)
                rs = slice(n0 * 128, n1 * 128)
                self.dma(kk[:, n0:n1, :], self.kB[rs, hs].rearrange("(n p) d -> p n d", p=128), kk, reads=[self.kB], writes=[kk])
                self.dma(vv[:, n0:n1, :], self.vB[rs, hs].rearrange("(n p) d -> p n d", p=128), vv, reads=[self.vB], writes=[vv])
                self.dma(gg[:, n0:n1, :], self.gB[rs, hs].rearrange("(n p) d -> p n d", p=128), gg, reads=[self.gB], writes=[gg])
            lf, lb, nlb = lg[:, h:h + 1], lg[:, 4 + h:5 + h], nlg[:, 4 + h:5 + h]
            self.act(DT[:], dif[:], AF.Exp, [dif, lg], [DT], scale=lf)
            self.tt("dve", DT[:], DT[:], mf[:], ALU.mult, [DT, mf], [DT])
            self.act(DT2[:], dif[:], AF.Exp, [dif, nlg], [DT2], scale=nlb)
            self.tt("dve", DT2[:], DT2[:], mb[:], ALU.mult, [DT2, mb], [DT2])
            self.tt("dve", DT[:], DT[:], DT2[:], ALU.add, [DT, DT2], [DT])
            self.act(xiF[:], c1[:], AF.Exp, [c1, lg], [xiF], scale=lf)
            self.act(xiB[:], c128[:], AF.Exp, [c128, lg], [xiB], scale=lb)
            self.act(zc[:, 0:1], pcol[:, 1:2], AF.Exp, [pcol, lg], [zc], scale=lf)
            self.act(zc[:, 1:2], pcol[:, 0:1], AF.Exp, [pcol, lg], [zc], scale=lb)
            self.act(zc[:, 2:3], pcol[:, 2:3], AF.Exp, [pcol, lg], [zc], scale=lf)
            self.act(zc[:, 3:4], pcol[:, 2:3], AF.Exp, [pcol, lg], [zc], scale=lb)
            self.ts("dve", Kzf[:], kk[:], zc[:, 0:1], None, ALU.mult, None, [kk, zc], [Kzf])
            self.ts("pool", Kzb[:], kk[:], zc[:, 1:2], None, ALU.mult, None, [kk, zc], [Kzb])
            self.memset("dve", Sf[:], 0.0, [Sf])
            self.memset("pool", Sb[:], 0.0, [Sb])
            for it, n in enumerate(range(NTA)):
                p_ = pkv[it % 2]
                self.cp("act", SfP[:, n, :], Sf[:], [Sf], [SfP])
                self.mm(p_[:, 0:128], Kzf[:, n, :], vv[:, n, :], True, True, [Kzf, vv], [p_])
                self.stt("dve", Sf[:], Sf[:], zc[:, 2:3], p_[:, 0:128], ALU.mult, ALU.add, [Sf, zc, p_], [Sf])
            for it, n in enumerate(order_b):
                p_ = pkv[it % 2]
                self.cp("act", SbP[:, n, :], Sb[:], [Sb], [SbP])
                self.mm(p_[:, 0:128], Kzb[:, n, :], vv[:, n, :], True, True, [Kzb, vv], [p_])
                self.stt("dve", Sb[:], Sb[:], zc[:, 3:4], p_[:, 0:128], ALU.mult, ALU.add, [Sb, zc, p_], [Sb])
            for n in range(NTA):
                i2 = n % 2
                cs = slice(n * 128, (n + 1) * 128)
                self.mm(psc[i2][:, 0:128], kT[:, cs], qT[:, cs], True, True, [kT, qT], [psc[i2]])
                self.tt("dve", Pm[i2][:], psc[i2][:, 0:128], DT[:], ALU.mult, [psc[i2], DT], [Pm[i2]])
                self.tt("pool", qxf[i2][:], qT[:, cs], xiF[:], ALU.mult, [qT, xiF], [qxf[i2]])
                self.tt("pool", qxb[i2][:], qT[:, cs], xiB[:], ALU.mult, [qT, xiB], [qxb[i2]])
                o_ = pO[i2]
                self.mm(o_[:, 0:128], Pm[i2][:], vv[:, n, :], True, False, [Pm[i2], vv], [o_])
                self.mm(o_[:, 0:128], qxf[i2][:], SfP[:, n, :], False, False, [qxf[i2], SfP], [o_])
                self.mm(o_[:, 0:128], qxb[i2][:], SbP[:, n, :], False, True, [qxb[i2], SbP], [o_])
                k.op("dve", lambda e, o_=o_: e.bn_stats(out=st6[:], in_=o_[:, 0:128]), [o_], [st6])
                k.op("dve", lambda e: e.bn_aggr(out=mv[:, 0:2], in_=st6[:]), [st6], [mv])
                self.act(mv[:, 2:3], mv[:, 1:2], AF.Sqrt, [mv], [mv], bias=1e-5)
                self.recip(mv[:, 2:3], mv[:, 2:3], [mv], [mv])
                self.stt("dve", mv[:, 3:4], mv[:, 0:1], -1.0, mv[:, 2:3], ALU.mult, ALU.mult, [mv], [mv])
                self.act(on[i2][:], o_[:, 0:128], AF.Identity, [o_, mv], [on[i2]], bias=mv[:, 3:4], scale=mv[:, 2:3])
                self.tt("pool", ystg[:, n, :], on[i2][:], gg[:, n, :], ALU.mult, [on[i2], gg], [ystg])
            for n0 in range(0, NTA, 8):
                n1 = min(NTA, n0 + 8)
                self.dma(self.cat[n0 * 128:n1 * 128, 512 + h * 128:512 + (h + 1) * 128].rearrange("(n p) c -> p n c", p=128), ystg[:, n0:n1, :], ystg,
                         reads=[ystg], writes=[self.cat])
        k.flush()
        ps.close()

    def layernorm(self, zn, z, st, mv, eps=1e-5):
        k = self.k
        for hf in range(2):
            k.op("dve", lambda e, hf=hf: e.bn_stats(out=st[:, hf * 6:(hf + 1) * 6], in_=z[:, hf * 512:(hf + 1) * 512]), [z], [st])
        k.op("dve", lambda e: e.bn_aggr(out=mv[:, 0:2], in_=st[:, 0:12]), [st], [mv])
        self.act(mv[:, 2:3], mv[:, 1:2], AF.Sqrt, [mv], [mv], bias=eps)
        self.recip(mv[:, 2:3], mv[:, 2:3], [mv], [mv])
        self.stt("dve", mv[:, 3:4], mv[:, 0:1], -1.0, mv[:, 2:3], ALU.mult, ALU.mult, [mv], [mv])
        self.act(zn[:], z[:], AF.Identity, [z, mv], [zn], bias=mv[:, 3:4], scale=mv[:, 2:3])

    def resid_src(self, i, tt):
        if i == 0:
            return self.xsrc(tt)
        return self.x2, self.x2[tt * 128:(tt + 1) * 128, :]

    def post_mixer(self, i, w_out, tiles):
        k = self.k
        ps = ExitStack()
        wo = k.sbuf("wo", [128, 8, D], BF16, ps)
        self.dma(wo[:], w_out[:, :].rearrange("(k p) n -> p k n", p=128), wo, writes=[wo], eng="pool")
        rw = k.sbuf("rw", [128, 8, NE], F32, ps)
        self.dma(rw[:], self.r_w[i].rearrange("(k p) e -> p k e", p=128), rw, writes=[rw])
        rb = k.sbuf("rb", [1, NE], F32, ps)
        self.dma(rb[:], self.r_b[i], rb, writes=[rb])
        lnB = k.sbuf("lnB", [128, 2, D], F32, ps)
        for q in range(2):
            self.dma(lnB[:, q, :], self.ln_rows[i][q:q + 1, :].partition_broadcast(128), lnB, writes=[lnB])
        gB = k.sbuf("g2B", [128, 2, D], F32, ps)
        for s in range(2):
            self.dma(gB[:, s, :], self.gate_rows[i, 0, s:s + 1, :].partition_broadcast(128), gB, reads=[self.gate_rows], writes=[gB])
        ct = [k.sbuf("ct", [128, D], BF16, ps) for _ in range(2)]
        catT = k.sbuf("catT", [128, 8, 128], BF16, ps)
        xt = [k.sbuf("xtp", [128, D], F32, ps) for _ in range(2)]
        z = k.sbuf("z", [128, D], F32, ps)
        zn = k.sbuf("zn", [128, D], F32, ps)
        xl = [k.sbuf("xl", [128, D], F32, ps) for _ in range(2)]
        st = k.sbuf("st", [128, 12], F32, ps)
        mv = k.sbuf("mvp", [128, 4], F32, ps)
        h2f = k.sbuf("h2f", [128, 8, 128], F32, ps)
        h2b = [k.sbuf("h2b", [128, 8, 128], BF16, ps) for _ in range(2)]
        lgt = k.sbuf("lgt", [128, NE], F32, ps)
        m8 = k.sbuf("m8", [128, 8], F32, ps)
        msk = k.sbuf("msk", [128, NE], F32, ps)
        ex = k.sbuf("ex", [128, NE], F32, ps)
        den = k.sbuf("den", [128, 2], F32, ps)
        pTb = k.psum("pTbp", [128, 1024], BF16, ps)
        pY = k.psum("pYp", [128, 1024], F32, ps)
        pT = [k.psum("pTp", [128, 512], F32, ps) for _ in range(2)]
        pR = k.psum("pR", [128, 512], F32, ps)
        coef = self.coef[i]
        G = self.G[i]
        for it, tt in enumerate(tiles):
            i2 = it % 2
            s = 1 if tt < self.NTC else 0
            rows = slice(tt * 128, (tt + 1) * 128)
            c_ = ct[i2]
            self.dma(c_[:], self.cat[rows, :], c_, reads=[self.cat], writes=[c_])
            for c in range(8):
                self.tr(pTb[:, c * 128:(c + 1) * 128], c_[:, c * 128:(c + 1) * 128], self.identB[:], [c_, self.identB], [pTb])
            self.cp("act", catT[:], pTb[:].rearrange("p (c t) -> p c t", c=8), [pTb], [catT])
            for n in range(2):
                for c in range(8):
                    self.mm(pY[:, n * 512:(n + 1) * 512], catT[:, c, :], wo[:, c, n * 512:(n + 1) * 512], c == 0, c == 7, [catT, wo], [pY])
            src_t, src_ap = self.resid_src(i, tt)
            x_ = xt[i2]
            self.dma(x_[:], src_ap, x_, reads=[src_t], writes=[x_])
            self.tt("dve", z[:], pY[:], gB[:, s, :], ALU.mult, [pY, gB], [z])
            self.stt("dve", z[:], x_[:], DN_ALPHA, z[:], ALU.mult, ALU.add, [x_, z], [z])
            self.layernorm(zn, z, st, mv)
            x1_ = xl[i2]
            self.tt("pool", x1_[:], zn[:], lnB[:, 0, :], ALU.mult, [zn, lnB], [x1_])
            self.tt("pool", x1_[:], x1_[:], lnB[:, 1, :], ALU.add, [x1_, lnB], [x1_])
            self.dma(self.x1[rows, :], x1_[:], x1_, reads=[x1_], writes=[self.x1])
            for half in range(2):
                for j in range(4):
                    c = half * 4 + j
                    self.tr(pT[half][:, j * 128:(j + 1) * 128], zn[:, c * 128:(c + 1) * 128], self.identF[:], [zn, self.identF], [pT[half]])
                for j in range(4):
                    c = half * 4 + j
                    self.act(h2f[:, c, :], pT[half][:, j * 128:(j + 1) * 128], AF.Identity, [pT[half], coef], [h2f],
                             bias=coef[:, 3, s, c:c + 1], scale=coef[:, 2, s, c:c + 1])
            hb = h2b[i2]
            self.cp("pool", hb[:], h2f[:], [h2f], [hb])
            self.dma(self.h2T[tt], hb[:], hb, reads=[hb], writes=[self.h2T])
            for c in range(8):
                self.mm(pR[:, 0:NE], h2f[:, c, :], rw[:, c, :], c == 0, False, [h2f, rw], [pR])
            self.mm(pR[:, 0:NE], self.ones[0:1, 0:128], rb[:], False, True, [self.ones, rb], [pR])
            self.cp("act", lgt[:], pR[:, 0:NE], [pR], [lgt])
            k.op("dve", lambda e: e.max(out=m8[:], in_=lgt[:]), [lgt], [m8])
            self.ts("dve", msk[:], lgt[:], m8[:, 3:4], None, ALU.is_ge, None, [lgt, m8], [msk])
            self.ts("dve", den[:, 0:1], m8[:, 0:1], -1.0, None, ALU.mult, None, [m8], [den])
            self.act(ex[:], lgt[:], AF.Exp, [lgt, den], [ex], bias=den[:, 0:1])
            self.tt("dve", ex[:], ex[:], msk[:], ALU.mult, [ex, msk], [ex])
            k.op("dve", lambda e: e.reduce_sum(out=den[:, 1:2], in_=ex[:], axis=AX.X), [ex], [den])
            self.recip(den[:, 1:2], den[:, 1:2], [den], [den])
            self.ts("dve", G[:, tt, :], ex[:], den[:, 1:2], None, ALU.mult, None, [ex, den], [G])
        k.flush()
        ps.close()

    def moe(self, i, tiles, out_fn, GT=4):
        k = self.k
        G = self.G[i]
        groups = [tiles[j:j + GT] for j in range(0, len(tiles), GT)]
        gps = ExitStack()
        bd = k.sbuf("bd", [NE, D], F32, gps)
        self.dma(bd[:], self.b_down[i], bd, writes=[bd])
        for grp in groups:
            ng = len(grp)
            N = ng * 128
            ps = ExitStack()
            H = k.sbuf("H", [128, 8, GT * 128], BF16, ps)
            facc = k.sbuf("facc", [128, GT, D], F32, ps)
            gT = k.sbuf("gT", [NE, 128], F32, ps)
            for j, tt in enumerate(grp):
                self.dma(H[:, :, j * 128:(j + 1) * 128], self.h2T[tt], H, reads=[self.h2T], writes=[H])
            ins = ExitStack()
            wu = [k.sbuf("wu", [128, 8, 2 * D], BF16, ins) for _ in range(2)]
            wd = [k.sbuf("wd", [128, 8, D], BF16, ins) for _ in range(2)]
            bu = [k.sbuf("bu", [128, 16], F32, ins) for _ in range(2)]
            actT = k.sbuf("actT", [128, 8, GT * 128], BF16, ins)
            glu = [k.sbuf("glu", [128, GT * 128], F32, ins) for _ in range(2)]
            sig = [k.sbuf("sig", [128, GT * 128], F32, ins) for _ in range(2)]
            l1 = [k.sbuf("l1", [128, GT * 128], F32, ins) for _ in range(2)]
            pU = [k.psum("pU", [128, 2, 512], F32, ins) for _ in range(2)]
            pY = [k.psum("pYm", [128, 1024], F32, ins) for _ in range(2)]
            for j, tt in enumerate(grp):
                py = pY[j % 2]
                self.tr(pU[0][0:NE, 0, 0:128], G[:, tt, :], self.identF[:], [G, self.identF], [pU[0]])
                self.cp("act", gT[:], pU[0][0:NE, 0, 0:128], [pU[0]], [gT])
                for n in range(2):
                    self.mm(py[:, n * 512:(n + 1) * 512], gT[:], bd[:, n * 512:(n + 1) * 512], True, True, [gT, bd], [py])
                self.cp("act", facc[:, j, :], py[:], [py], [facc])
            for e in range(NE):
                e2 = e % 2
                wu_, wd_, bu_ = wu[e2], wd[e2], bu[e2]
                for hf in range(2):
                    self.dma(wu_[:, :, hf * D:(hf + 1) * D], self.w_up[i, e][:, hf * D:(hf + 1) * D].rearrange("(k p) n -> p k n", p=128),
                             wu_, writes=[wu_], eng="pool")
                self.dma(wd_[:], self.w_down[i, e].rearrange("(k p) n -> p k n", p=128), wd_, writes=[wd_], eng="pool")
                self.dma(bu_[:], self.b_upF[i, e], bu_, writes=[bu_])
                self.ts("pool", bu_[:, 8:16], bu_[:, 8:16], 1.0, None, ALU.add, None, [bu_], [bu_])
                for m in range(8):
                    m2 = m % 2
                    pu = pU[m2]
                    for c in range(8):
                        self.mm(pu[:, 0, 0:N], wu_[:, c, m * 128:(m + 1) * 128], H[:, c, 0:N], c == 0, c == 7, [wu_, H], [pu])
                    for c in range(8):
                        self.mm(pu[:, 1, 0:N], wu_[:, c, D + m * 128:D + (m + 1) * 128], H[:, c, 0:N], c == 0, c == 7, [wu_, H], [pu])
                    g_, s_, l_ = glu[m2], sig[m2], l1[m2]
                    self.ts("dve", g_[:, 0:N], pu[:, 0, 0:N], bu_[:, m:m + 1], LIMIT, ALU.add, ALU.min, [pu, bu_], [g_])
                    self.act(s_[:, 0:N], g_[:, 0:N], AF.Sigmoid, [g_], [s_], scale=SW_ALPHA)
                    self.ts("dve", l_[:, 0:N], pu[:, 1, 0:N], bu_[:, 8 + m:9 + m], LIMIT + 1.0, ALU.add, ALU.min, [pu, bu_], [l_])
                    self.tt("pool", s_[:, 0:N], s_[:, 0:N], g_[:, 0:N], ALU.mult, [s_, g_], [s_])
                    self.stt("dve", actT[:, m, 0:N], l_[:, 0:N], 1.0 - LIMIT, s_[:, 0:N], ALU.max, ALU.mult, [l_, s_], [actT])
                for j, tt in enumerate(grp):
                    py = pY[j % 2]
                    for n in range(2):
                        for m in range(8):
                            self.mm(py[:, n * 512:(n + 1) * 512], actT[:, m, j * 128:(j + 1) * 128], wd_[:, m, n * 512:(n + 1) * 512], m == 0, m == 7,
                                    [actT, wd_], [py])
                    self.stt("dve", facc[:, j, :], py[:], G[:, tt, e:e + 1], facc[:, j, :], ALU.mult, ALU.add, [py, G, facc], [facc])
            k.flush()
            ins.close()
            ln = ExitStack()
            lnB = k.sbuf("lnB2", [128, 2, D], F32, ln)
            for q in range(2):
                self.dma(lnB[:, q, :], self.ln_rows[i][2 + q:3 + q, :].partition_broadcast(128), lnB, writes=[lnB])
            gB = k.sbuf("g5B", [128, 2, D], F32, ln)
            for s in range(2):
                self.dma(gB[:, s, :], self.gate_rows[i, 1, s:s + 1, :].partition_broadcast(128), gB, reads=[self.gate_rows], writes=[gB])
            x1t = [k.sbuf("x1t", [128, D], F32, ln) for _ in range(2)]
            z = k.sbuf("z2", [128, D], F32, ln)
            zn = k.sbuf("zn2", [128, D], F32, ln)
            ot = [k.sbuf("ot", [128, D], F32, ln) for _ in range(2)]
            st = k.sbuf("st2", [128, 12], F32, ln)
            mv = k.sbuf("mv2", [128, 4], F32, ln)
            for j, tt in enumerate(grp):
                s = 1 if tt < self.NTC else 0
                x_ = x1t[j % 2]
                self.dma(x_[:], self.x1[tt * 128:(tt + 1) * 128, :], x_, reads=[self.x1], writes=[x_])
                self.tt("dve", z[:], facc[:, j, :], gB[:, s, :], ALU.mult, [facc, gB], [z])
                self.stt("dve", z[:], x_[:], DN_ALPHA, z[:], ALU.mult, ALU.add, [x_, z], [z])
                self.layernorm(zn, z, st, mv)
                o_ = ot[j % 2]
                self.tt("pool", o_[:], zn[:], lnB[:, 0, :], ALU.mult, [zn, lnB], [o_])
                self.tt("pool", o_[:], o_[:], lnB[:, 1, :], ALU.add, [o_, lnB], [o_])
                dst_t, dst_ap = out_fn(tt)
                self.dma(dst_ap, o_[:], o_, reads=[o_], writes=[dst_t])
            k.flush()
            ln.close()
            ps.close()
        gps.close()

    def l1_passM(self):
        k = self.k
        NTA, NTC, TA = self.NTA, self.NTC, self.TA
        self.stM = ExitStack()
        self.qlatT = k.sbuf("qlatT", [128, 3, TA], BF16, self.stM)
        self.ckvT = k.sbuf("ckvT", [128, 2, TA], BF16, self.stM)
        self.kpeT = k.sbuf("kpeT", [128, TA], BF16, self.stM)
        ps = ExitStack()
        wM = k.sbuf("wM", [128, 8, 704], BF16, ps)
        self.dma(wM[:], self.mla_w_in[:, :].rearrange("(k p) n -> p k n", p=128), wM, writes=[wM], eng="pool")
        gn = k.sbuf("gnM", [128, 640], F32, ps)
        self.dma(gn[:, 0:384], self.mla_qn[0:1, :].partition_broadcast(128), gn, writes=[gn])
        self.dma(gn[:, 384:640], self.mla_kvn[0:1, :].partition_broadcast(128), gn, writes=[gn])
        xt = [k.sbuf("xtM", [128, D], F32, ps) for _ in range(2)]
        hT = [k.sbuf("hTM", [128, 8, 128], BF16, ps) for _ in range(2)]
        tab = [k.sbuf("tabM", [128, 2, 64], F32, ps) for _ in range(2)]
        ss5 = k.sbuf("ss5", [128, 8], F32, ps)
        ssq = k.sbuf("ssq", [128, 2], F32, ps)
        rsd = k.sbuf("rsd", [128, 2], F32, ps)
        xnf = k.sbuf("xnf", [128, 640], F32, ps)
        junk = k.sbuf("junkM", [128, 128], F32, ps)
        xn = [k.sbuf("xnM", [128, 640], BF16, ps) for _ in range(2)]
        kp = k.sbuf("kpM", [128, 64], F32, ps)
        t1 = k.sbuf("t1M", [128, 64], F32, ps)
        t2 = k.sbuf("t2M", [128, 64], F32, ps)
        kb = [k.sbuf("kbM", [128, 128], BF16, ps) for _ in range(2)]
        for _kb in kb:
            self.memset("pool", _kb[:], 0.0, [_kb])
        pT = [k.psum("pTM", [128, 512], F32, ps) for _ in range(2)]
        pM = k.psum("pM", [128, 1024], F32, ps)
        pTb = k.psum("pTbM", [128, 1024], BF16, ps)
        coef = self.coef[1]
        for tt in range(NTA):
            i2 = tt % 2
            s = 1 if tt < NTC else 0
            rows = slice(tt * 128, (tt + 1) * 128)
            x_, h_, tb = xt[i2], hT[i2], tab[i2]
            self.load_hT(self.x2, self.x2[rows, :], x_, pT, h_, coef, s, self.identF)
            self.dma(tb[:], self.ropeM[rows, :, 0:64], tb, writes=[tb])
            for c in range(8):
                self.mm(pM[:, 0:512], h_[:, c, :], wM[:, c, 0:512], c == 0, c == 7, [h_, wM], [pM])
            for c in range(8):
                self.mm(pM[:, 512:704], h_[:, c, :], wM[:, c, 512:704], c == 0, c == 7, [h_, wM], [pM])
            for c in range(5):
                self.act(junk[:, 0:128], pM[:, c * 128:(c + 1) * 128], AF.Square, [pM], [junk, ss5], accum_out=ss5[:, c:c + 1])
            self.tt("dve", ssq[:, 0:1], ss5[:, 0:1], ss5[:, 1:2], ALU.add, [ss5], [ssq])
            self.tt("dve", ssq[:, 0:1], ssq[:, 0:1], ss5[:, 2:3], ALU.add, [ss5, ssq], [ssq])
            self.tt("dve", ssq[:, 1:2], ss5[:, 3:4], ss5[:, 4:5], ALU.add, [ss5], [ssq])
            self.act(rsd[:, 0:1], ssq[:, 0:1], AF.Sqrt, [ssq], [rsd], bias=1e-6, scale=1.0 / 384)
            self.act(rsd[:, 1:2], ssq[:, 1:2], AF.Sqrt, [ssq], [rsd], bias=1e-6, scale=1.0 / 256)
            self.recip(rsd[:, 0:2], rsd[:, 0:2], [rsd], [rsd])
            x2_ = xn[i2]
            self.stt("dve", xnf[:, 0:384], pM[:, 0:384], rsd[:, 0:1], gn[:, 0:384], ALU.mult, ALU.mult, [pM, rsd, gn], [xnf])
            self.stt("dve", xnf[:, 384:640], pM[:, 384:640], rsd[:, 1:2], gn[:, 384:640], ALU.mult, ALU.mult, [pM, rsd, gn], [xnf])
            self.cp("pool", x2_[:], xnf[:], [xnf], [x2_])
            self.cp("act", kp[:], pM[:, 640:704], [pM], [kp])
            self.rope(kb[i2][:, 0:64], kb[i2], kp[:], tb[:, 0, :], tb[:, 1, :], 2, 16, t1, t2, [kp, tb])
            for c in range(5):
                self.tr(pTb[:, c * 128:(c + 1) * 128], x2_[:, c * 128:(c + 1) * 128], self.identB[:], [x2_, self.identB], [pTb])
            self.tr(pTb[:, 640:768], kb[i2][:], self.identB[:], [kb[i2], self.identB], [pTb])
            self.cp("act", self.qlatT[:, :, rows], pTb[:, 0:384].rearrange("p (c t) -> p c t", c=3), [pTb], [self.qlatT])
            self.cp("act", self.ckvT[:, :, rows], pTb[:, 384:640].rearrange("p (c t) -> p c t", c=2), [pTb], [self.ckvT])
            self.cp("act", self.kpeT[:, rows], pTb[:, 640:768], [pTb], [self.kpeT])
        k.flush()
        ps.close()

    def l1_attn(self):
        k = self.k
        NTA, NTC, TA, TC, TL = self.NTA, self.NTC, self.TA, self.TC, self.TL
        ps = ExitStack()
        wuq = k.sbuf("wuq", [128, 3, 1536], BF16, ps)
        self.dma(wuq[:], self.mla_w_uq[:, :].rearrange("(k p) n -> p k n", p=128), wuq, writes=[wuq], eng="pool")
        wukv = k.sbuf("wukv", [128, 2, 2048], BF16, ps)
        self.dma(wukv[:], self.mla_w_ukv[:, :].rearrange("(k p) n -> p k n", p=128), wukv, writes=[wukv], eng="pool")
        wuqr = k.sbuf("wuqr", [128, 3, 8, 128], BF16, ps)
        self.memset("pool", wuqr[:], 0.0, [wuqr])
        for c in range(3):
            self.dma(wuqr[:, c, :, 0:64], self.mla_w_uq[c * 128:(c + 1) * 128, :].rearrange("p (h c) -> p h c", c=192)[:, :, 128:192],
                     wuqr, writes=[wuqr], eng="pool")
        pmat = k.sbuf("pmat", [128, 128], F32, ps)
        self.dma(pmat[:], self.pmat_in[:, :], pmat, writes=[pmat])
        CT = k.sbuf("CTm", [128, 2, TA], F32, ps)
        self.dma(CT[:], self.ropeMT[:, :, :], CT, writes=[CT])
        KnT = k.sbuf("KnT", [128, TA], BF16, ps)
        Vh = k.sbuf("Vh", [128, NTA, 130], BF16, ps)
        QnT = k.sbuf("QnT", [128, TA], BF16, ps)
        QrT = k.sbuf("QrT", [128, TA], BF16, ps)
        raw = k.sbuf("rawq", [128, 512], F32, ps)
        u1 = k.sbuf("u1", [128, 512], F32, ps)
        u2 = k.sbuf("u2", [128, 512], F32, ps)
        self.memset("pool", Vh[:], 1.0, [Vh])
        pP = [k.psum("pP", [128, 512], F32, ps) for _ in range(2)]
        bufs = self.attn_bufs(ps)
        qscale = 192.0 ** -0.5
        chunks = lambda a, b_: [(q, min(512, b_ - q)) for q in range(a, b_, 512)]
        for h in range(8):
            kc, vc, qc, rc = h * 256, h * 256 + 128, h * 192, h * 192 + 128
            for ci, (t0, n) in enumerate(chunks(0, TA)):
                p_ = pP[ci % 2]
                for c in range(2):
                    self.mm(p_[:, 0:n], wukv[:, c, kc:kc + 128], self.ckvT[:, c, t0:t0 + n], c == 0, c == 1, [wukv, self.ckvT], [p_])
                self.cp("act", KnT[:, t0:t0 + n], p_[:, 0:n], [p_], [KnT])
            for tt in range(NTA):
                p_ = pP[tt % 2]
                for c in range(2):
                    self.mm(p_[:, 0:128], self.ckvT[:, c, tt * 128:(tt + 1) * 128], wukv[:, c, vc:vc + 128], c == 0, c == 1, [wukv, self.ckvT], [p_])
                self.cp("dve", Vh[:, tt, 0:128], p_[:, 0:128], [p_], [Vh])
            for ci, (t0, n) in enumerate(chunks(TC, TA)):
                p_ = pP[ci % 2]
                for c in range(3):
                    self.mm(p_[:, 0:n], wuq[:, c, qc:qc + 128], self.qlatT[:, c, t0:t0 + n], c == 0, c == 2, [wuq, self.qlatT], [p_])
                self.act(QnT[:, t0:t0 + n], p_[:, 0:n], AF.Copy, [p_], [QnT], scale=qscale)
                p2 = pP[(ci + 1) % 2]
                for c in range(3):
                    self.mm(p2[:, 0:n], wuqr[:, c, h, :], self.qlatT[:, c, t0:t0 + n], c == 0, c == 2, [wuqr, self.qlatT], [p2])
                self.act(raw[:, 0:n], p2[:, 0:n], AF.Copy, [p2], [raw], scale=qscale)
                self.mm(p2[:, 0:n], pmat[:], raw[:, 0:n], True, True, [pmat, raw], [p2])
                self.tt("dve", u1[:, 0:n], raw[:, 0:n], CT[:, 0, t0:t0 + n], ALU.mult, [raw, CT], [u1])
                self.tt("dve", u2[:, 0:n], p2[:, 0:n], CT[:, 1, t0:t0 + n], ALU.mult, [p2, CT], [u2])
                self.tt("dve", QrT[:, t0:t0 + n], u1[:, 0:n], u2[:, 0:n], ALU.add, [u1, u2], [QrT])
            parts = [(lambda q0, n: QnT[:, q0:q0 + n], lambda kt: KnT[:, kt * 128:(kt + 1) * 128], QnT, KnT),
                     (lambda q0, n: QrT[:, q0:q0 + n], lambda kt: self.kpeT[:, kt * 128:(kt + 1) * 128], QrT, self.kpeT)]
            V = lambda kt: Vh[:, kt, 0:129]
            self.attention(parts, V, Vh, self.q_chunks(TC, TA, list(range(NTA))), h * 128, bufs)
        k.flush()
        ps.close()
        self.stM.close()

    def build(self):
        self.declare()
        self.pmat_in = self.inp("pmat", [128, 128])
        self.ropeMT = self.inp("ropeMT", [128, 2, self.TA])
        self.consts()
        self.phase0()
        allt = list(range(self.NTA))
        latt = list(range(self.NTC, self.NTA))
        self.l0_passA(); self.l0_attnA(); self.l0_passB(); self.l0_retention()
        self.post_mixer(0, self.ab_w_out, allt)
        self.moe(0, allt, lambda tt: (self.x2, self.x2[tt * 128:(tt + 1) * 128, :]), GT=self.GT)
        if self.stop == "l0":
            return self.k.emit()
        self.l1_passM(); self.l1_attn()
        self.post_mixer(1, self.mla_w_out, latt)
        self.moe(1, latt, lambda tt: (self.y_out, self.y_out[(tt - self.NTC) * 128:(tt - self.NTC + 1) * 128, :]), GT=self.GT)
        return self.k.emit()


_TL, _TC, _NB = 4096, 256, 8


def kernel(**inputs):
    maps = prep_inputs(inputs, _TL, _TC, _NB)
    b = B2(_TL, _TC)
    nc = b.build()
    res = run_bass_kernel_spmd(nc, maps, core_ids=list(range(_NB)))
    return np.stack([np.asarray(r["y"], dtype=np.float32) for r in res.results], 0)
```

```python
import numpy as np
import ml_dtypes
from contextlib import ExitStack
import concourse.bass as bass
import concourse.mybir as mybir
from concourse.bass_utils import run_bass_kernel_spmd

F32 = mybir.dt.float32
BF16 = mybir.dt.bfloat16
I32 = mybir.dt.int32
AF = mybir.ActivationFunctionType
ALU = mybir.AluOpType
AX = mybir.AxisListType

D = 1024
GRID_W = 64
THETA = 10000.0
NE = 32
LIMIT = 7.0
SW_ALPHA = 1.702
DN_ALPHA = 4.0 ** 0.25


class SemSlot:
    __slots__ = ("sem", "count")

    def __init__(self):
        self.sem = None
        self.count = 0


class Res:
    __slots__ = ("name", "slot", "lastw", "readers", "lastdma")

    def __init__(self, name):
        self.name = name
        self.slot = {}
        self.lastw = None
        self.readers = []
        self.lastdma = None


class Op:
    __slots__ = ("eng", "fn", "deps", "marked", "event", "is_dma", "ei", "done")

    def __init__(self, eng, fn):
        self.eng = eng
        self.fn = fn
        self.deps = []
        self.marked = False
        self.event = None
        self.is_dma = False
        self.done = False


class T:
    __slots__ = ("ap", "r")

    def __init__(self, ap, r):
        self.ap = ap
        self.r = r

    def __getitem__(self, idx):
        return self.ap[idx]


def _rs(xs):
    return [x.r if isinstance(x, T) else x for x in xs]


class K:
    ENGS = ("pe", "act", "dve", "pool", "sp")

    def __init__(self):
        self.nc = bass.Bass("TRN2", target_bir_lowering=False)
        self.ops = []
        self.stack = ExitStack()
        self.last_on = {e: None for e in self.ENGS}
        self.owners = []
        self.free_slots = {"hw": [], "sw": []}
        self.n = 0
        self.esem = {e: self.stack.enter_context(self.nc.semaphore(f"s_{e}")) for e in self.ENGS}
        self.cnt = {e: 0 for e in self.ENGS}
        self.waited = {e: {} for e in self.ENGS}
        self.stats = dict(nops=0, nwait=0, ndrain=0, nsem=5)

    def res(self, name=None):
        self.n += 1
        return Res(f"{name or 'r'}{self.n}")

    def sbuf(self, name, shape, dt, stack=None):
        self.n += 1
        ap = (stack or self.stack).enter_context(self.nc.sbuf_tensor(f"{name}_{self.n}", list(shape), dt))
        return T(ap, self.res(name))

    def psum(self, name, shape, dt, stack=None):
        self.n += 1
        ap = (stack or self.stack).enter_context(self.nc.psum_tensor(f"{name}_{self.n}", list(shape), dt))
        return T(ap, self.res(name))

    def dram(self, name, shape, dt, kind="Internal"):
        return T(self.nc.dram_tensor(name, list(shape), dt, kind=kind).ap(), self.res(name))

    def _track(self, op, reads, writes):
        deps = op.deps
        for r in reads:
            if r.lastw is not None:
                deps.append(r.lastw)
            r.readers.append(op)
        for w in writes:
            if w.lastw is not None:
                deps.append(w.lastw)
            deps.extend(w.readers)
            w.lastw = op
            w.readers = []
        seen = set()
        out = []
        for d in deps:
            if d is op or d.done or id(d) in seen:
                continue
            seen.add(id(d))
            if d.eng == "pe" and op.eng == "pe" and not d.is_dma and not op.is_dma:
                continue
            out.append(d)
            if d.eng != op.eng or d.is_dma:
                d.marked = True
        op.deps = out
        self.ops.append(op)
        self.last_on[op.eng] = op

    def op(self, eng, fn, reads=(), writes=()):
        o = Op(eng, fn)
        self._track(o, _rs(reads), _rs(writes))
        return o

    def dma(self, eng, out, in_, owner, reads=(), writes=(), **kw):
        o = Op(eng, lambda e: e.dma_start(out=out, in_=in_, **kw))
        self._dma_common(o, owner, reads, writes)
        return o

    def dma_fn(self, eng, fn, owner, reads=(), writes=()):
        o = Op(eng, fn)
        self._dma_common(o, owner, reads, writes)
        return o

    def _dma_common(self, o, owner, reads, writes):
        owner = owner.r if isinstance(owner, T) else owner
        o.is_dma = True
        o.marked = True
        kind = "sw" if o.eng == "pool" else "hw"
        if kind not in owner.slot:
            fl = self.free_slots[kind]
            owner.slot[kind] = fl.pop() if fl else SemSlot()
            if owner not in self.owners:
                self.owners.append(owner)
        if owner.lastdma is not None and not owner.lastdma.done:
            o.deps.append(owner.lastdma)
        owner.lastdma = o
        sl = owner.slot[kind]
        sl.count += 16
        o.event = (sl, sl.count)
        self._track(o, _rs(reads), _rs(writes))

    def barrier(self):
        lasts = [self.last_on[e] for e in self.ENGS if self.last_on[e] is not None and not self.last_on[e].done]
        lasts += [o.lastdma for o in self.owners if o.lastdma is not None and not o.lastdma.done]
        for e in self.ENGS:
            o = Op(e, None)
            for d in lasts:
                if d.eng == e and not d.is_dma:
                    continue
                if d.fn is None and not d.is_dma:
                    continue
                o.deps.append(d)
                d.marked = True
            self.ops.append(o)
            self.last_on[e] = o

    def flush(self):
        self.barrier()
        nc = self.nc
        esem = self.esem
        for r in self.owners:
            for sl in r.slot.values():
                if sl.sem is None:
                    self.n += 1
                    sl.sem = self.stack.enter_context(nc.semaphore(f"d{self.n}"))
                    self.stats["nsem"] += 1
        for o in self.ops:
            if o.is_dma:
                o.event = (o.event[0].sem, o.event[1])
            elif o.marked:
                self.cnt[o.eng] += 1
                o.event = (esem[o.eng], self.cnt[o.eng])
        by_eng = {e: [o for o in self.ops if o.eng == e] for e in self.ENGS}
        for e in self.ENGS:
            for i, o in enumerate(by_eng[e]):
                o.ei = i

        def run(ename, eng):
            w = self.waited[ename]
            last_drain = -1
            for o in by_eng[ename]:
                need = {}
                drain = False
                for d in o.deps:
                    if d.eng == ename and not d.is_dma:
                        if d.fn is not None and d.ei > last_drain and o.ei - d.ei <= 8:
                            drain = True
                        continue
                    sem, val = d.event
                    key = id(sem)
                    if w.get(key, 0) >= val:
                        continue
                    if key not in need or need[key][1] < val:
                        need[key] = (sem, val)
                for key, (sem, val) in need.items():
                    eng.wait_ge(sem, val)
                    w[key] = val
                    self.stats["nwait"] += 1
                if drain or (o.fn is None and ename != "pe"):
                    eng.drain()
                    last_drain = o.ei - 1
                    self.stats["ndrain"] += 1
                if o.fn is None:
                    continue
                ins = o.fn(eng)
                if o.is_dma:
                    ins.then_inc(o.event[0], 16)
                elif o.marked:
                    ins.then_inc(esem[o.eng], 1)

        with nc.Block() as block:
            block.tensor(lambda e: run("pe", e))
            block.scalar(lambda e: run("act", e))
            block.vector(lambda e: run("dve", e))
            block.gpsimd(lambda e: run("pool", e))
            block.sync(lambda e: run("sp", e))
        self.stats["nops"] += len(self.ops)
        for o in self.ops:
            o.done = True
        self.ops = []
        for r in self.owners:
            for kind, sl in r.slot.items():
                self.free_slots[kind].append(sl)
            r.slot = {}
        self.owners = []

    def emit(self):
        self.flush()
        self.stack.close()
        return self.nc


def _rope_tables(pos, d):
    half = d // 2
    inv = (THETA ** (-np.arange(half, dtype=np.float32) / np.float32(half))).astype(np.float32)
    ang = pos.astype(np.float32)[:, None] * inv[None, :]
    c, s = np.cos(ang).astype(np.float32), np.sin(ang).astype(np.float32)
    return np.concatenate([c, c], 1), np.concatenate([-s, s], 1)


def _axial_tables(n_tok, d):
    rows = np.repeat(np.arange(n_tok // GRID_W), GRID_W)
    cols = np.tile(np.arange(GRID_W), n_tok // GRID_W)
    c1, s1 = _rope_tables(rows, d // 2)
    c2, s2 = _rope_tables(cols, d // 2)
    return np.concatenate([c1, c2], 1), np.concatenate([s1, s2], 1)


def make_tables(TL, TC):
    TA = TL + TC
    ca, sa = _axial_tables(TL, 128)
    CA = np.concatenate([np.ones((TC, 128), np.float32), ca], 0)
    SA = np.concatenate([np.zeros((TC, 128), np.float32), sa], 0)
    cb, sb = _rope_tables(np.arange(TA), 128)
    cm, sm = _axial_tables(TL, 64)
    CM = np.concatenate([np.ones((TC, 64), np.float32), cm], 0)
    SM = np.concatenate([np.zeros((TC, 64), np.float32), sm], 0)
    pm = np.zeros((128, 128), np.float32)
    for m in range(64):
        src = m + 16 if (m % 32) < 16 else m - 16
        pm[src, m] = 1.0
    rmt = np.zeros((128, 2, TA), np.float32)
    rmt[:64, 0] = CM.T
    rmt[:64, 1] = SM.T
    return dict(pmat=pm, ropeMT=rmt,
                ropeA=np.stack([CA, SA], 1).astype(np.float32),
                ropeB=np.stack([np.tile(cb, (1, 4)), np.tile(sb, (1, 4))], 1).astype(np.float32),
                ropeM=np.stack([np.tile(CM, (1, 8)), np.tile(SM, (1, 8))], 1).astype(np.float32))


class B:
    def __init__(self, TL, TC, debug=False, stop=None):
        self.k = K()
        self.TL, self.TC, self.TA = TL, TC, TL + TC
        self.NTL, self.NTC, self.NTA = TL // 128, TC // 128, (TL + TC) // 128
        self.debug = debug
        self.stop = stop
        self.GT = 12
        self.SC = 4

    def mm(self, out, lhsT, rhs, start, stop, reads, writes):
        self.k.op("pe", lambda e: e.matmul(out=out, lhsT=lhsT, rhs=rhs, start=start, stop=stop), reads, writes)

    def tr(self, out, in_, ident, reads, writes):
        self.k.op("pe", lambda e: e.transpose(out=out, in_=in_, identity=ident), reads, writes)

    def act(self, out, in_, func, reads, writes, bias=0.0, scale=1.0, accum_out=None):
        if accum_out is None:
            self.k.op("act", lambda e: e.activation(out=out, in_=in_, func=func, bias=bias, scale=scale), reads, writes)
        else:
            self.k.op("act", lambda e: e.activation(out=out, in_=in_, func=func, bias=bias, scale=scale, accum_out=accum_out), reads, writes)

    def ts(self, eng, out, in0, s1, s2, op0, op1, reads, writes):
        if s2 is None:
            self.k.op(eng, lambda e: e.tensor_scalar(out=out, in0=in0, scalar1=s1, scalar2=None, op0=op0), reads, writes)
        else:
            self.k.op(eng, lambda e: e.tensor_scalar(out=out, in0=in0, scalar1=s1, scalar2=s2, op0=op0, op1=op1), reads, writes)

    def tt(self, eng, out, in0, in1, op, reads, writes):
        self.k.op(eng, lambda e: e.tensor_tensor(out=out, in0=in0, in1=in1, op=op), reads, writes)

    def stt(self, eng, out, in0, scalar, in1, op0, op1, reads, writes):
        self.k.op(eng, lambda e: e.scalar_tensor_tensor(out=out, in0=in0, scalar=scalar, in1=in1, op0=op0, op1=op1), reads, writes)

    def cp(self, eng, out, in_, reads, writes):
        if eng == "act":
            self.k.op("act", lambda e: e.copy(out=out, in_=in_), reads, writes)
        else:
            self.k.op(eng, lambda e: e.tensor_copy(out=out, in_=in_), reads, writes)

    def memset(self, eng, ap, val, writes):
        self.k.op(eng, lambda e: e.memset(ap, val), (), writes)

    def recip(self, out, in_, reads, writes):
        self.k.op("dve", lambda e: e.reciprocal(out=out, in_=in_), reads, writes)

    def dma(self, out, in_, owner, reads=(), writes=(), eng="sp", **kw):
        self.k.dma(eng, out, in_, owner, reads, writes, **kw)

    def scratch(self, name, shape, dt):
        return self.k.dram(name, shape, dt, kind="ExternalOutput" if self.debug else "Internal")

    def inp(self, name, shape, dt=F32):
        return self.k.dram(name, shape, dt, kind="ExternalInput")

    def declare(self):
        TL, TC, TA = self.TL, self.TC, self.TA
        i = self.inp
        self.x_in = i("x", [TL, D]); self.ctx_in = i("ctx", [TC, D])
        self.cv2 = i("cv2", [128, 8, 2])
        self.ada_w = i("ada_w", [2, D, 6 * D]); self.ada_bF = i("ada_bF", [2, 128, 48]); self.ada_b = i("ada_b", [2, 1, 6 * D])
        self.lnF = i("lnF", [2, 128, 4, 8]); self.ln_rows = i("ln_rows", [2, 4, D])
        self.ab_w_in = i("ab_w_in", [D, 3072]); self.ab_qk = i("ab_qk", [2, 128]); self.ab_ld = i("ab_ld", [1, 8])
        self.ab_gn = i("ab_gn", [1, 512]); self.ab_w_out = i("ab_w_out", [D, D])
        self.mla_w_in = i("mla_w_in", [D, 704]); self.mla_qn = i("mla_qn", [1, 384]); self.mla_kvn = i("mla_kvn", [1, 256])
        self.mla_w_uq = i("mla_w_uq", [384, 1536]); self.mla_w_ukv = i("mla_w_ukv", [256, 2048]); self.mla_w_out = i("mla_w_out", [D, D])
        self.r_w = i("r_w", [2, D, NE]); self.r_b = i("r_b", [2, 1, NE])
        ne = getattr(self, "ne_decl", NE)
        self.w_up = i("w_up", [2, ne, D, 2 * D]); self.b_upF = i("b_upF", [2, NE, 128, 16])
        self.w_down = i("w_down", [2, ne, D, D]); self.b_down = i("b_down", [2, NE, D])
        self.identF_in = i("identF", [128, 128])
        self.ropeA = i("ropeA", [TA, 2, 128]); self.ropeB = i("ropeB", [TA, 2, 512]); self.ropeM = i("ropeM", [TA, 2, 512])
        self.y_out = self.k.dram("y", [TL, D], F32, kind="ExternalOutput")
        s = self.scratch
        self.hT0 = s("hT0", [self.NTA, 128, 8, 128], BF16)
        self.cat = s("cat", [TA, D], BF16)
        self.x1 = s("x1", [TA, D], F32)
        self.h2T = s("h2T", [self.NTA, 128, 8, 128], BF16)
        self.x2 = s("x2", [TA, D], F32)
        self.qTB = s("qTB", [4, 128, TA], BF16); self.kTB = s("kTB", [4, 128, TA], BF16)
        self.kB = s("kB", [TA, 512], BF16); self.vB = s("vB", [TA, 512], BF16); self.gB = s("gB", [TA, 512], BF16)

    def consts(self):
        k = self.k
        self.identF = k.sbuf("identF", [128, 128], F32)
        self.dma(self.identF[:], self.identF_in[:, :], self.identF, writes=[self.identF])
        self.identB = k.sbuf("identB", [128, 128], BF16)
        self.cp("dve", self.identB[:], self.identF[:], [self.identF], [self.identB])
        self.ones = k.sbuf("ones", [128, 512], F32)
        self.memset("pool", self.ones[:], 1.0, [self.ones])

    def xsrc(self, tt):
        if tt < self.NTC:
            return self.ctx_in, self.ctx_in[tt * 128:(tt + 1) * 128, :]
        t = tt - self.NTC
        return self.x_in, self.x_in[t * 128:(t + 1) * 128, :]

    def phase0(self):
        k = self.k
        P_modF = [k.sbuf("modF", [128, 48, 2], F32) for _ in range(2)]
        self.G = [k.sbuf("G", [128, self.NTA, NE], F32) for _ in range(2)]
        self.gate_rows = self.scratch("gate_rows", [2, 2, 2, D], F32)
        P_coef = [k.sbuf("coef", [128, 4, 2, 8], F32) for _ in range(2)]
        P_lnF = [k.sbuf("lnF", [128, 4, 8], F32) for _ in range(2)]
        ps = ExitStack()
        cv = k.sbuf("cv", [128, 8, 2], F32, ps)
        self.dma(cv[:], self.cv2[:, :, :], cv, writes=[cv])
        sc2 = k.sbuf("sc2", [128, 8, 2], F32, ps)
        self.act(sc2[:], cv[:], AF.Silu, [cv], [sc2])
        scB = k.sbuf("scB", [128, 8, 2, 128], F32, ps)
        for kk in range(8):
            for s in range(2):
                self.act(scB[:, kk, s, :], self.ones[:, 0:128], AF.Copy, [self.ones, sc2], [scB], scale=sc2[:, kk, s:s + 1])
        wch = [k.sbuf("wch", [128, 8, 512], F32, ps) for _ in range(2)]
        brow = [k.sbuf("brow", [1, 512], F32, ps) for _ in range(2)]
        grow = [k.sbuf("grow", [1, 512], F32, ps) for _ in range(2)]
        abFs = [k.sbuf("abF", [128, 48], F32, ps) for _ in range(2)]
        tmp = k.sbuf("ctmp", [128, 2, 8], F32, ps)
        psm = k.psum("psm", [128, 512], F32, ps)
        psg = [k.psum("psg", [128, 512], F32, ps) for _ in range(2)]
        self.modF, self.coef = [], []
        for i in range(2):
            modF, coef, lnF = P_modF[i], P_coef[i], P_lnF[i]
            abF = abFs[i]
            self.dma(abF[:], self.ada_bF[i], abF, writes=[abF])
            for n in range(12):
                w = wch[n % 2]
                self.dma(w[:], self.ada_w[i][:, n * 512:(n + 1) * 512].rearrange("(k p) n -> p k n", p=128), w, writes=[w])
                for m in range(4):
                    mc = n * 4 + m
                    for kk in range(8):
                        self.mm(psm[:, mc * 2:mc * 2 + 2], w[:, kk, m * 128:(m + 1) * 128], sc2[:, kk, :], kk == 0, kk == 7, [w, sc2], [psm])
                j = n // 2
                if j in (2, 5):
                    br = brow[n % 2]
                    self.dma(br[:], self.ada_b[i][:, n * 512:(n + 1) * 512], br, writes=[br])
                    for s in range(2):
                        for kk in range(8):
                            self.mm(psg[s][:], scB[:, kk, s, :], w[:, kk, :], kk == 0, False, [w, scB], [psg[s]])
                        self.mm(psg[s][:], self.ones[0:1, 0:128], br[:], False, True, [self.ones, br], [psg[s]])
                        gr = grow[(n + s) % 2]
                        self.cp("act", gr[:], psg[s][0:1, :], [psg[s]], [gr])
                        self.dma(self.gate_rows[i, 0 if j == 2 else 1, s:s + 1, (n % 2) * 512:(n % 2 + 1) * 512], gr[:], gr, reads=[gr], writes=[self.gate_rows])
            for s in range(2):
                self.tt("dve", modF[:, :, s], psm[:, s:96:2], abF[:], ALU.add, [psm, abF], [modF])
            self.dma(lnF[:], self.lnF[i], lnF, writes=[lnF])
            for s in range(2):
                self.ts("dve", coef[:, 0, s, :], modF[:, 8:16, s], 1.0, None, ALU.add, None, [modF], [coef])
                self.cp("dve", coef[:, 1, s, :], modF[:, 0:8, s], [modF], [coef])
                self.ts("dve", tmp[:, s, :], modF[:, 32:40, s], 1.0, None, ALU.add, None, [modF], [tmp])
                self.tt("dve", coef[:, 2, s, :], tmp[:, s, :], lnF[:, 0, :], ALU.mult, [tmp, lnF], [coef])
                self.tt("dve", coef[:, 3, s, :], tmp[:, s, :], lnF[:, 1, :], ALU.mult, [tmp, lnF], [coef])
                self.tt("dve", coef[:, 3, s, :], coef[:, 3, s, :], modF[:, 24:32, s], ALU.add, [coef, modF], [coef])
            self.modF.append(modF); self.coef.append(coef)
        k.flush()
        ps.close()

    def dbg(self, name, t, shape, dt=F32):
        if not self.debug:
            return
        d = self.k.dram("dbg_" + name, shape, dt, kind="ExternalOutput")
        self.dma(d.ap, t[:], t, reads=[t], writes=[d])


def _fm(v):
    return np.ascontiguousarray(np.swapaxes(v.reshape(v.shape[:-1] + (8, 128)), -1, -2))


def prep_inputs(inp, TL, TC, nb):
    f = lambda a: np.ascontiguousarray(np.asarray(a, dtype=np.float32))
    tabs = make_tables(TL, TC)
    shared = dict(
        ada_w=f(inp["ada_w"]),
        ada_bF=np.ascontiguousarray(np.transpose(f(inp["ada_b"]).reshape(2, 48, 128), (0, 2, 1))),
        ada_b=f(inp["ada_b"]).reshape(2, 1, 6 * D),
        lnF=np.ascontiguousarray(np.stack([_fm(f(inp[n])) for n in ("ln1_g", "ln1_b", "ln2_g", "ln2_b")], 2)),
        ln_rows=np.ascontiguousarray(np.stack([f(inp[n]) for n in ("ln1_g", "ln1_b", "ln2_g", "ln2_b")], 1)),
        ab_w_in=f(inp["ab_w_in"])[0], ab_qk=np.concatenate([f(inp["ab_q_norm"]), f(inp["ab_k_norm"])], 0),
        ab_ld=f(inp["ab_log_decay"]).reshape(1, 8), ab_gn=f(inp["ab_gn_g"]).reshape(1, 512), ab_w_out=f(inp["ab_w_out"])[0],
        mla_w_in=f(inp["mla_w_in"])[0], mla_qn=f(inp["mla_q_norm"]).reshape(1, 384), mla_kvn=f(inp["mla_kv_norm"]).reshape(1, 256),
        mla_w_uq=f(inp["mla_w_uq"])[0], mla_w_ukv=f(inp["mla_w_ukv"])[0], mla_w_out=f(inp["mla_w_out"])[0],
        r_w=f(inp["moe_router_w"]), r_b=f(inp["moe_router_b"]).reshape(2, 1, NE),
        w_up=f(inp["moe_w_up"]),
        b_upF=np.ascontiguousarray(np.transpose(f(inp["moe_b_up"]).reshape(2, NE, 16, 128), (0, 1, 3, 2))),
        w_down=f(inp["moe_w_down"]), b_down=f(inp["moe_b_down"]),
        identF=np.eye(128, dtype=np.float32), **tabs)
    x, c, ctx, c_ctx = f(inp["x"]), f(inp["c"]), f(inp["ctx"]), f(inp["c_ctx"])
    maps = []
    for b in range(nb):
        cv2 = np.ascontiguousarray(np.stack([_fm(c[b]), _fm(c_ctx)], -1))
        maps.append(dict(x=x[b], ctx=ctx[b], cv2=cv2, **shared))
    return maps


class B2(B):
    def load_hT(self, src_t, src_ap, xt, pT, hT, coef, s, ident, a_idx=0):
        self.dma(xt[:], src_ap, xt, reads=[src_t], writes=[xt])
        for half in range(2):
            for j in range(4):
                c = half * 4 + j
                self.tr(pT[half][:, j * 128:(j + 1) * 128], xt[:, c * 128:(c + 1) * 128], ident[:], [xt, ident], [pT[half]])
            for j in range(4):
                c = half * 4 + j
                self.act(hT[:, c, :], pT[half][:, j * 128:(j + 1) * 128], AF.Identity, [pT[half], coef], [hT],
                         bias=coef[:, a_idx + 1, s, c:c + 1], scale=coef[:, a_idx, s, c:c + 1])

    def rope(self, out, out_t, xin, C, S, nblk, h, tmp1, tmp2, reads):
        n = nblk * 2 * h
        v = lambda ap: ap.rearrange("p (b two h) -> p b two h", two=2, h=h)
        self.tt("dve", tmp1[:, 0:n], xin, C, ALU.mult, reads, [tmp1])
        self.tt("pool", v(tmp2[:, 0:n])[:, :, 0, :], v(xin)[:, :, 1, :], v(S)[:, :, 0, :], ALU.mult, reads, [tmp2])
        self.tt("pool", v(tmp2[:, 0:n])[:, :, 1, :], v(xin)[:, :, 0, :], v(S)[:, :, 1, :], ALU.mult, reads, [tmp2])
        self.tt("dve", out, tmp1[:, 0:n], tmp2[:, 0:n], ALU.add, [tmp1, tmp2], [out_t])

    def l0_passA(self):
        k = self.k
        NTA, TA = self.NTA, self.TA
        self.stA = ExitStack()
        self.QT = k.sbuf("QT", [128, 4, TA], BF16, self.stA)
        self.KT = k.sbuf("KT", [128, 2, TA], BF16, self.stA)
        self.VA = k.sbuf("VA", [128, NTA, 2, 130], BF16, self.stA)
        ps = ExitStack()
        wA = k.sbuf("wA", [128, 8, 1024], BF16, ps)
        self.dma(wA[:], self.ab_w_in[:, 0:1024].rearrange("(k p) n -> p k n", p=128), wA, writes=[wA], eng="pool")
        gq = k.sbuf("gq", [128, 2, 128], F32, ps)
        self.dma(gq[:, 0, :], self.ab_qk[0:1, :].partition_broadcast(128), gq, writes=[gq])
        self.dma(gq[:, 1, :], self.ab_qk[1:2, :].partition_broadcast(128), gq, writes=[gq])
        self.k.op("act", lambda e: e.mul(out=gq[:, 0, :], in_=gq[:, 0, :], mul=128.0 ** -0.5), [gq], [gq])
        self.memset("pool", self.VA[:], 1.0, [self.VA])
        xt = [k.sbuf("xt", [128, D], F32, ps) for _ in range(2)]
        hT = [k.sbuf("hT", [128, 8, 128], BF16, ps) for _ in range(2)]
        tab = [k.sbuf("tabA", [128, 2, 128], F32, ps) for _ in range(2)]
        ss = k.sbuf("ss", [128, 8], F32, ps)
        rstd = k.sbuf("rstd", [128, 8], F32, ps)
        junk = k.sbuf("junk", [128, 128], F32, ps)
        xn = [k.sbuf("xn", [128, 128], F32, ps) for _ in range(2)]
        t1 = [k.sbuf("t1", [128, 128], F32, ps) for _ in range(2)]
        t2 = [k.sbuf("t2", [128, 128], F32, ps) for _ in range(2)]
        ob = [k.sbuf("ob", [128, 128], BF16, ps) for _ in range(2)]
        pT = [k.psum("pT", [128, 512], F32, ps) for _ in range(2)]
        pA = [k.psum("pA", [128, 1024], F32, ps) for _ in range(2)]
        pTb = k.psum("pTb", [128, 1024], BF16, ps)
        coef = self.coef[0]
        for tt in range(NTA):
            s = 1 if tt < self.NTC else 0
            src_t, src_ap = self.xsrc(tt)
            x_, h_, tb, pa = xt[tt % 2], hT[tt % 2], tab[tt % 2], pA[tt % 2]
            self.load_hT(src_t, src_ap, x_, pT, h_, coef, s, self.identF)
            self.dma(self.hT0[tt], h_[:], h_, reads=[h_], writes=[self.hT0])
            self.dma(tb[:], self.ropeA[tt * 128:(tt + 1) * 128], tb, writes=[tb])
            for n in range(2):
                for c in range(8):
                    self.mm(pa[:, n * 512:(n + 1) * 512], h_[:, c, :], wA[:, c, n * 512:(n + 1) * 512], c == 0, c == 7, [h_, wA], [pa])
            for hh in range(6):
                self.act(junk[:], pa[:, hh * 128:(hh + 1) * 128], AF.Square, [pa], [junk, ss], accum_out=ss[:, hh:hh + 1])
            self.act(rstd[:, 0:6], ss[:, 0:6], AF.Sqrt, [ss], [rstd], bias=1e-6, scale=1.0 / 128)
            self.recip(rstd[:, 0:6], rstd[:, 0:6], [rstd], [rstd])
            for hh in range(6):
                i2 = hh % 2
                g = gq[:, 0, :] if hh < 4 else gq[:, 1, :]
                self.stt("dve", xn[i2][:], pa[:, hh * 128:(hh + 1) * 128], rstd[:, hh:hh + 1], g, ALU.mult, ALU.mult, [pa, rstd, gq], [xn[i2]])
                self.rope(ob[i2][:], ob[i2], xn[i2][:], tb[:, 0, :], tb[:, 1, :], 2, 32, t1[i2], t2[i2], [xn[i2], tb])
                self.tr(pTb[:, hh * 128:(hh + 1) * 128], ob[i2][:], self.identB[:], [ob[i2], self.identB], [pTb])
            self.cp("act", self.QT[:, :, tt * 128:(tt + 1) * 128], pTb[:, 0:512].rearrange("p (h t) -> p h t", h=4), [pTb], [self.QT])
            self.cp("act", self.KT[:, :, tt * 128:(tt + 1) * 128], pTb[:, 512:768].rearrange("p (h t) -> p h t", h=2), [pTb], [self.KT])
            self.cp("dve", self.VA[:, tt, :, 0:128], pa[:, 768:1024].rearrange("p (h d) -> p h d", h=2), [pa], [self.VA])
        k.flush()
        ps.close()

    def attention(self, parts, V, vres, q_ranges, cat_col, bufs, exp_bias=None):
        pS, pO, PT, osb, rec = bufs
        np_ = len(parts)
        it = 0
        for (q0, nq, ktiles) in q_ranges:
            nqb = nq // 128
            for ki, kt in enumerate(ktiles):
                sp = pS[it % 2]
                pt = PT[it % 2]
                it += 1
                for pi, (Qap, Kap, qres, kres) in enumerate(parts):
                    self.mm(sp[:, 0:nq], Kap(kt), Qap(q0, nq), pi == 0, pi == np_ - 1, [qres, kres], [sp])
                if exp_bias is None:
                    self.act(pt[:, 0:nq], sp[:, 0:nq], AF.Exp, [sp], [pt])
                else:
                    self.act(pt[:, 0:nq], sp[:, 0:nq], AF.Exp, [sp, exp_bias[1]], [pt], bias=exp_bias[0])
                for qb in range(nqb):
                    self.mm(pO[qb][:, 0:129], pt[:, qb * 128:(qb + 1) * 128], V(kt), ki == 0, ki == len(ktiles) - 1, [pt, vres], [pO[qb]])
            ob = osb[(q0 // 512) % 2]
            for qb in range(nqb):
                self.recip(rec[:, qb:qb + 1], pO[qb][:, 128:129], [pO[qb]], [rec])
                self.act(ob[:, qb, :], pO[qb][:, 0:128], AF.Copy, [pO[qb], rec], [ob], scale=rec[:, qb:qb + 1])
            self.dma(self.cat[q0:q0 + nq, cat_col:cat_col + 128].rearrange("(qb p) c -> p qb c", p=128), ob[:, 0:nqb, :], ob,
                     reads=[ob], writes=[self.cat])

    def attn_bufs(self, ps):
        k = self.k
        pS = [k.psum("pS", [128, 512], F32, ps) for _ in range(2)]
        pO = [k.psum("pO", [128, 512], F32, ps) for _ in range(4)]
        PT = [k.sbuf("PT", [128, 512], BF16, ps) for _ in range(2)]
        osb = [k.sbuf("osb", [128, 4, 128], BF16, ps) for _ in range(2)]
        rec = k.sbuf("rec", [128, 4], F32, ps)
        return pS, pO, PT, osb, rec

    def q_chunks(self, q0, q1, ktiles):
        out = []
        q = q0
        while q < q1:
            n = min(512, q1 - q)
            out.append((q, n, ktiles))
            q += n
        return out

    def l0_attnA(self):
        ps = ExitStack()
        bufs = self.attn_bufs(ps)
        TC, TA, NTC, NTA = self.TC, self.TA, self.NTC, self.NTA
        for h in range(4):
            kvh = h // 2
            parts = [(lambda q0, n, h=h: self.QT[:, h, q0:q0 + n], lambda kt, kvh=kvh: self.KT[:, kvh, kt * 128:(kt + 1) * 128], self.QT, self.KT)]
            V = lambda kt, kvh=kvh: self.VA[:, kt, kvh, 0:129]
            ranges = self.q_chunks(0, TC, list(range(NTC))) + self.q_chunks(TC, TA, list(range(NTA)))
            self.attention(parts, V, self.VA, ranges, h * 128, bufs)
        self.k.flush()
        ps.close()
        self.stA.close()

    def l0_passB(self):
        k = self.k
        NTA = self.NTA
        ps = ExitStack()
        wB = k.sbuf("wB", [128, 8, 2048], BF16, ps)
        for hf in range(2):
            self.dma(wB[:, :, hf * 1024:(hf + 1) * 1024], self.ab_w_in[:, 1024 + hf * 1024:2048 + hf * 1024].rearrange("(k p) n -> p k n", p=128),
                     wB, writes=[wB], eng="pool")
        gnB = k.sbuf("gnB", [128, 512], F32, ps)
        self.dma(gnB[:], self.ab_gn[0:1, :].partition_broadcast(128), gnB, writes=[gnB])
        hT = [k.sbuf("hTb", [128, 8, 128], BF16, ps) for _ in range(2)]
        tab = [k.sbuf("tabB", [128, 2, 512], F32, ps) for _ in range(2)]
        xs = [k.sbuf("xsB", [128, 512], F32, ps) for _ in range(2)]
        t1 = k.sbuf("t1B", [128, 512], F32, ps)
        t2 = k.sbuf("t2B", [128, 512], F32, ps)
        qr = [k.sbuf("qrB", [128, 512], BF16, ps) for _ in range(2)]
        kr = [k.sbuf("krB", [128, 512], BF16, ps) for _ in range(2)]
        vb = [k.sbuf("vbB", [128, 512], BF16, ps) for _ in range(2)]
        gb = [k.sbuf("gbB", [128, 512], BF16, ps) for _ in range(2)]
        gs = k.sbuf("gsB", [128, 512], F32, ps)
        stg = [k.sbuf("stgB", [128, 8, 128], BF16, ps) for _ in range(2)]
        pB = k.psum("pB", [128, 2048], F32, ps)
        pTb = k.psum("pTbB", [128, 1024], BF16, ps)
        for tt in range(NTA):
            i2 = tt % 2
            h_, tb = hT[i2], tab[i2]
            rows = slice(tt * 128, (tt + 1) * 128)
            self.dma(h_[:], self.hT0[tt], h_, reads=[self.hT0], writes=[h_])
            self.dma(tb[:], self.ropeB[rows], tb, writes=[tb])
            for n in range(4):
                for c in range(8):
                    self.mm(pB[:, n * 512:(n + 1) * 512], h_[:, c, :], wB[:, c, n * 512:(n + 1) * 512], c == 0, c == 7, [h_, wB], [pB])
            self.act(xs[0][:], pB[:, 0:512], AF.Copy, [pB], [xs[0]], scale=128.0 ** -0.5)
            self.rope(qr[i2][:], qr[i2], xs[0][:], tb[:, 0, :], tb[:, 1, :], 4, 64, t1, t2, [xs[0], tb])
            self.act(xs[1][:], pB[:, 512:1024], AF.Copy, [pB], [xs[1]])
            self.rope(kr[i2][:], kr[i2], xs[1][:], tb[:, 0, :], tb[:, 1, :], 4, 64, t1, t2, [xs[1], tb])
            self.dma(self.kB[rows, :], kr[i2][:], kr[i2], reads=[kr[i2]], writes=[self.kB])
            for hh in range(4):
                self.tr(pTb[:, hh * 128:(hh + 1) * 128], qr[i2][:, hh * 128:(hh + 1) * 128], self.identB[:], [qr[i2], self.identB], [pTb])
                self.tr(pTb[:, 512 + hh * 128:512 + (hh + 1) * 128], kr[i2][:, hh * 128:(hh + 1) * 128], self.identB[:], [kr[i2], self.identB], [pTb])
            self.cp("act", stg[i2][:], pTb[:].rearrange("p (h t) -> p h t", h=8), [pTb], [stg[i2]])
            self.dma(self.qTB[:, :, rows].rearrange("h d t -> d h t"), stg[i2][:, 0:4, :], stg[i2], reads=[stg[i2]], writes=[self.qTB])
            self.dma(self.kTB[:, :, rows].rearrange("h d t -> d h t"), stg[i2][:, 4:8, :], stg[i2], reads=[stg[i2]], writes=[self.kTB])
            self.cp("dve", vb[i2][:], pB[:, 1024:1536], [pB], [vb[i2]])
            self.dma(self.vB[rows, :], vb[i2][:], vb[i2], reads=[vb[i2]], writes=[self.vB])
            self.act(gs[:], pB[:, 1536:2048], AF.Silu, [pB], [gs])
            self.tt("dve", gb[i2][:], gs[:], gnB[:], ALU.mult, [gs, gnB], [gb[i2]])
            self.dma(self.gB[rows, :], gb[i2][:], gb[i2], reads=[gb[i2]], writes=[self.gB])
        k.flush()
        ps.close()

    def l0_retention(self):
        k = self.k
        NTA, NTC, TA = self.NTA, self.NTC, self.TA
        ps = ExitStack()
        ld = k.sbuf("ld", [128, 8], F32, ps)
        self.dma(ld[:], self.ab_ld[0:1, :].partition_broadcast(128), ld, writes=[ld])
        lg = k.sbuf("lg", [128, 8], F32, ps)
        nlg = k.sbuf("nlg", [128, 8], F32, ps)
        self.act(lg[:], ld[:], AF.Exp, [ld], [lg])
        self.ts("dve", lg[:], lg[:], -1.0, 1.0, ALU.mult, ALU.add, [lg], [lg])
        self.act(lg[:], lg[:], AF.Ln, [lg], [lg])
        self.ts("dve", nlg[:], lg[:], -1.0, None, ALU.mult, None, [lg], [nlg])
        ii = k.sbuf("ii", [128, 128], I32, ps)
        dif = k.sbuf("dif", [128, 128], F32, ps)
        c1 = k.sbuf("c1", [128, 128], F32, ps)
        c128 = k.sbuf("c128", [128, 128], F32, ps)
        pcol = k.sbuf("pcol", [128, 4], F32, ps)
        k.op("pool", lambda e: e.iota(ii[:], [[1, 128]], base=0, channel_multiplier=-1), (), [ii])
        self.cp("dve", dif[:], ii[:], [ii], [dif])
        k.op("pool", lambda e: e.iota(ii[:], [[1, 128]], base=1, channel_multiplier=0), [dif], [ii])
        self.cp("dve", c1[:], ii[:], [ii], [c1])
        self.ts("dve", c128[:], c1[:], -1.0, 129.0, ALU.mult, ALU.add, [c1], [c128])
        k.op("pool", lambda e: e.iota(ii[:, 0:1], [[1, 1]], base=0, channel_multiplier=1), [c1], [ii])
        self.cp("dve", pcol[:, 0:1], ii[:, 0:1], [ii], [pcol])
        self.ts("dve", pcol[:, 1:2], pcol[:, 0:1], -1.0, 127.0, ALU.mult, ALU.add, [pcol], [pcol])
        self.memset("dve", pcol[:, 2:3], 128.0, [pcol])
        mf = k.sbuf("mf", [128, 128], F32, ps)
        mb = k.sbuf("mb", [128, 128], F32, ps)
        self.ts("dve", mf[:], dif[:], 0.0, None, ALU.is_ge, None, [dif], [mf])
        self.ts("dve", mb[:], dif[:], 0.0, None, ALU.is_lt, None, [dif], [mb])
        DT = k.sbuf("DT", [128, 128], F32, ps)
        DT2 = k.sbuf("DT2", [128, 128], F32, ps)
        xiF = k.sbuf("xiF", [128, 128], F32, ps)
        xiB = k.sbuf("xiB", [128, 128], F32, ps)
        zc = k.sbuf("zc", [128, 4], F32, ps)
        qT = k.sbuf("qTr", [128, TA], BF16, ps)
        kT = k.sbuf("kTr", [128, TA], BF16, ps)
        kk = k.sbuf("kkr", [128, NTA, 128], BF16, ps)
        vv = k.sbuf("vvr", [128, NTA, 128], BF16, ps)
        gg = k.sbuf("ggr", [128, NTA, 128], BF16, ps)
        Kzf = k.sbuf("Kzf", [128, NTA, 128], BF16, ps)
        Kzb = k.sbuf("Kzb", [128, NTA, 128], BF16, ps)
        SfP = k.sbuf("SfP", [128, NTA, 128], BF16, ps)
        SbP = k.sbuf("SbP", [128, NTA, 128], BF16, ps)
        Sf = k.sbuf("Sf", [128, 128], F32, ps)
        Sb = k.sbuf("Sb", [128, 128], F32, ps)
        Pm = [k.sbuf("Pm", [128, 128], BF16, ps) for _ in range(2)]
        qxf = [k.sbuf("qxf", [128, 128], BF16, ps) for _ in range(2)]
        qxb = [k.sbuf("qxb", [128, 128], BF16, ps) for _ in range(2)]
        st6 = k.sbuf("st6", [128, 6], F32, ps)
        mv = k.sbuf("mv", [128, 4], F32, ps)
        on = [k.sbuf("on", [128, 128], F32, ps) for _ in range(2)]
        ystg = k.sbuf("ystg", [128, NTA, 128], BF16, ps)
        pkv = [k.psum("pkv", [128, 512], F32, ps) for _ in range(2)]
        psc = [k.psum("psc", [128, 512], F32, ps) for _ in range(2)]
        pO = [k.psum("pOr", [128, 512], F32, ps) for _ in range(2)]
        order_b = list(range(NTC - 1, -1, -1)) + list(range(NTA - 1, NTC - 1, -1))
        for h in range(4):
            hs = slice(h * 128, (h + 1) * 128)
            self.dma(qT[:], self.qTB[h], qT, reads=[self.qTB], writes=[qT])
            self.dma(kT[:], self.kTB[h], kT, reads=[self.kTB], writes=[kT])
            for n0 in range(0, NTA, 8):
                n1 = min(NTA, n0 + 8)
                rs = slice(n0 * 128, n1 * 128)
                self.dma(kk[:, n0:n1, :], self.kB[rs, hs].rearrange("(n p) d -> p n d", p=128), kk, reads=[self.kB], writes=[kk])
                self.dma(vv[:, n0:n1, :], self.vB[rs, hs].rearrange("(n p) d -> p n d", p=128), vv, reads=[self.vB], writes=[vv])
                self.dma(gg[:, n0:n1, :], self.gB[rs, hs].rearrange("(n p) d -> p n d", p=128), gg, reads=[self.gB], writes=[gg])
            lf, lb, nlb = lg[:, h:h + 1], lg[:, 4 + h:5 + h], nlg[:, 4 + h:5 + h]
            self.act(DT[:], dif[:], AF.Exp, [dif, lg], [DT], scale=lf)
            self.tt("dve", DT[:], DT[:], mf[:], ALU.mult, [DT, mf], [DT])
            self.act(DT2[:], dif[:], AF.Exp, [dif, nlg], [DT2], scale=nlb)
            self.tt("dve", DT2[:], DT2[:], mb[:], ALU.mult, [DT2, mb], [DT2])
            self.tt("dve", DT[:], DT[:], DT2[:], ALU.add, [DT, DT2], [DT])
            self.act(xiF[:], c1[:], AF.Exp, [c1, lg], [xiF], scale=lf)
            self.act(xiB[:], c128[:], AF.Exp, [c128, lg], [xiB], scale=lb)
            self.act(zc[:, 0:1], pcol[:, 1:2], AF.Exp, [pcol, lg], [zc], scale=lf)
            self.act(zc[:, 1:2], pcol[:, 0:1], AF.Exp, [pcol, lg], [zc], scale=lb)
            self.act(zc[:, 2:3], pcol[:, 2:3], AF.Exp, [pcol, lg], [zc], scale=lf)
            self.act(zc[:, 3:4], pcol[:, 2:3], AF.Exp, [pcol, lg], [zc], scale=lb)
            self.ts("dve", Kzf[:], kk[:], zc[:, 0:1], None, ALU.mult, None, [kk, zc], [Kzf])
            self.ts("pool", Kzb[:], kk[:], zc[:, 1:2], None, ALU.mult, None, [kk, zc], [Kzb])
            self.memset("dve", Sf[:], 0.0, [Sf])
            self.memset("pool", Sb[:], 0.0, [Sb])
            for it, n in enumerate(range(NTA)):
                p_ = pkv[it % 2]
                self.cp("act", SfP[:, n, :], Sf[:], [Sf], [SfP])
                self.mm(p_[:, 0:128], Kzf[:, n, :], vv[:, n, :], True, True, [Kzf, vv], [p_])
                self.stt("dve", Sf[:], Sf[:], zc[:, 2:3], p_[:, 0:128], ALU.mult, ALU.add, [Sf, zc, p_], [Sf])
            for it, n in enumerate(order_b):
                p_ = pkv[it % 2]
                self.cp("act", SbP[:, n, :], Sb[:], [Sb], [SbP])
                self.mm(p_[:, 0:128], Kzb[:, n, :], vv[:, n, :], True, True, [Kzb, vv], [p_])
                self.stt("dve", Sb[:], Sb[:], zc[:, 3:4], p_[:, 0:128], ALU.mult, ALU.add, [Sb, zc, p_], [Sb])
            for n in range(NTA):
                i2 = n % 2
                cs = slice(n * 128, (n + 1) * 128)
                self.mm(psc[i2][:, 0:128], kT[:, cs], qT[:, cs], True, True, [kT, qT], [psc[i2]])
                self.tt("dve", Pm[i2][:], psc[i2][:, 0:128], DT[:], ALU.mult, [psc[i2], DT], [Pm[i2]])
                self.tt("pool", qxf[i2][:], qT[:, cs], xiF[:], ALU.mult, [qT, xiF], [qxf[i2]])
                self.tt("pool", qxb[i2][:], qT[:, cs], xiB[:], ALU.mult, [qT, xiB], [qxb[i2]])
                o_ = pO[i2]
                self.mm(o_[:, 0:128], Pm[i2][:], vv[:, n, :], True, False, [Pm[i2], vv], [o_])
                self.mm(o_[:, 0:128], qxf[i2][:], SfP[:, n, :], False, False, [qxf[i2], SfP], [o_])
                self.mm(o_[:, 0:128], qxb[i2][:], SbP[:, n, :], False, True, [qxb[i2], SbP], [o_])
                k.op("dve", lambda e, o_=o_: e.bn_stats(out=st6[:], in_=o_[:, 0:128]), [o_], [st6])
                k.op("dve", lambda e: e.bn_aggr(out=mv[:, 0:2], in_=st6[:]), [st6], [mv])
                self.act(mv[:, 2:3], mv[:, 1:2], AF.Sqrt, [mv], [mv], bias=1e-5)
                self.recip(mv[:, 2:3], mv[:, 2:3], [mv], [mv])
                self.stt("dve", mv[:, 3:4], mv[:, 0:1], -1.0, mv[:, 2:3], ALU.mult, ALU.mult, [mv], [mv])
                self.act(on[i2][:], o_[:, 0:128], AF.Identity, [o_, mv], [on[i2]], bias=mv[:, 3:4], scale=mv[:, 2:3])
                self.tt("pool", ystg[:, n, :], on[i2][:], gg[:, n, :], ALU.mult, [on[i2], gg], [ystg])
            for n0 in range(0, NTA, 8):
                n1 = min(NTA, n0 + 8)
                self.dma(self.cat[n0 * 128:n1 * 128, 512 + h * 128:512 + (h + 1) * 128].rearrange("(n p) c -> p n c", p=128), ystg[:, n0:n1, :], ystg,
                         reads=[ystg], writes=[self.cat])
        k.flush()
        ps.close()

    def layernorm(self, zn, z, st, mv, eps=1e-5):
        k = self.k
        for hf in range(2):
            k.op("dve", lambda e, hf=hf: e.bn_stats(out=st[:, hf * 6:(hf + 1) * 6], in_=z[:, hf * 512:(hf + 1) * 512]), [z], [st])
        k.op("dve", lambda e: e.bn_aggr(out=mv[:, 0:2], in_=st[:, 0:12]), [st], [mv])
        self.act(mv[:, 2:3], mv[:, 1:2], AF.Sqrt, [mv], [mv], bias=eps)
        self.recip(mv[:, 2:3], mv[:, 2:3], [mv], [mv])
        self.stt("dve", mv[:, 3:4], mv[:, 0:1], -1.0, mv[:, 2:3], ALU.mult, ALU.mult, [mv], [mv])
        self.act(zn[:], z[:], AF.Identity, [z, mv], [zn], bias=mv[:, 3:4], scale=mv[:, 2:3])

    def resid_src(self, i, tt):
        if i == 0:
            return self.xsrc(tt)
        return self.x2, self.x2[tt * 128:(tt + 1) * 128, :]

    def post_mixer(self, i, w_out, tiles):
        k = self.k
        ps = ExitStack()
        wo = k.sbuf("wo", [128, 8, D], BF16, ps)
        self.dma(wo[:], w_out[:, :].rearrange("(k p) n -> p k n", p=128), wo, writes=[wo], eng="pool")
        rw = k.sbuf("rw", [128, 8, NE], F32, ps)
        self.dma(rw[:], self.r_w[i].rearrange("(k p) e -> p k e", p=128), rw, writes=[rw])
        rb = k.sbuf("rb", [1, NE], F32, ps)
        self.dma(rb[:], self.r_b[i], rb, writes=[rb])
        lnB = k.sbuf("lnB", [128, 2, D], F32, ps)
        for q in range(2):
            self.dma(lnB[:, q, :], self.ln_rows[i][q:q + 1, :].partition_broadcast(128), lnB, writes=[lnB])
        gB = k.sbuf("g2B", [128, 2, D], F32, ps)
        for s in range(2):
            self.dma(gB[:, s, :], self.gate_rows[i, 0, s:s + 1, :].partition_broadcast(128), gB, reads=[self.gate_rows], writes=[gB])
        ct = [k.sbuf("ct", [128, D], BF16, ps) for _ in range(2)]
        catT = k.sbuf("catT", [128, 8, 128], BF16, ps)
        xt = [k.sbuf("xtp", [128, D], F32, ps) for _ in range(2)]
        z = k.sbuf("z", [128, D], F32, ps)
        zn = k.sbuf("zn", [128, D], F32, ps)
        xl = [k.sbuf("xl", [128, D], F32, ps) for _ in range(2)]
        st = k.sbuf("st", [128, 12], F32, ps)
        mv = k.sbuf("mvp", [128, 4], F32, ps)
        h2f = k.sbuf("h2f", [128, 8, 128], F32, ps)
        h2b = [k.sbuf("h2b", [128, 8, 128], BF16, ps) for _ in range(2)]
        lgt = k.sbuf("lgt", [128, NE], F32, ps)
        m8 = k.sbuf("m8", [128, 8], F32, ps)
        msk = k.sbuf("msk", [128, NE], F32, ps)
        ex = k.sbuf("ex", [128, NE], F32, ps)
        den = k.sbuf("den", [128, 2], F32, ps)
        pTb = k.psum("pTbp", [128, 1024], BF16, ps)
        pY = k.psum("pYp", [128, 1024], F32, ps)
        pT = [k.psum("pTp", [128, 512], F32, ps) for _ in range(2)]
        pR = k.psum("pR", [128, 512], F32, ps)
        coef = self.coef[i]
        G = self.G[i]
        for it, tt in enumerate(tiles):
            i2 = it % 2
            s = 1 if tt < self.NTC else 0
            rows = slice(tt * 128, (tt + 1) * 128)
            c_ = ct[i2]
            self.dma(c_[:], self.cat[rows, :], c_, reads=[self.cat], writes=[c_])
            for c in range(8):
                self.tr(pTb[:, c * 128:(c + 1) * 128], c_[:, c * 128:(c + 1) * 128], self.identB[:], [c_, self.identB], [pTb])
            self.cp("act", catT[:], pTb[:].rearrange("p (c t) -> p c t", c=8), [pTb], [catT])
            for n in range(2):
                for c in range(8):
                    self.mm(pY[:, n * 512:(n + 1) * 512], catT[:, c, :], wo[:, c, n * 512:(n + 1) * 512], c == 0, c == 7, [catT, wo], [pY])
            src_t, src_ap = self.resid_src(i, tt)
            x_ = xt[i2]
            self.dma(x_[:], src_ap, x_, reads=[src_t], writes=[x_])
            self.tt("dve", z[:], pY[:], gB[:, s, :], ALU.mult, [pY, gB], [z])
            self.stt("dve", z[:], x_[:], DN_ALPHA, z[:], ALU.mult, ALU.add, [x_, z], [z])
            self.layernorm(zn, z, st, mv)
            x1_ = xl[i2]
            self.tt("pool", x1_[:], zn[:], lnB[:, 0, :], ALU.mult, [zn, lnB], [x1_])
            self.tt("pool", x1_[:], x1_[:], lnB[:, 1, :], ALU.add, [x1_, lnB], [x1_])
            self.dma(self.x1[rows, :], x1_[:], x1_, reads=[x1_], writes=[self.x1])
            for half in range(2):
                for j in range(4):
                    c = half * 4 + j
                    self.tr(pT[half][:, j * 128:(j + 1) * 128], zn[:, c * 128:(c + 1) * 128], self.identF[:], [zn, self.identF], [pT[half]])
                for j in range(4):
                    c = half * 4 + j
                    self.act(h2f[:, c, :], pT[half][:, j * 128:(j + 1) * 128], AF.Identity, [pT[half], coef], [h2f],
                             bias=coef[:, 3, s, c:c + 1], scale=coef[:, 2, s, c:c + 1])
            hb = h2b[i2]
            self.cp("pool", hb[:], h2f[:], [h2f], [hb])
            self.dma(self.h2T[tt], hb[:], hb, reads=[hb], writes=[self.h2T])
            for c in range(8):
                self.mm(pR[:, 0:NE], h2f[:, c, :], rw[:, c, :], c == 0, False, [h2f, rw], [pR])
            self.mm(pR[:, 0:NE], self.ones[0:1, 0:128], rb[:], False, True, [self.ones, rb], [pR])
            self.cp("act", lgt[:], pR[:, 0:NE], [pR], [lgt])
            k.op("dve", lambda e: e.max(out=m8[:], in_=lgt[:]), [lgt], [m8])
            self.ts("dve", msk[:], lgt[:], m8[:, 3:4], None, ALU.is_ge, None, [lgt, m8], [msk])
            self.ts("dve", den[:, 0:1], m8[:, 0:1], -1.0, None, ALU.mult, None, [m8], [den])
            self.act(ex[:], lgt[:], AF.Exp, [lgt, den], [ex], bias=den[:, 0:1])
            self.tt("dve", ex[:], ex[:], msk[:], ALU.mult, [ex, msk], [ex])
            k.op("dve", lambda e: e.reduce_sum(out=den[:, 1:2], in_=ex[:], axis=AX.X), [ex], [den])
            self.recip(den[:, 1:2], den[:, 1:2], [den], [den])
            self.ts("dve", G[:, tt, :], ex[:], den[:, 1:2], None, ALU.mult, None, [ex, den], [G])
        k.flush()
        ps.close()

    def moe(self, i, tiles, out_fn, GT=12, SC=4):
        k = self.k
        G = self.G[i]
        groups = [tiles[j:j + GT] for j in range(0, len(tiles), GT)]
        gps = ExitStack()
        bd = k.sbuf("bd", [NE, D], F32, gps)
        self.dma(bd[:], self.b_down[i], bd, writes=[bd])
        for grp in groups:
            ng = len(grp)
            subs = [list(range(j, min(ng, j + SC))) for j in range(0, ng, SC)]
            ps = ExitStack()
            facc = k.sbuf("facc", [128, GT, D], F32, ps)
            gT = k.sbuf("gT", [NE, 128], F32, ps)
            ins = ExitStack()
            Hb = [k.sbuf("H", [128, 8, SC * 128], BF16, ins) for _ in range(2)]
            wu = [k.sbuf("wu", [128, 8, 2 * D], BF16, ins) for _ in range(2)]
            wd = [k.sbuf("wd", [128, 8, D], BF16, ins) for _ in range(2)]
            bu = [k.sbuf("bu", [128, 16], F32, ins) for _ in range(2)]
            actT = k.sbuf("actT", [128, 8, SC * 128], BF16, ins)
            glu = [k.sbuf("glu", [128, SC * 128], F32, ins) for _ in range(2)]
            sig = [k.sbuf("sig", [128, SC * 128], F32, ins) for _ in range(2)]
            l1 = [k.sbuf("l1", [128, SC * 128], F32, ins) for _ in range(2)]
            pU = [k.psum("pU", [128, 2, 512], F32, ins) for _ in range(2)]
            pY = [k.psum("pYm", [128, 1024], F32, ins) for _ in range(2)]
            for j, tt in enumerate(grp):
                py = pY[j % 2]
                self.tr(pU[0][0:NE, 0, 0:128], G[:, tt, :], self.identF[:], [G, self.identF], [pU[0]])
                self.cp("act", gT[:], pU[0][0:NE, 0, 0:128], [pU[0]], [gT])
                for n in range(2):
                    self.mm(py[:, n * 512:(n + 1) * 512], gT[:], bd[:, n * 512:(n + 1) * 512], True, True, [gT, bd], [py])
                self.cp("act", facc[:, j, :], py[:], [py], [facc])
            hit = 0
            for e in range(NE):
                e2 = e % 2
                wu_, wd_, bu_ = wu[e2], wd[e2], bu[e2]
                for hf in range(2):
                    self.dma(wu_[:, :, hf * D:(hf + 1) * D], self.w_up[i, e][:, hf * D:(hf + 1) * D].rearrange("(k p) n -> p k n", p=128),
                             wu_, writes=[wu_], eng="pool")
                self.dma(wd_[:], self.w_down[i, e].rearrange("(k p) n -> p k n", p=128), wd_, writes=[wd_], eng="pool")
                self.dma(bu_[:], self.b_upF[i, e], bu_, writes=[bu_])
                self.ts("pool", bu_[:, 8:16], bu_[:, 8:16], 1.0, None, ALU.add, None, [bu_], [bu_])
                for sub in subs:
                    N = len(sub) * 128
                    H = Hb[hit % 2]
                    hit += 1
                    for jj, j in enumerate(sub):
                        self.dma(H[:, :, jj * 128:(jj + 1) * 128], self.h2T[grp[j]], H, reads=[self.h2T], writes=[H])
                    for m in range(8):
                        m2 = m % 2
                        pu = pU[m2]
                        for c in range(8):
                            self.mm(pu[:, 0, 0:N], wu_[:, c, m * 128:(m + 1) * 128], H[:, c, 0:N], c == 0, c == 7, [wu_, H], [pu])
                        for c in range(8):
                            self.mm(pu[:, 1, 0:N], wu_[:, c, D + m * 128:D + (m + 1) * 128], H[:, c, 0:N], c == 0, c == 7, [wu_, H], [pu])
                        g_, s_, l_ = glu[m2], sig[m2], l1[m2]
                        self.ts("dve", g_[:, 0:N], pu[:, 0, 0:N], bu_[:, m:m + 1], LIMIT, ALU.add, ALU.min, [pu, bu_], [g_])
                        self.act(s_[:, 0:N], g_[:, 0:N], AF.Sigmoid, [g_], [s_], scale=SW_ALPHA)
                        self.ts("dve", l_[:, 0:N], pu[:, 1, 0:N], bu_[:, 8 + m:9 + m], LIMIT + 1.0, ALU.add, ALU.min, [pu, bu_], [l_])
                        self.tt("pool", s_[:, 0:N], s_[:, 0:N], g_[:, 0:N], ALU.mult, [s_, g_], [s_])
                        self.stt("dve", actT[:, m, 0:N], l_[:, 0:N], 1.0 - LIMIT, s_[:, 0:N], ALU.max, ALU.mult, [l_, s_], [actT])
                    for jj, j in enumerate(sub):
                        tt = grp[j]
                        py = pY[j % 2]
                        for n in range(2):
                            for m in range(8):
                                self.mm(py[:, n * 512:(n + 1) * 512], actT[:, m, jj * 128:(jj + 1) * 128], wd_[:, m, n * 512:(n + 1) * 512], m == 0, m == 7,
                                        [actT, wd_], [py])
                        self.stt("dve", facc[:, j, :], py[:], G[:, tt, e:e + 1], facc[:, j, :], ALU.mult, ALU.add, [py, G, facc], [facc])
            k.flush()
            ins.close()
            ln = ExitStack()
            lnB = k.sbuf("lnB2", [128, 2, D], F32, ln)
            for q in range(2):
                self.dma(lnB[:, q, :], self.ln_rows[i][2 + q:3 + q, :].partition_broadcast(128), lnB, writes=[lnB])
            gB = k.sbuf("g5B", [128, 2, D], F32, ln)
            for s in range(2):
                self.dma(gB[:, s, :], self.gate_rows[i, 1, s:s + 1, :].partition_broadcast(128), gB, reads=[self.gate_rows], writes=[gB])
            x1t = [k.sbuf("x1t", [128, D], F32, ln) for _ in range(2)]
            z = k.sbuf("z2", [128, D], F32, ln)
            zn = k.sbuf("zn2", [128, D], F32, ln)
            ot = [k.sbuf("ot", [128, D], F32, ln) for _ in range(2)]
            st = k.sbuf("st2", [128, 12], F32, ln)
            mv = k.sbuf("mv2", [128, 4], F32, ln)
            for j, tt in enumerate(grp):
                s = 1 if tt < self.NTC else 0
                x_ = x1t[j % 2]
                self.dma(x_[:], self.x1[tt * 128:(tt + 1) * 128, :], x_, reads=[self.x1], writes=[x_])
                self.tt("dve", z[:], facc[:, j, :], gB[:, s, :], ALU.mult, [facc, gB], [z])
                self.stt("dve", z[:], x_[:], DN_ALPHA, z[:], ALU.mult, ALU.add, [x_, z], [z])
                self.layernorm(zn, z, st, mv)
                o_ = ot[j % 2]
                self.tt("pool", o_[:], zn[:], lnB[:, 0, :], ALU.mult, [zn, lnB], [o_])
                self.tt("pool", o_[:], o_[:], lnB[:, 1, :], ALU.add, [o_, lnB], [o_])
                dst_t, dst_ap = out_fn(tt)
                self.dma(dst_ap, o_[:], o_, reads=[o_], writes=[dst_t])
            k.flush()
            ln.close()
            ps.close()
        gps.close()

    def l1_passM(self):
        k = self.k
        NTA, NTC, TA = self.NTA, self.NTC, self.TA
        self.stM = ExitStack()
        self.qlatT = k.sbuf("qlatT", [128, 3, TA], BF16, self.stM)
        self.ckvT = k.sbuf("ckvT", [128, 2, TA], BF16, self.stM)
        self.kpeT = k.sbuf("kpeT", [128, TA], BF16, self.stM)
        ps = ExitStack()
        wM = k.sbuf("wM", [128, 8, 704], BF16, ps)
        self.dma(wM[:], self.mla_w_in[:, :].rearrange("(k p) n -> p k n", p=128), wM, writes=[wM], eng="pool")
        gn = k.sbuf("gnM", [128, 640], F32, ps)
        self.dma(gn[:, 0:384], self.mla_qn[0:1, :].partition_broadcast(128), gn, writes=[gn])
        self.dma(gn[:, 384:640], self.mla_kvn[0:1, :].partition_broadcast(128), gn, writes=[gn])
        xt = [k.sbuf("xtM", [128, D], F32, ps) for _ in range(2)]
        hT = [k.sbuf("hTM", [128, 8, 128], BF16, ps) for _ in range(2)]
        tab = [k.sbuf("tabM", [128, 2, 64], F32, ps) for _ in range(2)]
        ss5 = k.sbuf("ss5", [128, 8], F32, ps)
        ssq = k.sbuf("ssq", [128, 2], F32, ps)
        rsd = k.sbuf("rsd", [128, 2], F32, ps)
        xnf = k.sbuf("xnf", [128, 640], F32, ps)
        junk = k.sbuf("junkM", [128, 128], F32, ps)
        xn = [k.sbuf("xnM", [128, 640], BF16, ps) for _ in range(2)]
        kp = k.sbuf("kpM", [128, 64], F32, ps)
        t1 = k.sbuf("t1M", [128, 64], F32, ps)
        t2 = k.sbuf("t2M", [128, 64], F32, ps)
        kb = [k.sbuf("kbM", [128, 128], BF16, ps) for _ in range(2)]
        for _kb in kb:
            self.memset("pool", _kb[:], 0.0, [_kb])
        pT = [k.psum("pTM", [128, 512], F32, ps) for _ in range(2)]
        pM = k.psum("pM", [128, 1024], F32, ps)
        pTb = k.psum("pTbM", [128, 1024], BF16, ps)
        coef = self.coef[1]
        for tt in range(NTA):
            i2 = tt % 2
            s = 1 if tt < NTC else 0
            rows = slice(tt * 128, (tt + 1) * 128)
            x_, h_, tb = xt[i2], hT[i2], tab[i2]
            self.load_hT(self.x2, self.x2[rows, :], x_, pT, h_, coef, s, self.identF)
            self.dma(tb[:], self.ropeM[rows, :, 0:64], tb, writes=[tb])
            for c in range(8):
                self.mm(pM[:, 0:512], h_[:, c, :], wM[:, c, 0:512], c == 0, c == 7, [h_, wM], [pM])
            for c in range(8):
                self.mm(pM[:, 512:704], h_[:, c, :], wM[:, c, 512:704], c == 0, c == 7, [h_, wM], [pM])
            for c in range(5):
                self.act(junk[:, 0:128], pM[:, c * 128:(c + 1) * 128], AF.Square, [pM], [junk, ss5], accum_out=ss5[:, c:c + 1])
            self.tt("dve", ssq[:, 0:1], ss5[:, 0:1], ss5[:, 1:2], ALU.add, [ss5], [ssq])
            self.tt("dve", ssq[:, 0:1], ssq[:, 0:1], ss5[:, 2:3], ALU.add, [ss5, ssq], [ssq])
            self.tt("dve", ssq[:, 1:2], ss5[:, 3:4], ss5[:, 4:5], ALU.add, [ss5], [ssq])
            self.act(rsd[:, 0:1], ssq[:, 0:1], AF.Sqrt, [ssq], [rsd], bias=1e-6, scale=1.0 / 384)
            self.act(rsd[:, 1:2], ssq[:, 1:2], AF.Sqrt, [ssq], [rsd], bias=1e-6, scale=1.0 / 256)
            self.recip(rsd[:, 0:2], rsd[:, 0:2], [rsd], [rsd])
            x2_ = xn[i2]
            self.stt("dve", xnf[:, 0:384], pM[:, 0:384], rsd[:, 0:1], gn[:, 0:384], ALU.mult, ALU.mult, [pM, rsd, gn], [xnf])
            self.stt("dve", xnf[:, 384:640], pM[:, 384:640], rsd[:, 1:2], gn[:, 384:640], ALU.mult, ALU.mult, [pM, rsd, gn], [xnf])
            self.cp("pool", x2_[:], xnf[:], [xnf], [x2_])
            self.cp("act", kp[:], pM[:, 640:704], [pM], [kp])
            self.rope(kb[i2][:, 0:64], kb[i2], kp[:], tb[:, 0, :], tb[:, 1, :], 2, 16, t1, t2, [kp, tb])
            for c in range(5):
                self.tr(pTb[:, c * 128:(c + 1) * 128], x2_[:, c * 128:(c + 1) * 128], self.identB[:], [x2_, self.identB], [pTb])
            self.tr(pTb[:, 640:768], kb[i2][:], self.identB[:], [kb[i2], self.identB], [pTb])
            self.cp("act", self.qlatT[:, :, rows], pTb[:, 0:384].rearrange("p (c t) -> p c t", c=3), [pTb], [self.qlatT])
            self.cp("act", self.ckvT[:, :, rows], pTb[:, 384:640].rearrange("p (c t) -> p c t", c=2), [pTb], [self.ckvT])
            self.cp("act", self.kpeT[:, rows], pTb[:, 640:768], [pTb], [self.kpeT])
        k.flush()
        ps.close()

    def l1_attn(self):
        k = self.k
        NTA, NTC, TA, TC, TL = self.NTA, self.NTC, self.TA, self.TC, self.TL
        ps = ExitStack()
        wuq = k.sbuf("wuq", [128, 3, 1536], BF16, ps)
        self.dma(wuq[:], self.mla_w_uq[:, :].rearrange("(k p) n -> p k n", p=128), wuq, writes=[wuq], eng="pool")
        wukv = k.sbuf("wukv", [128, 2, 2048], BF16, ps)
        self.dma(wukv[:], self.mla_w_ukv[:, :].rearrange("(k p) n -> p k n", p=128), wukv, writes=[wukv], eng="pool")
        wuqr = k.sbuf("wuqr", [128, 3, 8, 128], BF16, ps)
        self.memset("pool", wuqr[:], 0.0, [wuqr])
        for c in range(3):
            self.dma(wuqr[:, c, :, 0:64], self.mla_w_uq[c * 128:(c + 1) * 128, :].rearrange("p (h c) -> p h c", c=192)[:, :, 128:192],
                     wuqr, writes=[wuqr], eng="pool")
        pmat = k.sbuf("pmat", [128, 128], F32, ps)
        self.dma(pmat[:], self.pmat_in[:, :], pmat, writes=[pmat])
        CT = k.sbuf("CTm", [128, 2, TA], F32, ps)
        self.dma(CT[:], self.ropeMT[:, :, :], CT, writes=[CT])
        KnT = k.sbuf("KnT", [128, TA], BF16, ps)
        Vh = k.sbuf("Vh", [128, NTA, 130], BF16, ps)
        QnT = k.sbuf("QnT", [128, TA], BF16, ps)
        QrT = k.sbuf("QrT", [128, TA], BF16, ps)
        raw = k.sbuf("rawq", [128, 512], F32, ps)
        u1 = k.sbuf("u1", [128, 512], F32, ps)
        u2 = k.sbuf("u2", [128, 512], F32, ps)
        self.memset("pool", Vh[:], 1.0, [Vh])
        pP = [k.psum("pP", [128, 512], F32, ps) for _ in range(2)]
        bufs = self.attn_bufs(ps)
        qscale = 192.0 ** -0.5
        chunks = lambda a, b_: [(q, min(512, b_ - q)) for q in range(a, b_, 512)]
        for h in range(8):
            kc, vc, qc, rc = h * 256, h * 256 + 128, h * 192, h * 192 + 128
            for ci, (t0, n) in enumerate(chunks(0, TA)):
                p_ = pP[ci % 2]
                for c in range(2):
                    self.mm(p_[:, 0:n], wukv[:, c, kc:kc + 128], self.ckvT[:, c, t0:t0 + n], c == 0, c == 1, [wukv, self.ckvT], [p_])
                self.cp("act", KnT[:, t0:t0 + n], p_[:, 0:n], [p_], [KnT])
            for tt in range(NTA):
                p_ = pP[tt % 2]
                for c in range(2):
                    self.mm(p_[:, 0:128], self.ckvT[:, c, tt * 128:(tt + 1) * 128], wukv[:, c, vc:vc + 128], c == 0, c == 1, [wukv, self.ckvT], [p_])
                self.cp("dve", Vh[:, tt, 0:128], p_[:, 0:128], [p_], [Vh])
            for ci, (t0, n) in enumerate(chunks(TC, TA)):
                p_ = pP[ci % 2]
                for c in range(3):
                    self.mm(p_[:, 0:n], wuq[:, c, qc:qc + 128], self.qlatT[:, c, t0:t0 + n], c == 0, c == 2, [wuq, self.qlatT], [p_])
                self.act(QnT[:, t0:t0 + n], p_[:, 0:n], AF.Copy, [p_], [QnT], scale=qscale)
                p2 = pP[(ci + 1) % 2]
                for c in range(3):
                    self.mm(p2[:, 0:n], wuqr[:, c, h, :], self.qlatT[:, c, t0:t0 + n], c == 0, c == 2, [wuqr, self.qlatT], [p2])
                self.act(raw[:, 0:n], p2[:, 0:n], AF.Copy, [p2], [raw], scale=qscale)
                self.mm(p2[:, 0:n], pmat[:], raw[:, 0:n], True, True, [pmat, raw], [p2])
                self.tt("dve", u1[:, 0:n], raw[:, 0:n], CT[:, 0, t0:t0 + n], ALU.mult, [raw, CT], [u1])
                self.tt("dve", u2[:, 0:n], p2[:, 0:n], CT[:, 1, t0:t0 + n], ALU.mult, [p2, CT], [u2])
                self.tt("dve", QrT[:, t0:t0 + n], u1[:, 0:n], u2[:, 0:n], ALU.add, [u1, u2], [QrT])
            parts = [(lambda q0, n: QnT[:, q0:q0 + n], lambda kt: KnT[:, kt * 128:(kt + 1) * 128], QnT, KnT),
                     (lambda q0, n: QrT[:, q0:q0 + n], lambda kt: self.kpeT[:, kt * 128:(kt + 1) * 128], QrT, self.kpeT)]
            V = lambda kt: Vh[:, kt, 0:129]
            self.attention(parts, V, Vh, self.q_chunks(TC, TA, list(range(NTA))), h * 128, bufs)
        k.flush()
        ps.close()
        self.stM.close()

    def build(self):
        self.declare()
        self.pmat_in = self.inp("pmat", [128, 128])
        self.ropeMT = self.inp("ropeMT", [128, 2, self.TA])
        self.consts()
        self.phase0()
        allt = list(range(self.NTA))
        latt = list(range(self.NTC, self.NTA))
        self.l0_passA(); self.l0_attnA(); self.l0_passB(); self.l0_retention()
        self.post_mixer(0, self.ab_w_out, allt)
        self.moe(0, allt, lambda tt: (self.x2, self.x2[tt * 128:(tt + 1) * 128, :]), GT=self.GT, SC=self.SC)
        if self.stop == "l0":
            return self.k.emit()
        self.l1_passM(); self.l1_attn()
        self.post_mixer(1, self.mla_w_out, latt)
        self.moe(1, latt, lambda tt: (self.y_out, self.y_out[(tt - self.NTC) * 128:(tt - self.NTC + 1) * 128, :]), GT=self.GT, SC=self.SC)
        return self.k.emit()


_TL, _TC, _NB = 4096, 256, 8


def kernel(**inputs):
    maps = prep_inputs(inputs, _TL, _TC, _NB)
    b = B2(_TL, _TC)
    nc = b.build()
    res = run_bass_kernel_spmd(nc, maps, core_ids=list(range(_NB)))
    return np.stack([np.asarray(r["y"], dtype=np.float32) for r in res.results], 0)
```

```python
import numpy as np
import ml_dtypes
from contextlib import ExitStack
import concourse.bass as bass
import concourse.mybir as mybir
from concourse.bass_utils import run_bass_kernel_spmd

F32 = mybir.dt.float32
BF16 = mybir.dt.bfloat16
I32 = mybir.dt.int32
AF = mybir.ActivationFunctionType
ALU = mybir.AluOpType
AX = mybir.AxisListType

D = 1024
GRID_W = 64
THETA = 10000.0
NE = 32
LIMIT = 7.0
SW_ALPHA = 1.702
DN_ALPHA = 4.0 ** 0.25


class SemSlot:
    __slots__ = ("sem", "count")

    def __init__(self):
        self.sem = None
        self.count = 0


class Res:
    __slots__ = ("name", "slot", "lastw", "readers", "lastdma")

    def __init__(self, name):
        self.name = name
        self.slot = {}
        self.lastw = None
        self.readers = []
        self.lastdma = None


class Op:
    __slots__ = ("eng", "fn", "deps", "marked", "event", "is_dma", "ei", "done")

    def __init__(self, eng, fn):
        self.eng = eng
        self.fn = fn
        self.deps = []
        self.marked = False
        self.event = None
        self.is_dma = False
        self.done = False


class T:
    __slots__ = ("ap", "r")

    def __init__(self, ap, r):
        self.ap = ap
        self.r = r

    def __getitem__(self, idx):
        return self.ap[idx]


def _rs(xs):
    return [x.r if isinstance(x, T) else x for x in xs]


class K:
    ENGS = ("pe", "act", "dve", "pool", "sp")

    def __init__(self):
        self.nc = bass.Bass("TRN2", target_bir_lowering=False)
        self.ops = []
        self.stack = ExitStack()
        self.last_on = {e: None for e in self.ENGS}
        self.owners = []
        self.free_slots = {"hw": [], "sw": []}
        self.n = 0
        self.esem = {e: self.stack.enter_context(self.nc.semaphore(f"s_{e}")) for e in self.ENGS}
        self.cnt = {e: 0 for e in self.ENGS}
        self.waited = {e: {} for e in self.ENGS}
        self.stats = dict(nops=0, nwait=0, ndrain=0, nsem=5)

    def res(self, name=None):
        self.n += 1
        return Res(f"{name or 'r'}{self.n}")

    def sbuf(self, name, shape, dt, stack=None):
        self.n += 1
        ap = (stack or self.stack).enter_context(self.nc.sbuf_tensor(f"{name}_{self.n}", list(shape), dt))
        return T(ap, self.res(name))

    def psum(self, name, shape, dt, stack=None):
        self.n += 1
        ap = (stack or self.stack).enter_context(self.nc.psum_tensor(f"{name}_{self.n}", list(shape), dt))
        return T(ap, self.res(name))

    def dram(self, name, shape, dt, kind="Internal"):
        return T(self.nc.dram_tensor(name, list(shape), dt, kind=kind).ap(), self.res(name))

    def _track(self, op, reads, writes):
        deps = op.deps
        for r in reads:
            if r.lastw is not None:
                deps.append(r.lastw)
            r.readers.append(op)
        for w in writes:
            if w.lastw is not None:
                deps.append(w.lastw)
            deps.extend(w.readers)
            w.lastw = op
            w.readers = []
        seen = set()
        out = []
        for d in deps:
            if d is op or d.done or id(d) in seen:
                continue
            seen.add(id(d))
            if d.eng == "pe" and op.eng == "pe" and not d.is_dma and not op.is_dma:
                continue
            out.append(d)
            if d.eng != op.eng or d.is_dma:
                d.marked = True
        op.deps = out
        self.ops.append(op)
        self.last_on[op.eng] = op

    def op(self, eng, fn, reads=(), writes=()):
        o = Op(eng, fn)
        self._track(o, _rs(reads), _rs(writes))
        return o

    def dma(self, eng, out, in_, owner, reads=(), writes=(), **kw):
        o = Op(eng, lambda e: e.dma_start(out=out, in_=in_, **kw))
        self._dma_common(o, owner, reads, writes)
        return o

    def dma_fn(self, eng, fn, owner, reads=(), writes=()):
        o = Op(eng, fn)
        self._dma_common(o, owner, reads, writes)
        return o

    def _dma_common(self, o, owner, reads, writes):
        owner = owner.r if isinstance(owner, T) else owner
        o.is_dma = True
        o.marked = True
        kind = "sw" if o.eng == "pool" else "hw"
        if kind not in owner.slot:
            fl = self.free_slots[kind]
            owner.slot[kind] = fl.pop() if fl else SemSlot()
            if owner not in self.owners:
                self.owners.append(owner)
        if owner.lastdma is not None and not owner.lastdma.done:
            o.deps.append(owner.lastdma)
        owner.lastdma = o
        sl = owner.slot[kind]
        sl.count += 16
        o.event = (sl, sl.count)
        self._track(o, _rs(reads), _rs(writes))

    def barrier(self):
        lasts = [self.last_on[e] for e in self.ENGS if self.last_on[e] is not None and not self.last_on[e].done]
        lasts += [o.lastdma for o in self.owners if o.lastdma is not None and not o.lastdma.done]
        for e in self.ENGS:
            o = Op(e, None)
            for d in lasts:
                if d.eng == e and not d.is_dma:
                    continue
                if d.fn is None and not d.is_dma:
                    continue
                o.deps.append(d)
                d.marked = True
            self.ops.append(o)
            self.last_on[e] = o

    def flush(self):
        self.barrier()
        nc = self.nc
        esem = self.esem
        for r in self.owners:
            for sl in r.slot.values():
                if sl.sem is None:
                    self.n += 1
                    sl.sem = self.stack.enter_context(nc.semaphore(f"d{self.n}"))
                    self.stats["nsem"] += 1
        for o in self.ops:
            if o.is_dma:
                o.event = (o.event[0].sem, o.event[1])
            elif o.marked:
                self.cnt[o.eng] += 1
                o.event = (esem[o.eng], self.cnt[o.eng])
        by_eng = {e: [o for o in self.ops if o.eng == e] for e in self.ENGS}
        for e in self.ENGS:
            for i, o in enumerate(by_eng[e]):
                o.ei = i

        def run(ename, eng):
            w = self.waited[ename]
            last_drain = -1
            for o in by_eng[ename]:
                need = {}
                drain = False
                for d in o.deps:
                    if d.eng == ename and not d.is_dma:
                        if d.fn is not None and d.ei > last_drain and o.ei - d.ei <= 8:
                            drain = True
                        continue
                    sem, val = d.event
                    key = id(sem)
                    if w.get(key, 0) >= val:
                        continue
                    if key not in need or need[key][1] < val:
                        need[key] = (sem, val)
                for key, (sem, val) in need.items():
                    eng.wait_ge(sem, val)
                    w[key] = val
                    self.stats["nwait"] += 1
                if drain or (o.fn is None and ename != "pe"):
                    eng.drain()
                    last_drain = o.ei - 1
                    self.stats["ndrain"] += 1
                if o.fn is None:
                    continue
                ins = o.fn(eng)
                if o.is_dma:
                    ins.then_inc(o.event[0], 16)
                elif o.marked:
                    ins.then_inc(esem[o.eng], 1)

        with nc.Block() as block:
            block.tensor(lambda e: run("pe", e))
            block.scalar(lambda e: run("act", e))
            block.vector(lambda e: run("dve", e))
            block.gpsimd(lambda e: run("pool", e))
            block.sync(lambda e: run("sp", e))
        self.stats["nops"] += len(self.ops)
        for o in self.ops:
            o.done = True
        self.ops = []
        for r in self.owners:
            for kind, sl in r.slot.items():
                self.free_slots[kind].append(sl)
            r.slot = {}
        self.owners = []

    def emit(self):
        self.flush()
        self.stack.close()
        return self.nc


def _rope_tables(pos, d):
    half = d // 2
    inv = (THETA ** (-np.arange(half, dtype=np.float32) / np.float32(half))).astype(np.float32)
    ang = pos.astype(np.float32)[:, None] * inv[None, :]
    c, s = np.cos(ang).astype(np.float32), np.sin(ang).astype(np.float32)
    return np.concatenate([c, c], 1), np.concatenate([-s, s], 1)


def _axial_tables(n_tok, d):
    rows = np.repeat(np.arange(n_tok // GRID_W), GRID_W)
    cols = np.tile(np.arange(GRID_W), n_tok // GRID_W)
    c1, s1 = _rope_tables(rows, d // 2)
    c2, s2 = _rope_tables(cols, d // 2)
    return np.concatenate([c1, c2], 1), np.concatenate([s1, s2], 1)


def make_tables(TL, TC):
    TA = TL + TC
    ca, sa = _axial_tables(TL, 128)
    CA = np.concatenate([np.ones((TC, 128), np.float32), ca], 0)
    SA = np.concatenate([np.zeros((TC, 128), np.float32), sa], 0)
    cb, sb = _rope_tables(np.arange(TA), 128)
    cm, sm = _axial_tables(TL, 64)
    CM = np.concatenate([np.ones((TC, 64), np.float32), cm], 0)
    SM = np.concatenate([np.zeros((TC, 64), np.float32), sm], 0)
    pm = np.zeros((128, 128), np.float32)
    for m in range(64):
        src = m + 16 if (m % 32) < 16 else m - 16
        pm[src, m] = 1.0
    rmt = np.zeros((128, 2, TA), np.float32)
    rmt[:64, 0] = CM.T
    rmt[:64, 1] = SM.T
    return dict(pmat=pm, ropeMT=rmt,
                ropeA=np.stack([CA, SA], 1).astype(np.float32),
                ropeB=np.stack([np.tile(cb, (1, 4)), np.tile(sb, (1, 4))], 1).astype(np.float32),
                ropeM=np.stack([np.tile(CM, (1, 8)), np.tile(SM, (1, 8))], 1).astype(np.float32))


class B:
    def __init__(self, TL, TC, debug=False, stop=None):
        self.k = K()
        self.TL, self.TC, self.TA = TL, TC, TL + TC
        self.NTL, self.NTC, self.NTA = TL // 128, TC // 128, (TL + TC) // 128
        self.debug = debug
        self.stop = stop
        self.GT = 12
        self.SC = 4

    def mm(self, out, lhsT, rhs, start, stop, reads, writes):
        self.k.op("pe", lambda e: e.matmul(out=out, lhsT=lhsT, rhs=rhs, start=start, stop=stop), reads, writes)

    def tr(self, out, in_, ident, reads, writes):
        self.k.op("pe", lambda e: e.transpose(out=out, in_=in_, identity=ident), reads, writes)

    def act(self, out, in_, func, reads, writes, bias=0.0, scale=1.0, accum_out=None):
        if accum_out is None:
            self.k.op("act", lambda e: e.activation(out=out, in_=in_, func=func, bias=bias, scale=scale), reads, writes)
        else:
            self.k.op("act", lambda e: e.activation(out=out, in_=in_, func=func, bias=bias, scale=scale, accum_out=accum_out), reads, writes)

    def ts(self, eng, out, in0, s1, s2, op0, op1, reads, writes):
        if s2 is None:
            self.k.op(eng, lambda e: e.tensor_scalar(out=out, in0=in0, scalar1=s1, scalar2=None, op0=op0), reads, writes)
        else:
            self.k.op(eng, lambda e: e.tensor_scalar(out=out, in0=in0, scalar1=s1, scalar2=s2, op0=op0, op1=op1), reads, writes)

    def tt(self, eng, out, in0, in1, op, reads, writes):
        self.k.op(eng, lambda e: e.tensor_tensor(out=out, in0=in0, in1=in1, op=op), reads, writes)

    def stt(self, eng, out, in0, scalar, in1, op0, op1, reads, writes):
        self.k.op(eng, lambda e: e.scalar_tensor_tensor(out=out, in0=in0, scalar=scalar, in1=in1, op0=op0, op1=op1), reads, writes)

    def cp(self, eng, out, in_, reads, writes):
        if eng == "act":
            self.k.op("act", lambda e: e.copy(out=out, in_=in_), reads, writes)
        else:
            self.k.op(eng, lambda e: e.tensor_copy(out=out, in_=in_), reads, writes)

    def memset(self, eng, ap, val, writes):
        self.k.op(eng, lambda e: e.memset(ap, val), (), writes)

    def recip(self, out, in_, reads, writes):
        self.k.op("dve", lambda e: e.reciprocal(out=out, in_=in_), reads, writes)

    def dma(self, out, in_, owner, reads=(), writes=(), eng="sp", **kw):
        self.k.dma(eng, out, in_, owner, reads, writes, **kw)

    def scratch(self, name, shape, dt):
        return self.k.dram(name, shape, dt, kind="ExternalOutput" if self.debug else "Internal")

    def inp(self, name, shape, dt=F32):
        return self.k.dram(name, shape, dt, kind="ExternalInput")

    def declare(self):
        TL, TC, TA = self.TL, self.TC, self.TA
        i = self.inp
        self.x_in = i("x", [TL, D]); self.ctx_in = i("ctx", [TC, D])
        self.cv2 = i("cv2", [128, 8, 2])
        self.ada_w = i("ada_w", [2, D, 6 * D]); self.ada_bF = i("ada_bF", [2, 128, 48]); self.ada_b = i("ada_b", [2, 1, 6 * D])
        self.lnF = i("lnF", [2, 128, 4, 8]); self.ln_rows = i("ln_rows", [2, 4, D])
        self.ab_w_in = i("ab_w_in", [D, 3072]); self.ab_qk = i("ab_qk", [2, 128]); self.ab_ld = i("ab_ld", [1, 8])
        self.ab_gn = i("ab_gn", [1, 512]); self.ab_w_out = i("ab_w_out", [D, D])
        self.mla_w_in = i("mla_w_in", [D, 704]); self.mla_qn = i("mla_qn", [1, 384]); self.mla_kvn = i("mla_kvn", [1, 256])
        self.mla_w_uq = i("mla_w_uq", [384, 1536]); self.mla_w_ukv = i("mla_w_ukv", [256, 2048]); self.mla_w_out = i("mla_w_out", [D, D])
        self.r_w = i("r_w", [2, D, NE]); self.r_b = i("r_b", [2, 1, NE])
        ne = getattr(self, "ne_decl", NE)
        self.w_up = i("w_up", [2, ne, D, 2 * D]); self.b_upF = i("b_upF", [2, NE, 128, 16])
        self.w_down = i("w_down", [2, ne, D, D]); self.b_down = i("b_down", [2, NE, D])
        self.identF_in = i("identF", [128, 128])
        self.ropeA = i("ropeA", [TA, 2, 128]); self.ropeB = i("ropeB", [TA, 2, 512]); self.ropeM = i("ropeM", [TA, 2, 512])
        self.y_out = self.k.dram("y", [TL, D], F32, kind="ExternalOutput")
        s = self.scratch
        self.hT0 = s("hT0", [self.NTA, 128, 8, 128], BF16)
        self.cat = s("cat", [TA, D], BF16)
        self.x1 = s("x1", [TA, D], F32)
        self.h2T = s("h2T", [self.NTA, 128, 8, 128], BF16)
        self.x2 = s("x2", [TA, D], F32)
        self.qTB = s("qTB", [4, 128, TA], BF16); self.kTB = s("kTB", [4, 128, TA], BF16)
        self.kB = s("kB", [TA, 512], BF16); self.vB = s("vB", [TA, 512], BF16); self.gB = s("gB", [TA, 512], BF16)

    def consts(self):
        k = self.k
        self.identF = k.sbuf("identF", [128, 128], F32)
        self.dma(self.identF[:], self.identF_in[:, :], self.identF, writes=[self.identF])
        self.identB = k.sbuf("identB", [128, 128], BF16)
        self.cp("dve", self.identB[:], self.identF[:], [self.identF], [self.identB])
        self.ones = k.sbuf("ones", [128, 512], F32)
        self.memset("pool", self.ones[:], 1.0, [self.ones])

    def xsrc(self, tt):
        if tt < self.NTC:
            return self.ctx_in, self.ctx_in[tt * 128:(tt + 1) * 128, :]
        t = tt - self.NTC
        return self.x_in, self.x_in[t * 128:(t + 1) * 128, :]

    def phase0(self):
        k = self.k
        P_modF = [k.sbuf("modF", [128, 48, 2], F32) for _ in range(2)]
        self.G = [k.sbuf("G", [128, self.NTA, NE], F32) for _ in range(2)]
        self.gate_rows = self.scratch("gate_rows", [2, 2, 2, D], F32)
        P_coef = [k.sbuf("coef", [128, 4, 2, 8], F32) for _ in range(2)]
        P_lnF = [k.sbuf("lnF", [128, 4, 8], F32) for _ in range(2)]
        ps = ExitStack()
        cv = k.sbuf("cv", [128, 8, 2], F32, ps)
        self.dma(cv[:], self.cv2[:, :, :], cv, writes=[cv])
        sc2 = k.sbuf("sc2", [128, 8, 2], F32, ps)
        self.act(sc2[:], cv[:], AF.Silu, [cv], [sc2])
        scB = k.sbuf("scB", [128, 8, 2, 128], F32, ps)
        for kk in range(8):
            for s in range(2):
                self.act(scB[:, kk, s, :], self.ones[:, 0:128], AF.Copy, [self.ones, sc2], [scB], scale=sc2[:, kk, s:s + 1])
        wch = [k.sbuf("wch", [128, 8, 512], F32, ps) for _ in range(2)]
        brow = [k.sbuf("brow", [1, 512], F32, ps) for _ in range(2)]
        grow = [k.sbuf("grow", [1, 512], F32, ps) for _ in range(2)]
        abFs = [k.sbuf("abF", [128, 48], F32, ps) for _ in range(2)]
        tmp = k.sbuf("ctmp", [128, 2, 8], F32, ps)
        psm = k.psum("psm", [128, 512], F32, ps)
        psg = [k.psum("psg", [128, 512], F32, ps) for _ in range(2)]
        self.modF, self.coef = [], []
        for i in range(2):
            modF, coef, lnF = P_modF[i], P_coef[i], P_lnF[i]
            abF = abFs[i]
            self.dma(abF[:], self.ada_bF[i], abF, writes=[abF])
            for n in range(12):
                w = wch[n % 2]
                self.dma(w[:], self.ada_w[i][:, n * 512:(n + 1) * 512].rearrange("(k p) n -> p k n", p=128), w, writes=[w])
                for m in range(4):
                    mc = n * 4 + m
                    for kk in range(8):
                        self.mm(psm[:, mc * 2:mc * 2 + 2], w[:, kk, m * 128:(m + 1) * 128], sc2[:, kk, :], kk == 0, kk == 7, [w, sc2], [psm])
                j = n // 2
                if j in (2, 5):
                    br = brow[n % 2]
                    self.dma(br[:], self.ada_b[i][:, n * 512:(n + 1) * 512], br, writes=[br])
                    for s in range(2):
                        for kk in range(8):
                            self.mm(psg[s][:], scB[:, kk, s, :], w[:, kk, :], kk == 0, False, [w, scB], [psg[s]])
                        self.mm(psg[s][:], self.ones[0:1, 0:128], br[:], False, True, [self.ones, br], [psg[s]])
                        gr = grow[(n + s) % 2]
                        self.cp("act", gr[:], psg[s][0:1, :], [psg[s]], [gr])
                        self.dma(self.gate_rows[i, 0 if j == 2 else 1, s:s + 1, (n % 2) * 512:(n % 2 + 1) * 512], gr[:], gr, reads=[gr], writes=[self.gate_rows])
            for s in range(2):
                self.tt("dve", modF[:, :, s], psm[:, s:96:2], abF[:], ALU.add, [psm, abF], [modF])
            self.dma(lnF[:], self.lnF[i], lnF, writes=[lnF])
            for s in range(2):
                self.ts("dve", coef[:, 0, s, :], modF[:, 8:16, s], 1.0, None, ALU.add, None, [modF], [coef])
                self.cp("dve", coef[:, 1, s, :], modF[:, 0:8, s], [modF], [coef])
                self.ts("dve", tmp[:, s, :], modF[:, 32:40, s], 1.0, None, ALU.add, None, [modF], [tmp])
                self.tt("dve", coef[:, 2, s, :], tmp[:, s, :], lnF[:, 0, :], ALU.mult, [tmp, lnF], [coef])
                self.tt("dve", coef[:, 3, s, :], tmp[:, s, :], lnF[:, 1, :], ALU.mult, [tmp, lnF], [coef])
                self.tt("dve", coef[:, 3, s, :], coef[:, 3, s, :], modF[:, 24:32, s], ALU.add, [coef, modF], [coef])
            self.modF.append(modF); self.coef.append(coef)
        k.flush()
        ps.close()

    def dbg(self, name, t, shape, dt=F32):
        if not self.debug:
            return
        d = self.k.dram("dbg_" + name, shape, dt, kind="ExternalOutput")
        self.dma(d.ap, t[:], t, reads=[t], writes=[d])


def _fm(v):
    return np.ascontiguousarray(np.swapaxes(v.reshape(v.shape[:-1] + (8, 128)), -1, -2))


def prep_inputs(inp, TL, TC, nb):
    f = lambda a: np.ascontiguousarray(np.asarray(a, dtype=np.float32))
    tabs = make_tables(TL, TC)
    shared = dict(
        ada_w=f(inp["ada_w"]),
        ada_bF=np.ascontiguousarray(np.transpose(f(inp["ada_b"]).reshape(2, 48, 128), (0, 2, 1))),
        ada_b=f(inp["ada_b"]).reshape(2, 1, 6 * D),
        lnF=np.ascontiguousarray(np.stack([_fm(f(inp[n])) for n in ("ln1_g", "ln1_b", "ln2_g", "ln2_b")], 2)),
        ln_rows=np.ascontiguousarray(np.stack([f(inp[n]) for n in ("ln1_g", "ln1_b", "ln2_g", "ln2_b")], 1)),
        ab_w_in=f(inp["ab_w_in"])[0], ab_qk=np.concatenate([f(inp["ab_q_norm"]), f(inp["ab_k_norm"])], 0),
        ab_ld=f(inp["ab_log_decay"]).reshape(1, 8), ab_gn=f(inp["ab_gn_g"]).reshape(1, 512), ab_w_out=f(inp["ab_w_out"])[0],
        mla_w_in=f(inp["mla_w_in"])[0], mla_qn=f(inp["mla_q_norm"]).reshape(1, 384), mla_kvn=f(inp["mla_kv_norm"]).reshape(1, 256),
        mla_w_uq=f(inp["mla_w_uq"])[0], mla_w_ukv=f(inp["mla_w_ukv"])[0], mla_w_out=f(inp["mla_w_out"])[0],
        r_w=f(inp["moe_router_w"]), r_b=f(inp["moe_router_b"]).reshape(2, 1, NE),
        w_up=f(inp["moe_w_up"]),
        b_upF=np.ascontiguousarray(np.transpose(f(inp["moe_b_up"]).reshape(2, NE, 16, 128), (0, 1, 3, 2))),
        w_down=f(inp["moe_w_down"]), b_down=f(inp["moe_b_down"]),
        identF=np.eye(128, dtype=np.float32), **tabs)
    x, c, ctx, c_ctx = f(inp["x"]), f(inp["c"]), f(inp["ctx"]), f(inp["c_ctx"])
    maps = []
    for b in range(nb):
        cv2 = np.ascontiguousarray(np.stack([_fm(c[b]), _fm(c_ctx)], -1))
        maps.append(dict(x=x[b], ctx=ctx[b], cv2=cv2, **shared))
    return maps


class B2(B):
    def load_hT(self, src_t, src_ap, xt, pT, hT, coef, s, ident, a_idx=0):
        self.dma(xt[:], src_ap, xt, reads=[src_t], writes=[xt])
        for half in range(2):
            for j in range(4):
                c = half * 4 + j
                self.tr(pT[half][:, j * 128:(j + 1) * 128], xt[:, c * 128:(c + 1) * 128], ident[:], [xt, ident], [pT[half]])
            for j in range(4):
                c = half * 4 + j
                self.act(hT[:, c, :], pT[half][:, j * 128:(j + 1) * 128], AF.Identity, [pT[half], coef], [hT],
                         bias=coef[:, a_idx + 1, s, c:c + 1], scale=coef[:, a_idx, s, c:c + 1])

    def rope(self, out, out_t, xin, C, S, nblk, h, tmp1, tmp2, reads):
        n = nblk * 2 * h
        v = lambda ap: ap.rearrange("p (b two h) -> p b two h", two=2, h=h)
        self.tt("dve", tmp1[:, 0:n], xin, C, ALU.mult, reads, [tmp1])
        self.tt("pool", v(tmp2[:, 0:n])[:, :, 0, :], v(xin)[:, :, 1, :], v(S)[:, :, 0, :], ALU.mult, reads, [tmp2])
        self.tt("pool", v(tmp2[:, 0:n])[:, :, 1, :], v(xin)[:, :, 0, :], v(S)[:, :, 1, :], ALU.mult, reads, [tmp2])
        self.tt("dve", out, tmp1[:, 0:n], tmp2[:, 0:n], ALU.add, [tmp1, tmp2], [out_t])

    def l0_passA(self):
        k = self.k
        NTA, TA = self.NTA, self.TA
        self.stA = ExitStack()
        self.QT = k.sbuf("QT", [128, 4, TA], BF16, self.stA)
        self.KT = k.sbuf("KT", [128, 2, TA], BF16, self.stA)
        self.VA = k.sbuf("VA", [128, NTA, 2, 130], BF16, self.stA)
        ps = ExitStack()
        wA = k.sbuf("wA", [128, 8, 1024], BF16, ps)
        self.dma(wA[:], self.ab_w_in[:, 0:1024].rearrange("(k p) n -> p k n", p=128), wA, writes=[wA], eng="pool")
        gq = k.sbuf("gq", [128, 2, 128], F32, ps)
        self.dma(gq[:, 0, :], self.ab_qk[0:1, :].partition_broadcast(128), gq, writes=[gq])
        self.dma(gq[:, 1, :], self.ab_qk[1:2, :].partition_broadcast(128), gq, writes=[gq])
        self.k.op("act", lambda e: e.mul(out=gq[:, 0, :], in_=gq[:, 0, :], mul=128.0 ** -0.5), [gq], [gq])
        self.memset("pool", self.VA[:], 1.0, [self.VA])
        xt = [k.sbuf("xt", [128, D], F32, ps) for _ in range(2)]
        hT = [k.sbuf("hT", [128, 8, 128], BF16, ps) for _ in range(2)]
        tab = [k.sbuf("tabA", [128, 2, 128], F32, ps) for _ in range(2)]
        ss = k.sbuf("ss", [128, 8], F32, ps)
        rstd = k.sbuf("rstd", [128, 8], F32, ps)
        junk = k.sbuf("junk", [128, 128], F32, ps)
        xn = [k.sbuf("xn", [128, 128], F32, ps) for _ in range(2)]
        t1 = [k.sbuf("t1", [128, 128], F32, ps) for _ in range(2)]
        t2 = [k.sbuf("t2", [128, 128], F32, ps) for _ in range(2)]
        ob = [k.sbuf("ob", [128, 128], BF16, ps) for _ in range(2)]
        pT = [k.psum("pT", [128, 512], F32, ps) for _ in range(2)]
        pA = [k.psum("pA", [128, 1024], F32, ps) for _ in range(2)]
        pTb = k.psum("pTb", [128, 1024], BF16, ps)
        coef = self.coef[0]
        for tt in range(NTA):
            s = 1 if tt < self.NTC else 0
            src_t, src_ap = self.xsrc(tt)
            x_, h_, tb, pa = xt[tt % 2], hT[tt % 2], tab[tt % 2], pA[tt % 2]
            self.load_hT(src_t, src_ap, x_, pT, h_, coef, s, self.identF)
            self.dma(self.hT0[tt], h_[:], h_, reads=[h_], writes=[self.hT0])
            self.dma(tb[:], self.ropeA[tt * 128:(tt + 1) * 128], tb, writes=[tb])
            for n in range(2):
                for c in range(8):
                    self.mm(pa[:, n * 512:(n + 1) * 512], h_[:, c, :], wA[:, c, n * 512:(n + 1) * 512], c == 0, c == 7, [h_, wA], [pa])
            for hh in range(6):
                self.act(junk[:], pa[:, hh * 128:(hh + 1) * 128], AF.Square, [pa], [junk, ss], accum_out=ss[:, hh:hh + 1])
            self.act(rstd[:, 0:6], ss[:, 0:6], AF.Sqrt, [ss], [rstd], bias=1e-6, scale=1.0 / 128)
            self.recip(rstd[:, 0:6], rstd[:, 0:6], [rstd], [rstd])
            for hh in range(6):
                i2 = hh % 2
                g = gq[:, 0, :] if hh < 4 else gq[:, 1, :]
                self.stt("dve", xn[i2][:], pa[:, hh * 128:(hh + 1) * 128], rstd[:, hh:hh + 1], g, ALU.mult, ALU.mult, [pa, rstd, gq], [xn[i2]])
                self.rope(ob[i2][:], ob[i2], xn[i2][:], tb[:, 0, :], tb[:, 1, :], 2, 32, t1[i2], t2[i2], [xn[i2], tb])
                self.tr(pTb[:, hh * 128:(hh + 1) * 128], ob[i2][:], self.identB[:], [ob[i2], self.identB], [pTb])
            self.cp("act", self.QT[:, :, tt * 128:(tt + 1) * 128], pTb[:, 0:512].rearrange("p (h t) -> p h t", h=4), [pTb], [self.QT])
            self.cp("act", self.KT[:, :, tt * 128:(tt + 1) * 128], pTb[:, 512:768].rearrange("p (h t) -> p h t", h=2), [pTb], [self.KT])
            self.cp("dve", self.VA[:, tt, :, 0:128], pa[:, 768:1024].rearrange("p (h d) -> p h d", h=2), [pa], [self.VA])
        k.flush()
        ps.close()

    def attention(self, parts, V, vres, q_ranges, cat_col, bufs, exp_bias=None):
        pS, pO, PT, osb, rec = bufs
        np_ = len(parts)
        it = 0
        for (q0, nq, ktiles) in q_ranges:
            nqb = nq // 128
            nk = len(ktiles)
            pts = []

            def emit_S(ki, it_):
                sp = pS[it_ % 2]
                pt = PT[it_ % 2]
                kt = ktiles[ki]
                for pi, (Qap, Kap, qres, kres) in enumerate(parts):
                    self.mm(sp[:, 0:nq], Kap(kt), Qap(q0, nq), pi == 0, pi == np_ - 1, [qres, kres], [sp])
                if exp_bias is None:
                    self.act(pt[:, 0:nq], sp[:, 0:nq], AF.Exp, [sp], [pt])
                else:
                    self.act(pt[:, 0:nq], sp[:, 0:nq], AF.Exp, [sp, exp_bias[1]], [pt], bias=exp_bias[0])
                return pt

            pts.append(emit_S(0, it))
            for ki in range(nk):
                if ki + 1 < nk:
                    pts.append(emit_S(ki + 1, it + ki + 1))
                pt = pts[ki]
                kt = ktiles[ki]
                for qb in range(nqb):
                    self.mm(pO[qb][:, 0:129], pt[:, qb * 128:(qb + 1) * 128], V(kt), ki == 0, ki == nk - 1, [pt, vres], [pO[qb]])
            it += nk
            ob = osb[(q0 // 512) % 2]
            for qb in range(nqb):
                self.recip(rec[:, qb:qb + 1], pO[qb][:, 128:129], [pO[qb]], [rec])
                self.act(ob[:, qb, :], pO[qb][:, 0:128], AF.Copy, [pO[qb], rec], [ob], scale=rec[:, qb:qb + 1])
            self.dma(self.cat[q0:q0 + nq, cat_col:cat_col + 128].rearrange("(qb p) c -> p qb c", p=128), ob[:, 0:nqb, :], ob,
                     reads=[ob], writes=[self.cat])

    def attn_bufs(self, ps):
        k = self.k
        pS = [k.psum("pS", [128, 512], F32, ps) for _ in range(2)]
        pO = [k.psum("pO", [128, 512], F32, ps) for _ in range(4)]
        PT = [k.sbuf("PT", [128, 512], BF16, ps) for _ in range(2)]
        osb = [k.sbuf("osb", [128, 4, 128], BF16, ps) for _ in range(2)]
        rec = k.sbuf("rec", [128, 4], F32, ps)
        return pS, pO, PT, osb, rec

    def q_chunks(self, q0, q1, ktiles):
        out = []
        q = q0
        while q < q1:
            n = min(512, q1 - q)
            out.append((q, n, ktiles))
            q += n
        return out

    def l0_attnA(self):
        ps = ExitStack()
        bufs = self.attn_bufs(ps)
        TC, TA, NTC, NTA = self.TC, self.TA, self.NTC, self.NTA
        for h in range(4):
            kvh = h // 2
            parts = [(lambda q0, n, h=h: self.QT[:, h, q0:q0 + n], lambda kt, kvh=kvh: self.KT[:, kvh, kt * 128:(kt + 1) * 128], self.QT, self.KT)]
            V = lambda kt, kvh=kvh: self.VA[:, kt, kvh, 0:129]
            ranges = self.q_chunks(0, TC, list(range(NTC))) + self.q_chunks(TC, TA, list(range(NTA)))
            self.attention(parts, V, self.VA, ranges, h * 128, bufs)
        self.k.flush()
        ps.close()
        self.stA.close()

    def l0_passB(self):
        k = self.k
        NTA = self.NTA
        ps = ExitStack()
        wB = k.sbuf("wB", [128, 8, 2048], BF16, ps)
        for hf in range(2):
            self.dma(wB[:, :, hf * 1024:(hf + 1) * 1024], self.ab_w_in[:, 1024 + hf * 1024:2048 + hf * 1024].rearrange("(k p) n -> p k n", p=128),
                     wB, writes=[wB], eng="pool")
        gnB = k.sbuf("gnB", [128, 512], F32, ps)
        self.dma(gnB[:], self.ab_gn[0:1, :].partition_broadcast(128), gnB, writes=[gnB])
        hT = [k.sbuf("hTb", [128, 8, 128], BF16, ps) for _ in range(2)]
        tab = [k.sbuf("tabB", [128, 2, 512], F32, ps) for _ in range(2)]
        xs = [k.sbuf("xsB", [128, 512], F32, ps) for _ in range(2)]
        t1 = k.sbuf("t1B", [128, 512], F32, ps)
        t2 = k.sbuf("t2B", [128, 512], F32, ps)
        qr = [k.sbuf("qrB", [128, 512], BF16, ps) for _ in range(2)]
        kr = [k.sbuf("krB", [128, 512], BF16, ps) for _ in range(2)]
        vb = [k.sbuf("vbB", [128, 512], BF16, ps) for _ in range(2)]
        gb = [k.sbuf("gbB", [128, 512], BF16, ps) for _ in range(2)]
        gs = k.sbuf("gsB", [128, 512], F32, ps)
        stg = [k.sbuf("stgB", [128, 8, 128], BF16, ps) for _ in range(2)]
        pB = k.psum("pB", [128, 2048], F32, ps)
        pTb = k.psum("pTbB", [128, 1024], BF16, ps)
        for tt in range(NTA):
            i2 = tt % 2
            h_, tb = hT[i2], tab[i2]
            rows = slice(tt * 128, (tt + 1) * 128)
            self.dma(h_[:], self.hT0[tt], h_, reads=[self.hT0], writes=[h_])
            self.dma(tb[:], self.ropeB[rows], tb, writes=[tb])
            for n in range(4):
                for c in range(8):
                    self.mm(pB[:, n * 512:(n + 1) * 512], h_[:, c, :], wB[:, c, n * 512:(n + 1) * 512], c == 0, c == 7, [h_, wB], [pB])
            self.act(xs[0][:], pB[:, 0:512], AF.Copy, [pB], [xs[0]], scale=128.0 ** -0.5)
            self.rope(qr[i2][:], qr[i2], xs[0][:], tb[:, 0, :], tb[:, 1, :], 4, 64, t1, t2, [xs[0], tb])
            self.act(xs[1][:], pB[:, 512:1024], AF.Copy, [pB], [xs[1]])
            self.rope(kr[i2][:], kr[i2], xs[1][:], tb[:, 0, :], tb[:, 1, :], 4, 64, t1, t2, [xs[1], tb])
            self.dma(self.kB[rows, :], kr[i2][:], kr[i2], reads=[kr[i2]], writes=[self.kB])
            for hh in range(4):
                self.tr(pTb[:, hh * 128:(hh + 1) * 128], qr[i2][:, hh * 128:(hh + 1) * 128], self.identB[:], [qr[i2], self.identB], [pTb])
                self.tr(pTb[:, 512 + hh * 128:512 + (hh + 1) * 128], kr[i2][:, hh * 128:(hh + 1) * 128], self.identB[:], [kr[i2], self.identB], [pTb])
            self.cp("act", stg[i2][:], pTb[:].rearrange("p (h t) -> p h t", h=8), [pTb], [stg[i2]])
            self.dma(self.qTB[:, :, rows].rearrange("h d t -> d h t"), stg[i2][:, 0:4, :], stg[i2], reads=[stg[i2]], writes=[self.qTB])
            self.dma(self.kTB[:, :, rows].rearrange("h d t -> d h t"), stg[i2][:, 4:8, :], stg[i2], reads=[stg[i2]], writes=[self.kTB])
            self.cp("dve", vb[i2][:], pB[:, 1024:1536], [pB], [vb[i2]])
            self.dma(self.vB[rows, :], vb[i2][:], vb[i2], reads=[vb[i2]], writes=[self.vB])
            self.act(gs[:], pB[:, 1536:2048], AF.Silu, [pB], [gs])
            self.tt("dve", gb[i2][:], gs[:], gnB[:], ALU.mult, [gs, gnB], [gb[i2]])
            self.dma(self.gB[rows, :], gb[i2][:], gb[i2], reads=[gb[i2]], writes=[self.gB])
        k.flush()
        ps.close()

    def l0_retention(self):
        k = self.k
        NTA, NTC, TA = self.NTA, self.NTC, self.TA
        ps = ExitStack()
        ld = k.sbuf("ld", [128, 8], F32, ps)
        self.dma(ld[:], self.ab_ld[0:1, :].partition_broadcast(128), ld, writes=[ld])
        lg = k.sbuf("lg", [128, 8], F32, ps)
        nlg = k.sbuf("nlg", [128, 8], F32, ps)
        self.act(lg[:], ld[:], AF.Exp, [ld], [lg])
        self.ts("dve", lg[:], lg[:], -1.0, 1.0, ALU.mult, ALU.add, [lg], [lg])
        self.act(lg[:], lg[:], AF.Ln, [lg], [lg])
        self.ts("dve", nlg[:], lg[:], -1.0, None, ALU.mult, None, [lg], [nlg])
        ii = k.sbuf("ii", [128, 128], I32, ps)
        dif = k.sbuf("dif", [128, 128], F32, ps)
        c1 = k.sbuf("c1", [128, 128], F32, ps)
        c128 = k.sbuf("c128", [128, 128], F32, ps)
        pcol = k.sbuf("pcol", [128, 4], F32, ps)
        k.op("pool", lambda e: e.iota(ii[:], [[1, 128]], base=0, channel_multiplier=-1), (), [ii])
        self.cp("dve", dif[:], ii[:], [ii], [dif])
        k.op("pool", lambda e: e.iota(ii[:], [[1, 128]], base=1, channel_multiplier=0), [dif], [ii])
        self.cp("dve", c1[:], ii[:], [ii], [c1])
        self.ts("dve", c128[:], c1[:], -1.0, 129.0, ALU.mult, ALU.add, [c1], [c128])
        k.op("pool", lambda e: e.iota(ii[:, 0:1], [[1, 1]], base=0, channel_multiplier=1), [c1], [ii])
        self.cp("dve", pcol[:, 0:1], ii[:, 0:1], [ii], [pcol])
        self.ts("dve", pcol[:, 1:2], pcol[:, 0:1], -1.0, 127.0, ALU.mult, ALU.add, [pcol], [pcol])
        self.memset("dve", pcol[:, 2:3], 128.0, [pcol])
        mf = k.sbuf("mf", [128, 128], F32, ps)
        mb = k.sbuf("mb", [128, 128], F32, ps)
        self.ts("dve", mf[:], dif[:], 0.0, None, ALU.is_ge, None, [dif], [mf])
        self.ts("dve", mb[:], dif[:], 0.0, None, ALU.is_lt, None, [dif], [mb])
        DT = k.sbuf("DT", [128, 128], F32, ps)
        DT2 = k.sbuf("DT2", [128, 128], F32, ps)
        xiF = k.sbuf("xiF", [128, 128], F32, ps)
        xiB = k.sbuf("xiB", [128, 128], F32, ps)
        zc = k.sbuf("zc", [128, 4], F32, ps)
        qT = k.sbuf("qTr", [128, TA], BF16, ps)
        kT = k.sbuf("kTr", [128, TA], BF16, ps)
        kk = k.sbuf("kkr", [128, NTA, 128], BF16, ps)
        vv = k.sbuf("vvr", [128, NTA, 128], BF16, ps)
        gg = k.sbuf("ggr", [128, NTA, 128], BF16, ps)
        Kzf = k.sbuf("Kzf", [128, NTA, 128], BF16, ps)
        Kzb = k.sbuf("Kzb", [128, NTA, 128], BF16, ps)
        SfP = k.sbuf("SfP", [128, NTA, 128], BF16, ps)
        SbP = k.sbuf("SbP", [128, NTA, 128], BF16, ps)
        Sf = k.sbuf("Sf", [128, 128], F32, ps)
        Sb = k.sbuf("Sb", [128, 128], F32, ps)
        Pm = [k.sbuf("Pm", [128, 128], BF16, ps) for _ in range(2)]
        qxf = [k.sbuf("qxf", [128, 128], BF16, ps) for _ in range(2)]
        qxb = [k.sbuf("qxb", [128, 128], BF16, ps) for _ in range(2)]
        st6 = k.sbuf("st6", [128, 6], F32, ps)
        mv = k.sbuf("mv", [128, 4], F32, ps)
        on = [k.sbuf("on", [128, 128], F32, ps) for _ in range(2)]
        ystg = k.sbuf("ystg", [128, NTA, 128], BF16, ps)
        pkv = [k.psum("pkv", [128, 512], F32, ps) for _ in range(2)]
        psc = [k.psum("psc", [128, 512], F32, ps) for _ in range(2)]
        pO = [k.psum("pOr", [128, 512], F32, ps) for _ in range(2)]
        order_b = list(range(NTC - 1, -1, -1)) + list(range(NTA - 1, NTC - 1, -1))
        for h in range(4):
            hs = slice(h * 128, (h + 1) * 128)
            self.dma(qT[:], self.qTB[h], qT, reads=[self.qTB], writes=[qT])
            self.dma(kT[:], self.kTB[h], kT, reads=[self.kTB], writes=[kT])
            for n0 in range(0, NTA, 8):
                n1 = min(NTA, n0 + 8)
                rs = slice(n0 * 128, n1 * 128)
                self.dma(kk[:, n0:n1, :], self.kB[rs, hs].rearrange("(n p) d -> p n d", p=128), kk, reads=[self.kB], writes=[kk])
                self.dma(vv[:, n0:n1, :], self.vB[rs, hs].rearrange("(n p) d -> p n d", p=128), vv, reads=[self.vB], writes=[vv])
                self.dma(gg[:, n0:n1, :], self.gB[rs, hs].rearrange("(n p) d -> p n d", p=128), gg, reads=[self.gB], writes=[gg])
            lf, lb, nlb = lg[:, h:h + 1], lg[:, 4 + h:5 + h], nlg[:, 4 + h:5 + h]
            self.act(DT[:], dif[:], AF.Exp, [dif, lg], [DT], scale=lf)
            self.tt("dve", DT[:], DT[:], mf[:], ALU.mult, [DT, mf], [DT])
            self.act(DT2[:], dif[:], AF.Exp, [dif, nlg], [DT2], scale=nlb)
            self.tt("dve", DT2[:], DT2[:], mb[:], ALU.mult, [DT2, mb], [DT2])
            self.tt("dve", DT[:], DT[:], DT2[:], ALU.add, [DT, DT2], [DT])
            self.act(xiF[:], c1[:], AF.Exp, [c1, lg], [xiF], scale=lf)
            self.act(xiB[:], c128[:], AF.Exp, [c128, lg], [xiB], scale=lb)
            self.act(zc[:, 0:1], pcol[:, 1:2], AF.Exp, [pcol, lg], [zc], scale=lf)
            self.act(zc[:, 1:2], pcol[:, 0:1], AF.Exp, [pcol, lg], [zc], scale=lb)
            self.act(zc[:, 2:3], pcol[:, 2:3], AF.Exp, [pcol, lg], [zc], scale=lf)
            self.act(zc[:, 3:4], pcol[:, 2:3], AF.Exp, [pcol, lg], [zc], scale=lb)
            self.ts("dve", Kzf[:], kk[:], zc[:, 0:1], None, ALU.mult, None, [kk, zc], [Kzf])
            self.ts("pool", Kzb[:], kk[:], zc[:, 1:2], None, ALU.mult, None, [kk, zc], [Kzb])
            self.memset("dve", Sf[:], 0.0, [Sf])
            self.memset("pool", Sb[:], 0.0, [Sb])
            for it, n in enumerate(range(NTA)):
                p_ = pkv[it % 2]
                self.cp("act", SfP[:, n, :], Sf[:], [Sf], [SfP])
                self.mm(p_[:, 0:128], Kzf[:, n, :], vv[:, n, :], True, True, [Kzf, vv], [p_])
                self.stt("dve", Sf[:], Sf[:], zc[:, 2:3], p_[:, 0:128], ALU.mult, ALU.add, [Sf, zc, p_], [Sf])
            for it, n in enumerate(order_b):
                p_ = pkv[it % 2]
                self.cp("act", SbP[:, n, :], Sb[:], [Sb], [SbP])
                self.mm(p_[:, 0:128], Kzb[:, n, :], vv[:, n, :], True, True, [Kzb, vv], [p_])
                self.stt("dve", Sb[:], Sb[:], zc[:, 3:4], p_[:, 0:128], ALU.mult, ALU.add, [Sb, zc, p_], [Sb])
            for n in range(NTA):
                i2 = n % 2
                cs = slice(n * 128, (n + 1) * 128)
                self.mm(psc[i2][:, 0:128], kT[:, cs], qT[:, cs], True, True, [kT, qT], [psc[i2]])
                self.tt("dve", Pm[i2][:], psc[i2][:, 0:128], DT[:], ALU.mult, [psc[i2], DT], [Pm[i2]])
                self.tt("pool", qxf[i2][:], qT[:, cs], xiF[:], ALU.mult, [qT, xiF], [qxf[i2]])
                self.tt("pool", qxb[i2][:], qT[:, cs], xiB[:], ALU.mult, [qT, xiB], [qxb[i2]])
                o_ = pO[i2]
                self.mm(o_[:, 0:128], Pm[i2][:], vv[:, n, :], True, False, [Pm[i2], vv], [o_])
                self.mm(o_[:, 0:128], qxf[i2][:], SfP[:, n, :], False, False, [qxf[i2], SfP], [o_])
                self.mm(o_[:, 0:128], qxb[i2][:], SbP[:, n, :], False, True, [qxb[i2], SbP], [o_])
                k.op("dve", lambda e, o_=o_: e.bn_stats(out=st6[:], in_=o_[:, 0:128]), [o_], [st6])
                k.op("dve", lambda e: e.bn_aggr(out=mv[:, 0:2], in_=st6[:]), [st6], [mv])
                self.act(mv[:, 2:3], mv[:, 1:2], AF.Sqrt, [mv], [mv], bias=1e-5)
                self.recip(mv[:, 2:3], mv[:, 2:3], [mv], [mv])
                self.stt("dve", mv[:, 3:4], mv[:, 0:1], -1.0, mv[:, 2:3], ALU.mult, ALU.mult, [mv], [mv])
                self.act(on[i2][:], o_[:, 0:128], AF.Identity, [o_, mv], [on[i2]], bias=mv[:, 3:4], scale=mv[:, 2:3])
                self.tt("pool", ystg[:, n, :], on[i2][:], gg[:, n, :], ALU.mult, [on[i2], gg], [ystg])
            for n0 in range(0, NTA, 8):
                n1 = min(NTA, n0 + 8)
                self.dma(self.cat[n0 * 128:n1 * 128, 512 + h * 128:512 + (h + 1) * 128].rearrange("(n p) c -> p n c", p=128), ystg[:, n0:n1, :], ystg,
                         reads=[ystg], writes=[self.cat])
        k.flush()
        ps.close()

    def layernorm(self, zn, z, st, mv, eps=1e-5):
        k = self.k
        for hf in range(2):
            k.op("dve", lambda e, hf=hf: e.bn_stats(out=st[:, hf * 6:(hf + 1) * 6], in_=z[:, hf * 512:(hf + 1) * 512]), [z], [st])
        k.op("dve", lambda e: e.bn_aggr(out=mv[:, 0:2], in_=st[:, 0:12]), [st], [mv])
        self.act(mv[:, 2:3], mv[:, 1:2], AF.Sqrt, [mv], [mv], bias=eps)
        self.recip(mv[:, 2:3], mv[:, 2:3], [mv], [mv])
        self.stt("dve", mv[:, 3:4], mv[:, 0:1], -1.0, mv[:, 2:3], ALU.mult, ALU.mult, [mv], [mv])
        self.act(zn[:], z[:], AF.Identity, [z, mv], [zn], bias=mv[:, 3:4], scale=mv[:, 2:3])

    def resid_src(self, i, tt):
        if i == 0:
            return self.xsrc(tt)
        return self.x2, self.x2[tt * 128:(tt + 1) * 128, :]

    def post_mixer(self, i, w_out, tiles):
        k = self.k
        ps = ExitStack()
        wo = k.sbuf("wo", [128, 8, D], BF16, ps)
        self.dma(wo[:], w_out[:, :].rearrange("(k p) n -> p k n", p=128), wo, writes=[wo], eng="pool")
        rw = k.sbuf("rw", [128, 8, NE], F32, ps)
        self.dma(rw[:], self.r_w[i].rearrange("(k p) e -> p k e", p=128), rw, writes=[rw])
        rb = k.sbuf("rb", [1, NE], F32, ps)
        self.dma(rb[:], self.r_b[i], rb, writes=[rb])
        lnB = k.sbuf("lnB", [128, 2, D], F32, ps)
        for q in range(2):
            self.dma(lnB[:, q, :], self.ln_rows[i][q:q + 1, :].partition_broadcast(128), lnB, writes=[lnB])
        gB = k.sbuf("g2B", [128, 2, D], F32, ps)
        for s in range(2):
            self.dma(gB[:, s, :], self.gate_rows[i, 0, s:s + 1, :].partition_broadcast(128), gB, reads=[self.gate_rows], writes=[gB])
        ct = [k.sbuf("ct", [128, D], BF16, ps) for _ in range(2)]
        catT = k.sbuf("catT", [128, 8, 128], BF16, ps)
        xt = [k.sbuf("xtp", [128, D], F32, ps) for _ in range(2)]
        z = k.sbuf("z", [128, D], F32, ps)
        zn = k.sbuf("zn", [128, D], F32, ps)
        xl = [k.sbuf("xl", [128, D], F32, ps) for _ in range(2)]
        st = k.sbuf("st", [128, 12], F32, ps)
        mv = k.sbuf("mvp", [128, 4], F32, ps)
        h2f = k.sbuf("h2f", [128, 8, 128], F32, ps)
        h2b = [k.sbuf("h2b", [128, 8, 128], BF16, ps) for _ in range(2)]
        lgt = k.sbuf("lgt", [128, NE], F32, ps)
        m8 = k.sbuf("m8", [128, 8], F32, ps)
        msk = k.sbuf("msk", [128, NE], F32, ps)
        ex = k.sbuf("ex", [128, NE], F32, ps)
        den = k.sbuf("den", [128, 2], F32, ps)
        pTb = k.psum("pTbp", [128, 1024], BF16, ps)
        pY = k.psum("pYp", [128, 1024], F32, ps)
        pT = [k.psum("pTp", [128, 512], F32, ps) for _ in range(2)]
        pR = k.psum("pR", [128, 512], F32, ps)
        coef = self.coef[i]
        G = self.G[i]
        for it, tt in enumerate(tiles):
            i2 = it % 2
            s = 1 if tt < self.NTC else 0
            rows = slice(tt * 128, (tt + 1) * 128)
            c_ = ct[i2]
            self.dma(c_[:], self.cat[rows, :], c_, reads=[self.cat], writes=[c_])
            for c in range(8):
                self.tr(pTb[:, c * 128:(c + 1) * 128], c_[:, c * 128:(c + 1) * 128], self.identB[:], [c_, self.identB], [pTb])
            self.cp("act", catT[:], pTb[:].rearrange("p (c t) -> p c t", c=8), [pTb], [catT])
            for n in range(2):
                for c in range(8):
                    self.mm(pY[:, n * 512:(n + 1) * 512], catT[:, c, :], wo[:, c, n * 512:(n + 1) * 512], c == 0, c == 7, [catT, wo], [pY])
            src_t, src_ap = self.resid_src(i, tt)
            x_ = xt[i2]
            self.dma(x_[:], src_ap, x_, reads=[src_t], writes=[x_])
            self.tt("dve", z[:], pY[:], gB[:, s, :], ALU.mult, [pY, gB], [z])
            self.stt("dve", z[:], x_[:], DN_ALPHA, z[:], ALU.mult, ALU.add, [x_, z], [z])
            self.layernorm(zn, z, st, mv)
            x1_ = xl[i2]
            self.tt("pool", x1_[:], zn[:], lnB[:, 0, :], ALU.mult, [zn, lnB], [x1_])
            self.tt("pool", x1_[:], x1_[:], lnB[:, 1, :], ALU.add, [x1_, lnB], [x1_])
            self.dma(self.x1[rows, :], x1_[:], x1_, reads=[x1_], writes=[self.x1])
            for half in range(2):
                for j in range(4):
                    c = half * 4 + j
                    self.tr(pT[half][:, j * 128:(j + 1) * 128], zn[:, c * 128:(c + 1) * 128], self.identF[:], [zn, self.identF], [pT[half]])
                for j in range(4):
                    c = half * 4 + j
                    self.act(h2f[:, c, :], pT[half][:, j * 128:(j + 1) * 128], AF.Identity, [pT[half], coef], [h2f],
                             bias=coef[:, 3, s, c:c + 1], scale=coef[:, 2, s, c:c + 1])
            hb = h2b[i2]
            self.cp("pool", hb[:], h2f[:], [h2f], [hb])
            self.dma(self.h2T[tt], hb[:], hb, reads=[hb], writes=[self.h2T])
            for c in range(8):
                self.mm(pR[:, 0:NE], h2f[:, c, :], rw[:, c, :], c == 0, False, [h2f, rw], [pR])
            self.mm(pR[:, 0:NE], self.ones[0:1, 0:128], rb[:], False, True, [self.ones, rb], [pR])
            self.cp("act", lgt[:], pR[:, 0:NE], [pR], [lgt])
            k.op("dve", lambda e: e.max(out=m8[:], in_=lgt[:]), [lgt], [m8])
            self.ts("dve", msk[:], lgt[:], m8[:, 3:4], None, ALU.is_ge, None, [lgt, m8], [msk])
            self.ts("dve", den[:, 0:1], m8[:, 0:1], -1.0, None, ALU.mult, None, [m8], [den])
            self.act(ex[:], lgt[:], AF.Exp, [lgt, den], [ex], bias=den[:, 0:1])
            self.tt("dve", ex[:], ex[:], msk[:], ALU.mult, [ex, msk], [ex])
            k.op("dve", lambda e: e.reduce_sum(out=den[:, 1:2], in_=ex[:], axis=AX.X), [ex], [den])
            self.recip(den[:, 1:2], den[:, 1:2], [den], [den])
            self.ts("dve", G[:, tt, :], ex[:], den[:, 1:2], None, ALU.mult, None, [ex, den], [G])
        k.flush()
        ps.close()

    def moe(self, i, tiles, out_fn, GT=12, SC=4):
        k = self.k
        G = self.G[i]
        groups = [tiles[j:j + GT] for j in range(0, len(tiles), GT)]
        gps = ExitStack()
        bd = k.sbuf("bd", [NE, D], F32, gps)
        self.dma(bd[:], self.b_down[i], bd, writes=[bd])
        for grp in groups:
            ng = len(grp)
            subs = [list(range(j, min(ng, j + SC))) for j in range(0, ng, SC)]
            ps = ExitStack()
            facc = k.sbuf("facc", [128, GT, D], F32, ps)
            gT = k.sbuf("gT", [NE, 128], F32, ps)
            ins = ExitStack()
            Hb = [k.sbuf("H", [128, 8, SC * 128], BF16, ins) for _ in range(2)]
            wu = [k.sbuf("wu", [128, 8, 2 * D], BF16, ins) for _ in range(2)]
            wd = [k.sbuf("wd", [128, 8, D], BF16, ins) for _ in range(2)]
            bu = [k.sbuf("bu", [128, 16], F32, ins) for _ in range(2)]
            actT = k.sbuf("actT", [128, 8, SC * 128], BF16, ins)
            glu = [k.sbuf("glu", [128, SC * 128], F32, ins) for _ in range(2)]
            sig = [k.sbuf("sig", [128, SC * 128], F32, ins) for _ in range(2)]
            l1 = [k.sbuf("l1", [128, SC * 128], F32, ins) for _ in range(2)]
            pU = [k.psum("pU", [128, 2, 512], F32, ins) for _ in range(2)]
            pY = [k.psum("pYm", [128, 1024], F32, ins) for _ in range(2)]
            for j, tt in enumerate(grp):
                py = pY[j % 2]
                self.tr(pU[0][0:NE, 0, 0:128], G[:, tt, :], self.identF[:], [G, self.identF], [pU[0]])
                self.cp("act", gT[:], pU[0][0:NE, 0, 0:128], [pU[0]], [gT])
                for n in range(2):
                    self.mm(py[:, n * 512:(n + 1) * 512], gT[:], bd[:, n * 512:(n + 1) * 512], True, True, [gT, bd], [py])
                self.cp("act", facc[:, j, :], py[:], [py], [facc])
            hit = 0
            for e in range(NE):
                e2 = e % 2
                wu_, wd_, bu_ = wu[e2], wd[e2], bu[e2]
                for hf in range(2):
                    self.dma(wu_[:, :, hf * D:(hf + 1) * D], self.w_up[i, e][:, hf * D:(hf + 1) * D].rearrange("(k p) n -> p k n", p=128),
                             wu_, writes=[wu_], eng="pool")
                self.dma(wd_[:], self.w_down[i, e].rearrange("(k p) n -> p k n", p=128), wd_, writes=[wd_], eng="pool")
                self.dma(bu_[:], self.b_upF[i, e], bu_, writes=[bu_])
                self.ts("pool", bu_[:, 8:16], bu_[:, 8:16], 1.0, None, ALU.add, None, [bu_], [bu_])
                for sub in subs:
                    N = len(sub) * 128
                    H = Hb[hit % 2]
                    hit += 1
                    for jj, j in enumerate(sub):
                        self.dma(H[:, :, jj * 128:(jj + 1) * 128], self.h2T[grp[j]], H, reads=[self.h2T], writes=[H])
                    for m in range(8):
                        m2 = m % 2
                        pu = pU[m2]
                        for c in range(8):
                            self.mm(pu[:, 0, 0:N], wu_[:, c, m * 128:(m + 1) * 128], H[:, c, 0:N], c == 0, c == 7, [wu_, H], [pu])
                        for c in range(8):
                            self.mm(pu[:, 1, 0:N], wu_[:, c, D + m * 128:D + (m + 1) * 128], H[:, c, 0:N], c == 0, c == 7, [wu_, H], [pu])
                        g_, s_, l_ = glu[m2], sig[m2], l1[m2]
                        self.ts("dve", g_[:, 0:N], pu[:, 0, 0:N], bu_[:, m:m + 1], LIMIT, ALU.add, ALU.min, [pu, bu_], [g_])
                        self.act(s_[:, 0:N], g_[:, 0:N], AF.Sigmoid, [g_], [s_], scale=SW_ALPHA)
                        self.ts("dve", l_[:, 0:N], pu[:, 1, 0:N], bu_[:, 8 + m:9 + m], LIMIT + 1.0, ALU.add, ALU.min, [pu, bu_], [l_])
                        self.tt("pool", s_[:, 0:N], s_[:, 0:N], g_[:, 0:N], ALU.mult, [s_, g_], [s_])
                        self.stt("dve", actT[:, m, 0:N], l_[:, 0:N], 1.0 - LIMIT, s_[:, 0:N], ALU.max, ALU.mult, [l_, s_], [actT])
                    for jj, j in enumerate(sub):
                        tt = grp[j]
                        py = pY[j % 2]
                        for n in range(2):
                            for m in range(8):
                                self.mm(py[:, n * 512:(n + 1) * 512], actT[:, m, jj * 128:(jj + 1) * 128], wd_[:, m, n * 512:(n + 1) * 512], m == 0, m == 7,
                                        [actT, wd_], [py])
                        self.stt("dve", facc[:, j, :], py[:], G[:, tt, e:e + 1], facc[:, j, :], ALU.mult, ALU.add, [py, G, facc], [facc])
            k.flush()
            ins.close()
            ln = ExitStack()
            lnB = k.sbuf("lnB2", [128, 2, D], F32, ln)
            for q in range(2):
                self.dma(lnB[:, q, :], self.ln_rows[i][2 + q:3 + q, :].partition_broadcast(128), lnB, writes=[lnB])
            gB = k.sbuf("g5B", [128, 2, D], F32, ln)
            for s in range(2):
                self.dma(gB[:, s, :], self.gate_rows[i, 1, s:s + 1, :].partition_broadcast(128), gB, reads=[self.gate_rows], writes=[gB])
            x1t = [k.sbuf("x1t", [128, D], F32, ln) for _ in range(2)]
            z = k.sbuf("z2", [128, D], F32, ln)
            zn = k.sbuf("zn2", [128, D], F32, ln)
            ot = [k.sbuf("ot", [128, D], F32, ln) for _ in range(2)]
            st = k.sbuf("st2", [128, 12], F32, ln)
            mv = k.sbuf("mv2", [128, 4], F32, ln)
            for j, tt in enumerate(grp):
                s = 1 if tt < self.NTC else 0
                x_ = x1t[j % 2]
                self.dma(x_[:], self.x1[tt * 128:(tt + 1) * 128, :], x_, reads=[self.x1], writes=[x_])
                self.tt("dve", z[:], facc[:, j, :], gB[:, s, :], ALU.mult, [facc, gB], [z])
                self.stt("dve", z[:], x_[:], DN_ALPHA, z[:], ALU.mult, ALU.add, [x_, z], [z])
                self.layernorm(zn, z, st, mv)
                o_ = ot[j % 2]
                self.tt("pool", o_[:], zn[:], lnB[:, 0, :], ALU.mult, [zn, lnB], [o_])
                self.tt("pool", o_[:], o_[:], lnB[:, 1, :], ALU.add, [o_, lnB], [o_])
                dst_t, dst_ap = out_fn(tt)
                self.dma(dst_ap, o_[:], o_, reads=[o_], writes=[dst_t])
            k.flush()
            ln.close()
            ps.close()
        gps.close()

    def l1_passM(self):
        k = self.k
        NTA, NTC, TA = self.NTA, self.NTC, self.TA
        self.stM = ExitStack()
        self.qlatT = k.sbuf("qlatT", [128, 3, TA], BF16, self.stM)
        self.ckvT = k.sbuf("ckvT", [128, 2, TA], BF16, self.stM)
        self.kpeT = k.sbuf("kpeT", [128, TA], BF16, self.stM)
        ps = ExitStack()
        wM = k.sbuf("wM", [128, 8, 704], BF16, ps)
        self.dma(wM[:], self.mla_w_in[:, :].rearrange("(k p) n -> p k n", p=128), wM, writes=[wM], eng="pool")
        gn = k.sbuf("gnM", [128, 640], F32, ps)
        self.dma(gn[:, 0:384], self.mla_qn[0:1, :].partition_broadcast(128), gn, writes=[gn])
        self.dma(gn[:, 384:640], self.mla_kvn[0:1, :].partition_broadcast(128), gn, writes=[gn])
        xt = [k.sbuf("xtM", [128, D], F32, ps) for _ in range(2)]
        hT = [k.sbuf("hTM", [128, 8, 128], BF16, ps) for _ in range(2)]
        tab = [k.sbuf("tabM", [128, 2, 64], F32, ps) for _ in range(2)]
        ss5 = k.sbuf("ss5", [128, 8], F32, ps)
        ssq = k.sbuf("ssq", [128, 2], F32, ps)
        rsd = k.sbuf("rsd", [128, 2], F32, ps)
        xnf = k.sbuf("xnf", [128, 640], F32, ps)
        junk = k.sbuf("junkM", [128, 128], F32, ps)
        xn = [k.sbuf("xnM", [128, 640], BF16, ps) for _ in range(2)]
        kp = k.sbuf("kpM", [128, 64], F32, ps)
        t1 = k.sbuf("t1M", [128, 64], F32, ps)
        t2 = k.sbuf("t2M", [128, 64], F32, ps)
        kb = [k.sbuf("kbM", [128, 128], BF16, ps) for _ in range(2)]
        for _kb in kb:
            self.memset("pool", _kb[:], 0.0, [_kb])
        pT = [k.psum("pTM", [128, 512], F32, ps) for _ in range(2)]
        pM = k.psum("pM", [128, 1024], F32, ps)
        pTb = k.psum("pTbM", [128, 1024], BF16, ps)
        coef = self.coef[1]
        for tt in range(NTA):
            i2 = tt % 2
            s = 1 if tt < NTC else 0
            rows = slice(tt * 128, (tt + 1) * 128)
            x_, h_, tb = xt[i2], hT[i2], tab[i2]
            self.load_hT(self.x2, self.x2[rows, :], x_, pT, h_, coef, s, self.identF)
            self.dma(tb[:], self.ropeM[rows, :, 0:64], tb, writes=[tb])
            for c in range(8):
                self.mm(pM[:, 0:512], h_[:, c, :], wM[:, c, 0:512], c == 0, c == 7, [h_, wM], [pM])
            for c in range(8):
                self.mm(pM[:, 512:704], h_[:, c, :], wM[:, c, 512:704], c == 0, c == 7, [h_, wM], [pM])
            for c in range(5):
                self.act(junk[:, 0:128], pM[:, c * 128:(c + 1) * 128], AF.Square, [pM], [junk, ss5], accum_out=ss5[:, c:c + 1])
            self.tt("dve", ssq[:, 0:1], ss5[:, 0:1], ss5[:, 1:2], ALU.add, [ss5], [ssq])
            self.tt("dve", ssq[:, 0:1], ssq[:, 0:1], ss5[:, 2:3], ALU.add, [ss5, ssq], [ssq])
            self.tt("dve", ssq[:, 1:2], ss5[:, 3:4], ss5[:, 4:5], ALU.add, [ss5], [ssq])
            self.act(rsd[:, 0:1], ssq[:, 0:1], AF.Sqrt, [ssq], [rsd], bias=1e-6, scale=1.0 / 384)
            self.act(rsd[:, 1:2], ssq[:, 1:2], AF.Sqrt, [ssq], [rsd], bias=1e-6, scale=1.0 / 256)
            self.recip(rsd[:, 0:2], rsd[:, 0:2], [rsd], [rsd])
            x2_ = xn[i2]
            self.stt("dve", xnf[:, 0:384], pM[:, 0:384], rsd[:, 0:1], gn[:, 0:384], ALU.mult, ALU.mult, [pM, rsd, gn], [xnf])
            self.stt("dve", xnf[:, 384:640], pM[:, 384:640], rsd[:, 1:2], gn[:, 384:640], ALU.mult, ALU.mult, [pM, rsd, gn], [xnf])
            self.cp("pool", x2_[:], xnf[:], [xnf], [x2_])
            self.cp("act", kp[:], pM[:, 640:704], [pM], [kp])
            self.rope(kb[i2][:, 0:64], kb[i2], kp[:], tb[:, 0, :], tb[:, 1, :], 2, 16, t1, t2, [kp, tb])
            for c in range(5):
                self.tr(pTb[:, c * 128:(c + 1) * 128], x2_[:, c * 128:(c + 1) * 128], self.identB[:], [x2_, self.identB], [pTb])
            self.tr(pTb[:, 640:768], kb[i2][:], self.identB[:], [kb[i2], self.identB], [pTb])
            self.cp("act", self.qlatT[:, :, rows], pTb[:, 0:384].rearrange("p (c t) -> p c t", c=3), [pTb], [self.qlatT])
            self.cp("act", self.ckvT[:, :, rows], pTb[:, 384:640].rearrange("p (c t) -> p c t", c=2), [pTb], [self.ckvT])
            self.cp("act", self.kpeT[:, rows], pTb[:, 640:768], [pTb], [self.kpeT])
        k.flush()
        ps.close()

    def l1_attn(self):
        k = self.k
        NTA, NTC, TA, TC, TL = self.NTA, self.NTC, self.TA, self.TC, self.TL
        ps = ExitStack()
        wuq = k.sbuf("wuq", [128, 3, 1536], BF16, ps)
        self.dma(wuq[:], self.mla_w_uq[:, :].rearrange("(k p) n -> p k n", p=128), wuq, writes=[wuq], eng="pool")
        wukv = k.sbuf("wukv", [128, 2, 2048], BF16, ps)
        self.dma(wukv[:], self.mla_w_ukv[:, :].rearrange("(k p) n -> p k n", p=128), wukv, writes=[wukv], eng="pool")
        wuqr = k.sbuf("wuqr", [128, 3, 8, 128], BF16, ps)
        self.memset("pool", wuqr[:], 0.0, [wuqr])
        for c in range(3):
            self.dma(wuqr[:, c, :, 0:64], self.mla_w_uq[c * 128:(c + 1) * 128, :].rearrange("p (h c) -> p h c", c=192)[:, :, 128:192],
                     wuqr, writes=[wuqr], eng="pool")
        pmat = k.sbuf("pmat", [128, 128], F32, ps)
        self.dma(pmat[:], self.pmat_in[:, :], pmat, writes=[pmat])
        CT = k.sbuf("CTm", [128, 2, TA], F32, ps)
        self.dma(CT[:], self.ropeMT[:, :, :], CT, writes=[CT])
        KnT = k.sbuf("KnT", [128, TA], BF16, ps)
        Vh = k.sbuf("Vh", [128, NTA, 130], BF16, ps)
        QnT = k.sbuf("QnT", [128, TA], BF16, ps)
        QrT = k.sbuf("QrT", [128, TA], BF16, ps)
        raw = k.sbuf("rawq", [128, 512], F32, ps)
        u1 = k.sbuf("u1", [128, 512], F32, ps)
        u2 = k.sbuf("u2", [128, 512], F32, ps)
        self.memset("pool", Vh[:], 1.0, [Vh])
        pP = [k.psum("pP", [128, 512], F32, ps) for _ in range(2)]
        bufs = self.attn_bufs(ps)
        qscale = 192.0 ** -0.5
        chunks = lambda a, b_: [(q, min(512, b_ - q)) for q in range(a, b_, 512)]
        for h in range(8):
            kc, vc, qc, rc = h * 256, h * 256 + 128, h * 192, h * 192 + 128
            for ci, (t0, n) in enumerate(chunks(0, TA)):
                p_ = pP[ci % 2]
                for c in range(2):
                    self.mm(p_[:, 0:n], wukv[:, c, kc:kc + 128], self.ckvT[:, c, t0:t0 + n], c == 0, c == 1, [wukv, self.ckvT], [p_])
                self.cp("act", KnT[:, t0:t0 + n], p_[:, 0:n], [p_], [KnT])
            for tt in range(NTA):
                p_ = pP[tt % 2]
                for c in range(2):
                    self.mm(p_[:, 0:128], self.ckvT[:, c, tt * 128:(tt + 1) * 128], wukv[:, c, vc:vc + 128], c == 0, c == 1, [wukv, self.ckvT], [p_])
                self.cp("dve", Vh[:, tt, 0:128], p_[:, 0:128], [p_], [Vh])
            for ci, (t0, n) in enumerate(chunks(TC, TA)):
                p_ = pP[ci % 2]
                for c in range(3):
                    self.mm(p_[:, 0:n], wuq[:, c, qc:qc + 128], self.qlatT[:, c, t0:t0 + n], c == 0, c == 2, [wuq, self.qlatT], [p_])
                self.act(QnT[:, t0:t0 + n], p_[:, 0:n], AF.Copy, [p_], [QnT], scale=qscale)
                p2 = pP[(ci + 1) % 2]
                for c in range(3):
                    self.mm(p2[:, 0:n], wuqr[:, c, h, :], self.qlatT[:, c, t0:t0 + n], c == 0, c == 2, [wuqr, self.qlatT], [p2])
                self.act(raw[:, 0:n], p2[:, 0:n], AF.Copy, [p2], [raw], scale=qscale)
                self.mm(p2[:, 0:n], pmat[:], raw[:, 0:n], True, True, [pmat, raw], [p2])
                self.tt("dve", u1[:, 0:n], raw[:, 0:n], CT[:, 0, t0:t0 + n], ALU.mult, [raw, CT], [u1])
                self.tt("dve", u2[:, 0:n], p2[:, 0:n], CT[:, 1, t0:t0 + n], ALU.mult, [p2, CT], [u2])
                self.tt("dve", QrT[:, t0:t0 + n], u1[:, 0:n], u2[:, 0:n], ALU.add, [u1, u2], [QrT])
            parts = [(lambda q0, n: QnT[:, q0:q0 + n], lambda kt: KnT[:, kt * 128:(kt + 1) * 128], QnT, KnT),
                     (lambda q0, n: QrT[:, q0:q0 + n], lambda kt: self.kpeT[:, kt * 128:(kt + 1) * 128], QrT, self.kpeT)]
            V = lambda kt: Vh[:, kt, 0:129]
            self.attention(parts, V, Vh, self.q_chunks(TC, TA, list(range(NTA))), h * 128, bufs)
        k.flush()
        ps.close()
        self.stM.close()

    def build(self):
        self.declare()
        self.pmat_in = self.inp("pmat", [128, 128])
        self.ropeMT = self.inp("ropeMT", [128, 2, self.TA])
        self.consts()
        self.phase0()
        allt = list(range(self.NTA))
        latt = list(range(self.NTC, self.NTA))
        self.l0_passA(); self.l0_attnA(); self.l0_passB(); self.l0_retention()
        self.post_mixer(0, self.ab_w_out, allt)
        self.moe(0, allt, lambda tt: (self.x2, self.x2[tt * 128:(tt + 1) * 128, :]), GT=self.GT, SC=self.SC)
        if self.stop == "l0":
            return self.k.emit()
        self.l1_passM(); self.l1_attn()
        self.post_mixer(1, self.mla_w_out, latt)
        self.moe(1, latt, lambda tt: (self.y_out, self.y_out[(tt - self.NTC) * 128:(tt - self.NTC + 1) * 128, :]), GT=self.GT, SC=self.SC)
        return self.k.emit()


_TL, _TC, _NB = 4096, 256, 8


def kernel(**inputs):
    maps = prep_inputs(inputs, _TL, _TC, _NB)
    b = B2(_TL, _TC)
    nc = b.build()
    res = run_bass_kernel_spmd(nc, maps, core_ids=list(range(_NB)))
    return np.stack([np.asarray(r["y"], dtype=np.float32) for r in res.results], 0)
```

```python
import numpy as np
import ml_dtypes
from contextlib import ExitStack
import concourse.bass as bass
import concourse.mybir as mybir
from concourse.bass_utils import run_bass_kernel_spmd

F32 = mybir.dt.float32
BF16 = mybir.dt.bfloat16
I32 = mybir.dt.int32
AF = mybir.ActivationFunctionType
ALU = mybir.AluOpType
AX = mybir.AxisListType

D = 1024
GRID_W = 64
THETA = 10000.0
NE = 32
LIMIT = 7.0
SW_ALPHA = 1.702
DN_ALPHA = 4.0 ** 0.25


class SemSlot:
    __slots__ = ("sem", "count")

    def __init__(self):
        self.sem = None
        self.count = 0


class Res:
    __slots__ = ("name", "slot", "lastw", "readers", "lastdma")

    def __init__(self, name):
        self.name = name
        self.slot = {}
        self.lastw = None
        self.readers = []
        self.lastdma = None


class Op:
    __slots__ = ("eng", "fn", "deps", "marked", "event", "is_dma", "ei", "done")

    def __init__(self, eng, fn):
        self.eng = eng
        self.fn = fn
        self.deps = []
        self.marked = False
        self.event = None
        self.is_dma = False
        self.done = False


class T:
    __slots__ = ("ap", "r")

    def __init__(self, ap, r):
        self.ap = ap
        self.r = r

    def __getitem__(self, idx):
        return self.ap[idx]


def _rs(xs):
    return [x.r if isinstance(x, T) else x for x in xs]


class K:
    ENGS = ("pe", "act", "dve", "pool", "sp")

    def __init__(self):
        self.nc = bass.Bass("TRN2", target_bir_lowering=False)
        self.ops = []
        self.stack = ExitStack()
        self.last_on = {e: None for e in self.ENGS}
        self.owners = []
        self.free_slots = {"hw": [], "sw": []}
        self.n = 0
        self.esem = {e: self.stack.enter_context(self.nc.semaphore(f"s_{e}")) for e in self.ENGS}
        self.cnt = {e: 0 for e in self.ENGS}
        self.waited = {e: {} for e in self.ENGS}
        self.stats = dict(nops=0, nwait=0, ndrain=0, nsem=5)

    def res(self, name=None):
        self.n += 1
        return Res(f"{name or 'r'}{self.n}")

    def sbuf(self, name, shape, dt, stack=None):
        self.n += 1
        ap = (stack or self.stack).enter_context(self.nc.sbuf_tensor(f"{name}_{self.n}", list(shape), dt))
        return T(ap, self.res(name))

    def psum(self, name, shape, dt, stack=None):
        self.n += 1
        ap = (stack or self.stack).enter_context(self.nc.psum_tensor(f"{name}_{self.n}", list(shape), dt))
        return T(ap, self.res(name))

    def dram(self, name, shape, dt, kind="Internal"):
        return T(self.nc.dram_tensor(name, list(shape), dt, kind=kind).ap(), self.res(name))

    def _track(self, op, reads, writes):
        deps = op.deps
        for r in reads:
            if r.lastw is not None:
                deps.append(r.lastw)
            r.readers.append(op)
        for w in writes:
            if w.lastw is not None:
                deps.append(w.lastw)
            deps.extend(w.readers)
            w.lastw = op
            w.readers = []
        seen = set()
        out = []
        for d in deps:
            if d is op or d.done or id(d) in seen:
                continue
            seen.add(id(d))
            if d.eng == "pe" and op.eng == "pe" and not d.is_dma and not op.is_dma:
                continue
            out.append(d)
            if d.eng != op.eng or d.is_dma:
                d.marked = True
        op.deps = out
        self.ops.append(op)
        self.last_on[op.eng] = op

    def op(self, eng, fn, reads=(), writes=()):
        o = Op(eng, fn)
        self._track(o, _rs(reads), _rs(writes))
        return o

    def dma(self, eng, out, in_, owner, reads=(), writes=(), **kw):
        o = Op(eng, lambda e: e.dma_start(out=out, in_=in_, **kw))
        self._dma_common(o, owner, reads, writes)
        return o

    def dma_fn(self, eng, fn, owner, reads=(), writes=()):
        o = Op(eng, fn)
        self._dma_common(o, owner, reads, writes)
        return o

    def _dma_common(self, o, owner, reads, writes):
        owner = owner.r if isinstance(owner, T) else owner
        o.is_dma = True
        o.marked = True
        kind = "sw" if o.eng == "pool" else "hw"
        if kind not in owner.slot:
            fl = self.free_slots[kind]
            owner.slot[kind] = fl.pop() if fl else SemSlot()
            if owner not in self.owners:
                self.owners.append(owner)
        if owner.lastdma is not None and not owner.lastdma.done:
            o.deps.append(owner.lastdma)
        owner.lastdma = o
        sl = owner.slot[kind]
        sl.count += 16
        o.event = (sl, sl.count)
        self._track(o, _rs(reads), _rs(writes))

    def barrier(self):
        lasts = [self.last_on[e] for e in self.ENGS if self.last_on[e] is not None and not self.last_on[e].done]
        lasts += [o.lastdma for o in self.owners if o.lastdma is not None and not o.lastdma.done]
        for e in self.ENGS:
            o = Op(e, None)
            for d in lasts:
                if d.eng == e and not d.is_dma:
                    continue
                if d.fn is None and not d.is_dma:
                    continue
                o.deps.append(d)
                d.marked = True
            self.ops.append(o)
            self.last_on[e] = o

    def flush(self):
        self.barrier()
        nc = self.nc
        esem = self.esem
        for r in self.owners:
            for sl in r.slot.values():
                if sl.sem is None:
                    self.n += 1
                    sl.sem = self.stack.enter_context(nc.semaphore(f"d{self.n}"))
                    self.stats["nsem"] += 1
        for o in self.ops:
            if o.is_dma:
                o.event = (o.event[0].sem, o.event[1])
            elif o.marked:
                self.cnt[o.eng] += 1
                o.event = (esem[o.eng], self.cnt[o.eng])
        by_eng = {e: [o for o in self.ops if o.eng == e] for e in self.ENGS}
        for e in self.ENGS:
            for i, o in enumerate(by_eng[e]):
                o.ei = i

        def run(ename, eng):
            w = self.waited[ename]
            last_drain = -1
            for o in by_eng[ename]:
                need = {}
                drain = False
                for d in o.deps:
                    if d.eng == ename and not d.is_dma:
                        if d.fn is not None and d.ei > last_drain and o.ei - d.ei <= 8:
                            drain = True
                        continue
                    sem, val = d.event
                    key = id(sem)
                    if w.get(key, 0) >= val:
                        continue
                    if key not in need or need[key][1] < val:
                        need[key] = (sem, val)
                for key, (sem, val) in need.items():
                    eng.wait_ge(sem, val)
                    w[key] = val
                    self.stats["nwait"] += 1
                if drain or (o.fn is None and ename != "pe"):
                    eng.drain()
                    last_drain = o.ei - 1
                    self.stats["ndrain"] += 1
                if o.fn is None:
                    continue
                ins = o.fn(eng)
                if o.is_dma:
                    ins.then_inc(o.event[0], 16)
                elif o.marked:
                    ins.then_inc(esem[o.eng], 1)

        with nc.Block() as block:
            block.tensor(lambda e: run("pe", e))
            block.scalar(lambda e: run("act", e))
            block.vector(lambda e: run("dve", e))
            block.gpsimd(lambda e: run("pool", e))
            block.sync(lambda e: run("sp", e))
        self.stats["nops"] += len(self.ops)
        for o in self.ops:
            o.done = True
        self.ops = []
        for r in self.owners:
            for kind, sl in r.slot.items():
                self.free_slots[kind].append(sl)
            r.slot = {}
        self.owners = []

    def emit(self):
        self.flush()
        self.stack.close()
        return self.nc


def _rope_tables(pos, d):
    half = d // 2
    inv = (THETA ** (-np.arange(half, dtype=np.float32) / np.float32(half))).astype(np.float32)
    ang = pos.astype(np.float32)[:, None] * inv[None, :]
    c, s = np.cos(ang).astype(np.float32), np.sin(ang).astype(np.float32)
    return np.concatenate([c, c], 1), np.concatenate([-s, s], 1)


def _axial_tables(n_tok, d):
    rows = np.repeat(np.arange(n_tok // GRID_W), GRID_W)
    cols = np.tile(np.arange(GRID_W), n_tok // GRID_W)
    c1, s1 = _rope_tables(rows, d // 2)
    c2, s2 = _rope_tables(cols, d // 2)
    return np.concatenate([c1, c2], 1), np.concatenate([s1, s2], 1)


def make_tables(TL, TC):
    TA = TL + TC
    ca, sa = _axial_tables(TL, 128)
    CA = np.concatenate([np.ones((TC, 128), np.float32), ca], 0)
    SA = np.concatenate([np.zeros((TC, 128), np.float32), sa], 0)
    cb, sb = _rope_tables(np.arange(TA), 128)
    cm, sm = _axial_tables(TL, 64)
    CM = np.concatenate([np.ones((TC, 64), np.float32), cm], 0)
    SM = np.concatenate([np.zeros((TC, 64), np.float32), sm], 0)
    pm = np.zeros((128, 128), np.float32)
    for m in range(64):
        src = m + 16 if (m % 32) < 16 else m - 16
        pm[src, m] = 1.0
    rmt = np.zeros((128, 2, TA), np.float32)
    rmt[:64, 0] = CM.T
    rmt[:64, 1] = SM.T
    return dict(pmat=pm, ropeMT=rmt,
                ropeA=np.stack([CA, SA], 1).astype(np.float32),
                ropeB=np.stack([np.tile(cb, (1, 4)), np.tile(sb, (1, 4))], 1).astype(np.float32),
                ropeM=np.stack([np.tile(CM, (1, 8)), np.tile(SM, (1, 8))], 1).astype(np.float32))


class B:
    def __init__(self, TL, TC, debug=False, stop=None):
        self.k = K()
        self.TL, self.TC, self.TA = TL, TC, TL + TC
        self.NTL, self.NTC, self.NTA = TL // 128, TC // 128, (TL + TC) // 128
        self.debug = debug
        self.stop = stop
        self.GT = 12
        self.SC = 4

    def mm(self, out, lhsT, rhs, start, stop, reads, writes):
        self.k.op("pe", lambda e: e.matmul(out=out, lhsT=lhsT, rhs=rhs, start=start, stop=stop), reads, writes)

    def tr(self, out, in_, ident, reads, writes):
        self.k.op("pe", lambda e: e.transpose(out=out, in_=in_, identity=ident), reads, writes)

    def act(self, out, in_, func, reads, writes, bias=0.0, scale=1.0, accum_out=None):
        if accum_out is None:
            self.k.op("act", lambda e: e.activation(out=out, in_=in_, func=func, bias=bias, scale=scale), reads, writes)
        else:
            self.k.op("act", lambda e: e.activation(out=out, in_=in_, func=func, bias=bias, scale=scale, accum_out=accum_out), reads, writes)

    def ts(self, eng, out, in0, s1, s2, op0, op1, reads, writes):
        if s2 is None:
            self.k.op(eng, lambda e: e.tensor_scalar(out=out, in0=in0, scalar1=s1, scalar2=None, op0=op0), reads, writes)
        else:
            self.k.op(eng, lambda e: e.tensor_scalar(out=out, in0=in0, scalar1=s1, scalar2=s2, op0=op0, op1=op1), reads, writes)

    def tt(self, eng, out, in0, in1, op, reads, writes):
        self.k.op(eng, lambda e: e.tensor_tensor(out=out, in0=in0, in1=in1, op=op), reads, writes)

    def stt(self, eng, out, in0, scalar, in1, op0, op1, reads, writes):
        self.k.op(eng, lambda e: e.scalar_tensor_tensor(out=out, in0=in0, scalar=scalar, in1=in1, op0=op0, op1=op1), reads, writes)

    def cp(self, eng, out, in_, reads, writes):
        if eng == "act":
            self.k.op("act", lambda e: e.copy(out=out, in_=in_), reads, writes)
        else:
            self.k.op(eng, lambda e: e.tensor_copy(out=out, in_=in_), reads, writes)

    def memset(self, eng, ap, val, writes):
        self.k.op(eng, lambda e: e.memset(ap, val), (), writes)

    def recip(self, out, in_, reads, writes):
        self.k.op("dve", lambda e: e.reciprocal(out=out, in_=in_), reads, writes)

    def dma(self, out, in_, owner, reads=(), writes=(), eng="sp", **kw):
        self.k.dma(eng, out, in_, owner, reads, writes, **kw)

    def scratch(self, name, shape, dt):
        return self.k.dram(name, shape, dt, kind="ExternalOutput" if self.debug else "Internal")

    def inp(self, name, shape, dt=F32):
        return self.k.dram(name, shape, dt, kind="ExternalInput")

    def declare(self):
        TL, TC, TA = self.TL, self.TC, self.TA
        i = self.inp
        self.x_in = i("x", [TL, D]); self.ctx_in = i("ctx", [TC, D])
        self.cv2 = i("cv2", [128, 8, 2])
        self.ada_w = i("ada_w", [2, D, 6 * D]); self.ada_bF = i("ada_bF", [2, 128, 48]); self.ada_b = i("ada_b", [2, 1, 6 * D])
        self.lnF = i("lnF", [2, 128, 4, 8]); self.ln_rows = i("ln_rows", [2, 4, D])
        self.ab_w_in = i("ab_w_in", [D, 3072]); self.ab_qk = i("ab_qk", [2, 128]); self.ab_ld = i("ab_ld", [1, 8])
        self.ab_gn = i("ab_gn", [1, 512]); self.ab_w_out = i("ab_w_out", [D, D])
        self.mla_w_in = i("mla_w_in", [D, 704]); self.mla_qn = i("mla_qn", [1, 384]); self.mla_kvn = i("mla_kvn", [1, 256])
        self.mla_w_uq = i("mla_w_uq", [384, 1536]); self.mla_w_ukv = i("mla_w_ukv", [256, 2048]); self.mla_w_out = i("mla_w_out", [D, D])
        self.r_w = i("r_w", [2, D, NE]); self.r_b = i("r_b", [2, 1, NE])
        ne = getattr(self, "ne_decl", NE)
        self.w_up = i("w_up", [2, ne, D, 2 * D]); self.b_upF = i("b_upF", [2, NE, 128, 16])
        self.w_down = i("w_down", [2, ne, D, D]); self.b_down = i("b_down", [2, NE, D])
        self.identF_in = i("identF", [128, 128])
        self.ropeA = i("ropeA", [TA, 2, 128]); self.ropeB = i("ropeB", [TA, 2, 512]); self.ropeM = i("ropeM", [TA, 2, 512])
        self.y_out = self.k.dram("y", [TL, D], F32, kind="ExternalOutput")
        s = self.scratch
        self.hT0 = s("hT0", [self.NTA, 128, 8, 128], BF16)
        self.cat = s("cat", [TA, D], BF16)
        self.x1 = s("x1", [TA, D], F32)
        self.h2T = s("h2T", [self.NTA, 128, 8, 128], BF16)
        self.x2 = s("x2", [TA, D], F32)
        self.qTB = s("qTB", [4, 128, TA], BF16); self.kTB = s("kTB", [4, 128, TA], BF16)
        self.kB = s("kB", [TA, 512], BF16); self.vB = s("vB", [TA, 512], BF16); self.gB = s("gB", [TA, 512], BF16)

    def consts(self):
        k = self.k
        self.identF = k.sbuf("identF", [128, 128], F32)
        self.dma(self.identF[:], self.identF_in[:, :], self.identF, writes=[self.identF])
        self.identB = k.sbuf("identB", [128, 128], BF16)
        self.cp("dve", self.identB[:], self.identF[:], [self.identF], [self.identB])
        self.ones = k.sbuf("ones", [128, 512], F32)
        self.memset("pool", self.ones[:], 1.0, [self.ones])

    def xsrc(self, tt):
        if tt < self.NTC:
            return self.ctx_in, self.ctx_in[tt * 128:(tt + 1) * 128, :]
        t = tt - self.NTC
        return self.x_in, self.x_in[t * 128:(t + 1) * 128, :]

    def phase0(self):
        k = self.k
        P_modF = [k.sbuf("modF", [128, 48, 2], F32) for _ in range(2)]
        self.G = [k.sbuf("G", [128, self.NTA, NE], F32) for _ in range(2)]
        self.gate_rows = self.scratch("gate_rows", [2, 2, 2, D], F32)
        P_coef = [k.sbuf("coef", [128, 4, 2, 8], F32) for _ in range(2)]
        P_lnF = [k.sbuf("lnF", [128, 4, 8], F32) for _ in range(2)]
        ps = ExitStack()
        cv = k.sbuf("cv", [128, 8, 2], F32, ps)
        self.dma(cv[:], self.cv2[:, :, :], cv, writes=[cv])
        sc2 = k.sbuf("sc2", [128, 8, 2], F32, ps)
        self.act(sc2[:], cv[:], AF.Silu, [cv], [sc2])
        scB = k.sbuf("scB", [128, 8, 2, 128], F32, ps)
        for kk in range(8):
            for s in range(2):
                self.act(scB[:, kk, s, :], self.ones[:, 0:128], AF.Copy, [self.ones, sc2], [scB], scale=sc2[:, kk, s:s + 1])
        wch = [k.sbuf("wch", [128, 8, 512], F32, ps) for _ in range(2)]
        brow = [k.sbuf("brow", [1, 512], F32, ps) for _ in range(2)]
        grow = [k.sbuf("grow", [1, 512], F32, ps) for _ in range(2)]
        abFs = [k.sbuf("abF", [128, 48], F32, ps) for _ in range(2)]
        tmp = k.sbuf("ctmp", [128, 2, 8], F32, ps)
        psm = k.psum("psm", [128, 512], F32, ps)
        psg = [k.psum("psg", [128, 512], F32, ps) for _ in range(2)]
        self.modF, self.coef = [], []
        for i in range(2):
            modF, coef, lnF = P_modF[i], P_coef[i], P_lnF[i]
            abF = abFs[i]
            self.dma(abF[:], self.ada_bF[i], abF, writes=[abF])
            for n in range(12):
                w = wch[n % 2]
                self.dma(w[:], self.ada_w[i][:, n * 512:(n + 1) * 512].rearrange("(k p) n -> p k n", p=128), w, writes=[w])
                for m in range(4):
                    mc = n * 4 + m
                    for kk in range(8):
                        self.mm(psm[:, mc * 2:mc * 2 + 2], w[:, kk, m * 128:(m + 1) * 128], sc2[:, kk, :], kk == 0, kk == 7, [w, sc2], [psm])
                j = n // 2
                if j in (2, 5):
                    br = brow[n % 2]
                    self.dma(br[:], self.ada_b[i][:, n * 512:(n + 1) * 512], br, writes=[br])
                    for s in range(2):
                        for kk in range(8):
                            self.mm(psg[s][:], scB[:, kk, s, :], w[:, kk, :], kk == 0, False, [w, scB], [psg[s]])
                        self.mm(psg[s][:], self.ones[0:1, 0:128], br[:], False, True, [self.ones, br], [psg[s]])
                        gr = grow[(n + s) % 2]
                        self.cp("act", gr[:], psg[s][0:1, :], [psg[s]], [gr])
                        self.dma(self.gate_rows[i, 0 if j == 2 else 1, s:s + 1, (n % 2) * 512:(n % 2 + 1) * 512], gr[:], gr, reads=[gr], writes=[self.gate_rows])
            for s in range(2):
                self.tt("dve", modF[:, :, s], psm[:, s:96:2], abF[:], ALU.add, [psm, abF], [modF])
            self.dma(lnF[:], self.lnF[i], lnF, writes=[lnF])
            for s in range(2):
                self.ts("dve", coef[:, 0, s, :], modF[:, 8:16, s], 1.0, None, ALU.add, None, [modF], [coef])
                self.cp("dve", coef[:, 1, s, :], modF[:, 0:8, s], [modF], [coef])
                self.ts("dve", tmp[:, s, :], modF[:, 32:40, s], 1.0, None, ALU.add, None, [modF], [tmp])
                self.tt("dve", coef[:, 2, s, :], tmp[:, s, :], lnF[:, 0, :], ALU.mult, [tmp, lnF], [coef])
                self.tt("dve", coef[:, 3, s, :], tmp[:, s, :], lnF[:, 1, :], ALU.mult, [tmp, lnF], [coef])
                self.tt("dve", coef[:, 3, s, :], coef[:, 3, s, :], modF[:, 24:32, s], ALU.add, [coef, modF], [coef])
            self.modF.append(modF); self.coef.append(coef)
        k.flush()
        ps.close()

    def dbg(self, name, t, shape, dt=F32):
        if not self.debug:
            return
        d = self.k.dram("dbg_" + name, shape, dt, kind="ExternalOutput")
        self.dma(d.ap, t[:], t, reads=[t], writes=[d])


def _fm(v):
    return np.ascontiguousarray(np.swapaxes(v.reshape(v.shape[:-1] + (8, 128)), -1, -2))


def prep_inputs(inp, TL, TC, nb):
    f = lambda a: np.ascontiguousarray(np.asarray(a, dtype=np.float32))
    tabs = make_tables(TL, TC)
    shared = dict(
        ada_w=f(inp["ada_w"]),
        ada_bF=np.ascontiguousarray(np.transpose(f(inp["ada_b"]).reshape(2, 48, 128), (0, 2, 1))),
        ada_b=f(inp["ada_b"]).reshape(2, 1, 6 * D),
        lnF=np.ascontiguousarray(np.stack([_fm(f(inp[n])) for n in ("ln1_g", "ln1_b", "ln2_g", "ln2_b")], 2)),
        ln_rows=np.ascontiguousarray(np.stack([f(inp[n]) for n in ("ln1_g", "ln1_b", "ln2_g", "ln2_b")], 1)),
        ab_w_in=f(inp["ab_w_in"])[0], ab_qk=np.concatenate([f(inp["ab_q_norm"]), f(inp["ab_k_norm"])], 0),
        ab_ld=f(inp["ab_log_decay"]).reshape(1, 8), ab_gn=f(inp["ab_gn_g"]).reshape(1, 512), ab_w_out=f(inp["ab_w_out"])[0],
        mla_w_in=f(inp["mla_w_in"])[0], mla_qn=f(inp["mla_q_norm"]).reshape(1, 384), mla_kvn=f(inp["mla_kv_norm"]).reshape(1, 256),
        mla_w_uq=f(inp["mla_w_uq"])[0], mla_w_ukv=f(inp["mla_w_ukv"])[0], mla_w_out=f(inp["mla_w_out"])[0],
        r_w=f(inp["moe_router_w"]), r_b=f(inp["moe_router_b"]).reshape(2, 1, NE),
        w_up=f(inp["moe_w_up"]),
        b_upF=np.ascontiguousarray(np.transpose(f(inp["moe_b_up"]).reshape(2, NE, 16, 128), (0, 1, 3, 2))),
        w_down=f(inp["moe_w_down"]), b_down=f(inp["moe_b_down"]),
        identF=np.eye(128, dtype=np.float32), **tabs)
    x, c, ctx, c_ctx = f(inp["x"]), f(inp["c"]), f(inp["ctx"]), f(inp["c_ctx"])
    maps = []
    for b in range(nb):
        cv2 = np.ascontiguousarray(np.stack([_fm(c[b]), _fm(c_ctx)], -1))
        maps.append(dict(x=x[b], ctx=ctx[b], cv2=cv2, **shared))
    return maps


class B2(B):
    def load_hT(self, src_t, src_ap, xt, pT, hT, coef, s, ident, a_idx=0):
        self.dma(xt[:], src_ap, xt, reads=[src_t], writes=[xt])
        for half in range(2):
            for j in range(4):
                c = half * 4 + j
                self.tr(pT[half][:, j * 128:(j + 1) * 128], xt[:, c * 128:(c + 1) * 128], ident[:], [xt, ident], [pT[half]])
            for j in range(4):
                c = half * 4 + j
                self.act(hT[:, c, :], pT[half][:, j * 128:(j + 1) * 128], AF.Identity, [pT[half], coef], [hT],
                         bias=coef[:, a_idx + 1, s, c:c + 1], scale=coef[:, a_idx, s, c:c + 1])

    def rope(self, out, out_t, xin, C, S, nblk, h, tmp1, tmp2, reads):
        n = nblk * 2 * h
        v = lambda ap: ap.rearrange("p (b two h) -> p b two h", two=2, h=h)
        self.tt("dve", tmp1[:, 0:n], xin, C, ALU.mult, reads, [tmp1])
        self.tt("pool", v(tmp2[:, 0:n])[:, :, 0, :], v(xin)[:, :, 1, :], v(S)[:, :, 0, :], ALU.mult, reads, [tmp2])
        self.tt("pool", v(tmp2[:, 0:n])[:, :, 1, :], v(xin)[:, :, 0, :], v(S)[:, :, 1, :], ALU.mult, reads, [tmp2])
        self.tt("dve", out, tmp1[:, 0:n], tmp2[:, 0:n], ALU.add, [tmp1, tmp2], [out_t])

    def l0_passA(self):
        k = self.k
        NTA, TA = self.NTA, self.TA
        self.stA = ExitStack()
        self.QT = k.sbuf("QT", [128, 4, TA], BF16, self.stA)
        self.KT = k.sbuf("KT", [128, 2, TA], BF16, self.stA)
        self.VA = k.sbuf("VA", [128, NTA, 2, 130], BF16, self.stA)
        ps = ExitStack()
        wA = k.sbuf("wA", [128, 8, 1024], BF16, ps)
        self.dma(wA[:], self.ab_w_in[:, 0:1024].rearrange("(k p) n -> p k n", p=128), wA, writes=[wA], eng="pool")
        gq = k.sbuf("gq", [128, 2, 128], F32, ps)
        self.dma(gq[:, 0, :], self.ab_qk[0:1, :].partition_broadcast(128), gq, writes=[gq])
        self.dma(gq[:, 1, :], self.ab_qk[1:2, :].partition_broadcast(128), gq, writes=[gq])
        self.k.op("act", lambda e: e.mul(out=gq[:, 0, :], in_=gq[:, 0, :], mul=128.0 ** -0.5), [gq], [gq])
        self.memset("pool", self.VA[:], 1.0, [self.VA])
        xt = [k.sbuf("xt", [128, D], F32, ps) for _ in range(2)]
        hT = [k.sbuf("hT", [128, 8, 128], BF16, ps) for _ in range(2)]
        tab = [k.sbuf("tabA", [128, 2, 128], F32, ps) for _ in range(2)]
        ss = k.sbuf("ss", [128, 8], F32, ps)
        rstd = k.sbuf("rstd", [128, 8], F32, ps)
        junk = k.sbuf("junk", [128, 128], F32, ps)
        xn = [k.sbuf("xn", [128, 128], F32, ps) for _ in range(2)]
        t1 = [k.sbuf("t1", [128, 128], F32, ps) for _ in range(2)]
        t2 = [k.sbuf("t2", [128, 128], F32, ps) for _ in range(2)]
        ob = [k.sbuf("ob", [128, 128], BF16, ps) for _ in range(2)]
        pT = [k.psum("pT", [128, 512], F32, ps) for _ in range(2)]
        pA = [k.psum("pA", [128, 1024], F32, ps) for _ in range(2)]
        pTb = k.psum("pTb", [128, 1024], BF16, ps)
        coef = self.coef[0]
        for tt in range(NTA):
            s = 1 if tt < self.NTC else 0
            src_t, src_ap = self.xsrc(tt)
            x_, h_, tb, pa = xt[tt % 2], hT[tt % 2], tab[tt % 2], pA[tt % 2]
            self.load_hT(src_t, src_ap, x_, pT, h_, coef, s, self.identF)
            self.dma(self.hT0[tt], h_[:], h_, reads=[h_], writes=[self.hT0])
            self.dma(tb[:], self.ropeA[tt * 128:(tt + 1) * 128], tb, writes=[tb])
            for n in range(2):
                for c in range(8):
                    self.mm(pa[:, n * 512:(n + 1) * 512], h_[:, c, :], wA[:, c, n * 512:(n + 1) * 512], c == 0, c == 7, [h_, wA], [pa])
            for hh in range(6):
                self.act(junk[:], pa[:, hh * 128:(hh + 1) * 128], AF.Square, [pa], [junk, ss], accum_out=ss[:, hh:hh + 1])
            self.act(rstd[:, 0:6], ss[:, 0:6], AF.Sqrt, [ss], [rstd], bias=1e-6, scale=1.0 / 128)
            self.recip(rstd[:, 0:6], rstd[:, 0:6], [rstd], [rstd])
            for hh in range(6):
                i2 = hh % 2
                g = gq[:, 0, :] if hh < 4 else gq[:, 1, :]
                self.stt("dve", xn[i2][:], pa[:, hh * 128:(hh + 1) * 128], rstd[:, hh:hh + 1], g, ALU.mult, ALU.mult, [pa, rstd, gq], [xn[i2]])
                self.rope(ob[i2][:], ob[i2], xn[i2][:], tb[:, 0, :], tb[:, 1, :], 2, 32, t1[i2], t2[i2], [xn[i2], tb])
                self.tr(pTb[:, hh * 128:(hh + 1) * 128], ob[i2][:], self.identB[:], [ob[i2], self.identB], [pTb])
            self.cp("act", self.QT[:, :, tt * 128:(tt + 1) * 128], pTb[:, 0:512].rearrange("p (h t) -> p h t", h=4), [pTb], [self.QT])
            self.cp("act", self.KT[:, :, tt * 128:(tt + 1) * 128], pTb[:, 512:768].rearrange("p (h t) -> p h t", h=2), [pTb], [self.KT])
            self.cp("dve", self.VA[:, tt, :, 0:128], pa[:, 768:1024].rearrange("p (h d) -> p h d", h=2), [pa], [self.VA])
        k.flush()
        ps.close()

    def attention(self, parts, V, vres, q_ranges, cat_col, bufs, exp_bias=None):
        pS, pO, PT, osb, rec = bufs
        np_ = len(parts)
        it = 0
        for (q0, nq, ktiles) in q_ranges:
            nqb = nq // 128
            nk = len(ktiles)
            pts = []

            def emit_S(ki, it_):
                sp = pS[it_ % 2]
                pt = PT[it_ % 2]
                kt = ktiles[ki]
                for pi, (Qap, Kap, qres, kres) in enumerate(parts):
                    self.mm(sp[:, 0:nq], Kap(kt), Qap(q0, nq), pi == 0, pi == np_ - 1, [qres, kres], [sp])
                if exp_bias is None:
                    self.act(pt[:, 0:nq], sp[:, 0:nq], AF.Exp, [sp], [pt])
                else:
                    self.act(pt[:, 0:nq], sp[:, 0:nq], AF.Exp, [sp, exp_bias[1]], [pt], bias=exp_bias[0])
                return pt

            pts.append(emit_S(0, it))
            for ki in range(nk):
                if ki + 1 < nk:
                    pts.append(emit_S(ki + 1, it + ki + 1))
                pt = pts[ki]
                kt = ktiles[ki]
                for qb in range(nqb):
                    self.mm(pO[qb][:, 0:129], pt[:, qb * 128:(qb + 1) * 128], V(kt), ki == 0, ki == nk - 1, [pt, vres], [pO[qb]])
            it += nk
            ob = osb[(q0 // 512) % 2]
            for qb in range(nqb):
                self.recip(rec[:, qb:qb + 1], pO[qb][:, 128:129], [pO[qb]], [rec])
                self.act(ob[:, qb, :], pO[qb][:, 0:128], AF.Copy, [pO[qb], rec], [ob], scale=rec[:, qb:qb + 1])
            self.dma(self.cat[q0:q0 + nq, cat_col:cat_col + 128].rearrange("(qb p) c -> p qb c", p=128), ob[:, 0:nqb, :], ob,
                     reads=[ob], writes=[self.cat])

    def attn_bufs(self, ps):
        k = self.k
        pS = [k.psum("pS", [128, 512], F32, ps) for _ in range(2)]
        pO = [k.psum("pO", [128, 512], F32, ps) for _ in range(4)]
        PT = [k.sbuf("PT", [128, 512], BF16, ps) for _ in range(2)]
        osb = [k.sbuf("osb", [128, 4, 128], BF16, ps) for _ in range(2)]
        rec = k.sbuf("rec", [128, 4], F32, ps)
        return pS, pO, PT, osb, rec

    def q_chunks(self, q0, q1, ktiles):
        out = []
        q = q0
        while q < q1:
            n = min(512, q1 - q)
            out.append((q, n, ktiles))
            q += n
        return out

    def l0_attnA(self):
        ps = ExitStack()
        bufs = self.attn_bufs(ps)
        TC, TA, NTC, NTA = self.TC, self.TA, self.NTC, self.NTA
        for h in range(4):
            kvh = h // 2
            parts = [(lambda q0, n, h=h: self.QT[:, h, q0:q0 + n], lambda kt, kvh=kvh: self.KT[:, kvh, kt * 128:(kt + 1) * 128], self.QT, self.KT)]
            V = lambda kt, kvh=kvh: self.VA[:, kt, kvh, 0:129]
            ranges = self.q_chunks(0, TC, list(range(NTC))) + self.q_chunks(TC, TA, list(range(NTA)))
            self.attention(parts, V, self.VA, ranges, h * 128, bufs)
        self.k.flush()
        ps.close()
        self.stA.close()

    def l0_passB(self):
        k = self.k
        NTA = self.NTA
        ps = ExitStack()
        wB = k.sbuf("wB", [128, 8, 2048], BF16, ps)
        for hf in range(2):
            self.dma(wB[:, :, hf * 1024:(hf + 1) * 1024], self.ab_w_in[:, 1024 + hf * 1024:2048 + hf * 1024].rearrange("(k p) n -> p k n", p=128),
                     wB, writes=[wB], eng="pool")
        gnB = k.sbuf("gnB", [128, 512], F32, ps)
        self.dma(gnB[:], self.ab_gn[0:1, :].partition_broadcast(128), gnB, writes=[gnB])
        hT = [k.sbuf("hTb", [128, 8, 128], BF16, ps) for _ in range(2)]
        tab = [k.sbuf("tabB", [128, 2, 512], F32, ps) for _ in range(2)]
        xs = [k.sbuf("xsB", [128, 512], F32, ps) for _ in range(2)]
        t1 = k.sbuf("t1B", [128, 512], F32, ps)
        t2 = k.sbuf("t2B", [128, 512], F32, ps)
        qr = [k.sbuf("qrB", [128, 512], BF16, ps) for _ in range(2)]
        kr = [k.sbuf("krB", [128, 512], BF16, ps) for _ in range(2)]
        vb = [k.sbuf("vbB", [128, 512], BF16, ps) for _ in range(2)]
        gb = [k.sbuf("gbB", [128, 512], BF16, ps) for _ in range(2)]
        gs = k.sbuf("gsB", [128, 512], F32, ps)
        stg = [k.sbuf("stgB", [128, 8, 128], BF16, ps) for _ in range(2)]
        pB = k.psum("pB", [128, 2048], F32, ps)
        pTb = k.psum("pTbB", [128, 1024], BF16, ps)
        for tt in range(NTA):
            i2 = tt % 2
            h_, tb = hT[i2], tab[i2]
            rows = slice(tt * 128, (tt + 1) * 128)
            self.dma(h_[:], self.hT0[tt], h_, reads=[self.hT0], writes=[h_])
            self.dma(tb[:], self.ropeB[rows], tb, writes=[tb])
            for n in range(4):
                for c in range(8):
                    self.mm(pB[:, n * 512:(n + 1) * 512], h_[:, c, :], wB[:, c, n * 512:(n + 1) * 512], c == 0, c == 7, [h_, wB], [pB])
            self.act(xs[0][:], pB[:, 0:512], AF.Copy, [pB], [xs[0]], scale=128.0 ** -0.5)
            self.rope(qr[i2][:], qr[i2], xs[0][:], tb[:, 0, :], tb[:, 1, :], 4, 64, t1, t2, [xs[0], tb])
            self.act(xs[1][:], pB[:, 512:1024], AF.Copy, [pB], [xs[1]])
            self.rope(kr[i2][:], kr[i2], xs[1][:], tb[:, 0, :], tb[:, 1, :], 4, 64, t1, t2, [xs[1], tb])
            self.dma(self.kB[rows, :], kr[i2][:], kr[i2], reads=[kr[i2]], writes=[self.kB])
            for hh in range(4):
                self.tr(pTb[:, hh * 128:(hh + 1) * 128], qr[i2][:, hh * 128:(hh + 1) * 128], self.identB[:], [qr[i2], self.identB], [pTb])
                self.tr(pTb[:, 512 + hh * 128:512 + (hh + 1) * 128], kr[i2][:, hh * 128:(hh + 1) * 128], self.identB[:], [kr[i2], self.identB], [pTb])
            self.cp("act", stg[i2][:], pTb[:].rearrange("p (h t) -> p h t", h=8), [pTb], [stg[i2]])
            self.dma(self.qTB[:, :, rows].rearrange("h d t -> d h t"), stg[i2][:, 0:4, :], stg[i2], reads=[stg[i2]], writes=[self.qTB])
            self.dma(self.kTB[:, :, rows].rearrange("h d t -> d h t"), stg[i2][:, 4:8, :], stg[i2], reads=[stg[i2]], writes=[self.kTB])
            self.cp("dve", vb[i2][:], pB[:, 1024:1536], [pB], [vb[i2]])
            self.dma(self.vB[rows, :], vb[i2][:], vb[i2], reads=[vb[i2]], writes=[self.vB])
            self.act(gs[:], pB[:, 1536:2048], AF.Silu, [pB], [gs])
            self.tt("dve", gb[i2][:], gs[:], gnB[:], ALU.mult, [gs, gnB], [gb[i2]])
            self.dma(self.gB[rows, :], gb[i2][:], gb[i2], reads=[gb[i2]], writes=[self.gB])
        k.flush()
        ps.close()

    def l0_retention(self):
        k = self.k
        NTA, NTC, TA = self.NTA, self.NTC, self.TA
        ps = ExitStack()
        ld = k.sbuf("ld", [128, 8], F32, ps)
        self.dma(ld[:], self.ab_ld[0:1, :].partition_broadcast(128), ld, writes=[ld])
        lg = k.sbuf("lg", [128, 8], F32, ps)
        nlg = k.sbuf("nlg", [128, 8], F32, ps)
        self.act(lg[:], ld[:], AF.Exp, [ld], [lg])
        self.ts("dve", lg[:], lg[:], -1.0, 1.0, ALU.mult, ALU.add, [lg], [lg])
        self.act(lg[:], lg[:], AF.Ln, [lg], [lg])
        self.ts("dve", nlg[:], lg[:], -1.0, None, ALU.mult, None, [lg], [nlg])
        ii = k.sbuf("ii", [128, 128], I32, ps)
        dif = k.sbuf("dif", [128, 128], F32, ps)
        c1 = k.sbuf("c1", [128, 128], F32, ps)
        c128 = k.sbuf("c128", [128, 128], F32, ps)
        pcol = k.sbuf("pcol", [128, 4], F32, ps)
        k.op("pool", lambda e: e.iota(ii[:], [[1, 128]], base=0, channel_multiplier=-1), (), [ii])
        self.cp("dve", dif[:], ii[:], [ii], [dif])
        k.op("pool", lambda e: e.iota(ii[:], [[1, 128]], base=1, channel_multiplier=0), [dif], [ii])
        self.cp("dve", c1[:], ii[:], [ii], [c1])
        self.ts("dve", c128[:], c1[:], -1.0, 129.0, ALU.mult, ALU.add, [c1], [c128])
        k.op("pool", lambda e: e.iota(ii[:, 0:1], [[1, 1]], base=0, channel_multiplier=1), [c1], [ii])
        self.cp("dve", pcol[:, 0:1], ii[:, 0:1], [ii], [pcol])
        self.ts("dve", pcol[:, 1:2], pcol[:, 0:1], -1.0, 127.0, ALU.mult, ALU.add, [pcol], [pcol])
        self.memset("dve", pcol[:, 2:3], 128.0, [pcol])
        mf = k.sbuf("mf", [128, 128], F32, ps)
        mb = k.sbuf("mb", [128, 128], F32, ps)
        self.ts("dve", mf[:], dif[:], 0.0, None, ALU.is_ge, None, [dif], [mf])
        self.ts("dve", mb[:], dif[:], 0.0, None, ALU.is_lt, None, [dif], [mb])
        DT = k.sbuf("DT", [128, 128], F32, ps)
        DT2 = k.sbuf("DT2", [128, 128], F32, ps)
        xiF = k.sbuf("xiF", [128, 128], F32, ps)
        xiB = k.sbuf("xiB", [128, 128], F32, ps)
        zc = k.sbuf("zc", [128, 4], F32, ps)
        qT = k.sbuf("qTr", [128, TA], BF16, ps)
        kT = k.sbuf("kTr", [128, TA], BF16, ps)
        kk = k.sbuf("kkr", [128, NTA, 128], BF16, ps)
        vv = k.sbuf("vvr", [128, NTA, 128], BF16, ps)
        gg = k.sbuf("ggr", [128, NTA, 128], BF16, ps)
        Kzf = k.sbuf("Kzf", [128, NTA, 128], BF16, ps)
        Kzb = k.sbuf("Kzb", [128, NTA, 128], BF16, ps)
        SfP = k.sbuf("SfP", [128, NTA, 128], BF16, ps)
        SbP = k.sbuf("SbP", [128, NTA, 128], BF16, ps)
        Sf = k.sbuf("Sf", [128, 128], F32, ps)
        Sb = k.sbuf("Sb", [128, 128], F32, ps)
        Pm = [k.sbuf("Pm", [128, 128], BF16, ps) for _ in range(2)]
        qxf = [k.sbuf("qxf", [128, 128], BF16, ps) for _ in range(2)]
        qxb = [k.sbuf("qxb", [128, 128], BF16, ps) for _ in range(2)]
        st6 = k.sbuf("st6", [128, 6], F32, ps)
        mv = k.sbuf("mv", [128, 4], F32, ps)
        on = [k.sbuf("on", [128, 128], F32, ps) for _ in range(2)]
        ystg = k.sbuf("ystg", [128, NTA, 128], BF16, ps)
        pkv = [k.psum("pkv", [128, 512], F32, ps) for _ in range(2)]
        psc = [k.psum("psc", [128, 512], F32, ps) for _ in range(2)]
        pO = [k.psum("pOr", [128, 512], F32, ps) for _ in range(2)]
        order_b = list(range(NTC - 1, -1, -1)) + list(range(NTA - 1, NTC - 1, -1))
        for h in range(4):
            hs = slice(h * 128, (h + 1) * 128)
            self.dma(qT[:], self.qTB[h], qT, reads=[self.qTB], writes=[qT])
            self.dma(kT[:], self.kTB[h], kT, reads=[self.kTB], writes=[kT])
            for n0 in range(0, NTA, 8):
                n1 = min(NTA, n0 + 8)
                rs = slice(n0 * 128, n1 * 128)
                self.dma(kk[:, n0:n1, :], self.kB[rs, hs].rearrange("(n p) d -> p n d", p=128), kk, reads=[self.kB], writes=[kk])
                self.dma(vv[:, n0:n1, :], self.vB[rs, hs].rearrange("(n p) d -> p n d", p=128), vv, reads=[self.vB], writes=[vv])
                self.dma(gg[:, n0:n1, :], self.gB[rs, hs].rearrange("(n p) d -> p n d", p=128), gg, reads=[self.gB], writes=[gg])
            lf, lb, nlb = lg[:, h:h + 1], lg[:, 4 + h:5 + h], nlg[:, 4 + h:5 + h]
            self.act(DT[:], dif[:], AF.Exp, [dif, lg], [DT], scale=lf)
            self.tt("dve", DT[:], DT[:], mf[:], ALU.mult, [DT, mf], [DT])
            self.act(DT2[:], dif[:], AF.Exp, [dif, nlg], [DT2], scale=nlb)
            self.tt("dve", DT2[:], DT2[:], mb[:], ALU.mult, [DT2, mb], [DT2])
            self.tt("dve", DT[:], DT[:], DT2[:], ALU.add, [DT, DT2], [DT])
            self.act(xiF[:], c1[:], AF.Exp, [c1, lg], [xiF], scale=lf)
            self.act(xiB[:], c128[:], AF.Exp, [c128, lg], [xiB], scale=lb)
            self.act(zc[:, 0:1], pcol[:, 1:2], AF.Exp, [pcol, lg], [zc], scale=lf)
            self.act(zc[:, 1:2], pcol[:, 0:1], AF.Exp, [pcol, lg], [zc], scale=lb)
            self.act(zc[:, 2:3], pcol[:, 2:3], AF.Exp, [pcol, lg], [zc], scale=lf)
            self.act(zc[:, 3:4], pcol[:, 2:3], AF.Exp, [pcol, lg], [zc], scale=lb)
            self.ts("dve", Kzf[:], kk[:], zc[:, 0:1], None, ALU.mult, None, [kk, zc], [Kzf])
            self.ts("pool", Kzb[:], kk[:], zc[:, 1:2], None, ALU.mult, None, [kk, zc], [Kzb])
            self.memset("dve", Sf[:], 0.0, [Sf])
            self.memset("pool", Sb[:], 0.0, [Sb])
            for it, n in enumerate(range(NTA)):
                p_ = pkv[it % 2]
                self.cp("act", SfP[:, n, :], Sf[:], [Sf], [SfP])
                self.mm(p_[:, 0:128], Kzf[:, n, :], vv[:, n, :], True, True, [Kzf, vv], [p_])
                self.stt("dve", Sf[:], Sf[:], zc[:, 2:3], p_[:, 0:128], ALU.mult, ALU.add, [Sf, zc, p_], [Sf])
            for it, n in enumerate(order_b):
                p_ = pkv[it % 2]
                self.cp("act", SbP[:, n, :], Sb[:], [Sb], [SbP])
                self.mm(p_[:, 0:128], Kzb[:, n, :], vv[:, n, :], True, True, [Kzb, vv], [p_])
                self.stt("dve", Sb[:], Sb[:], zc[:, 3:4], p_[:, 0:128], ALU.mult, ALU.add, [Sb, zc, p_], [Sb])
            for n in range(NTA):
                i2 = n % 2
                cs = slice(n * 128, (n + 1) * 128)
                self.mm(psc[i2][:, 0:128], kT[:, cs], qT[:, cs], True, True, [kT, qT], [psc[i2]])
                self.tt("dve", Pm[i2][:], psc[i2][:, 0:128], DT[:], ALU.mult, [psc[i2], DT], [Pm[i2]])
                self.tt("pool", qxf[i2][:], qT[:, cs], xiF[:], ALU.mult, [qT, xiF], [qxf[i2]])
                self.tt("pool", qxb[i2][:], qT[:, cs], xiB[:], ALU.mult, [qT, xiB], [qxb[i2]])
                o_ = pO[i2]
                self.mm(o_[:, 0:128], Pm[i2][:], vv[:, n, :], True, False, [Pm[i2], vv], [o_])
                self.mm(o_[:, 0:128], qxf[i2][:], SfP[:, n, :], False, False, [qxf[i2], SfP], [o_])
                self.mm(o_[:, 0:128], qxb[i2][:], SbP[:, n, :], False, True, [qxb[i2], SbP], [o_])
                k.op("dve", lambda e, o_=o_: e.bn_stats(out=st6[:], in_=o_[:, 0:128]), [o_], [st6])
                k.op("dve", lambda e: e.bn_aggr(out=mv[:, 0:2], in_=st6[:]), [st6], [mv])
                self.act(mv[:, 2:3], mv[:, 1:2], AF.Sqrt, [mv], [mv], bias=1e-5)
                self.recip(mv[:, 2:3], mv[:, 2:3], [mv], [mv])
                self.stt("dve", mv[:, 3:4], mv[:, 0:1], -1.0, mv[:, 2:3], ALU.mult, ALU.mult, [mv], [mv])
                self.act(on[i2][:], o_[:, 0:128], AF.Identity, [o_, mv], [on[i2]], bias=mv[:, 3:4], scale=mv[:, 2:3])
                self.tt("pool", ystg[:, n, :], on[i2][:], gg[:, n, :], ALU.mult, [on[i2], gg], [ystg])
            for n0 in range(0, NTA, 8):
                n1 = min(NTA, n0 + 8)
                self.dma(self.cat[n0 * 128:n1 * 128, 512 + h * 128:512 + (h + 1) * 128].rearrange("(n p) c -> p n c", p=128), ystg[:, n0:n1, :], ystg,
                         reads=[ystg], writes=[self.cat])
        k.flush()
        ps.close()

    def layernorm(self, zn, z, st, mv, eps=1e-5):
        k = self.k
        for hf in range(2):
            k.op("dve", lambda e, hf=hf: e.bn_stats(out=st[:, hf * 6:(hf + 1) * 6], in_=z[:, hf * 512:(hf + 1) * 512]), [z], [st])
        k.op("dve", lambda e: e.bn_aggr(out=mv[:, 0:2], in_=st[:, 0:12]), [st], [mv])
        self.act(mv[:, 2:3], mv[:, 1:2], AF.Sqrt, [mv], [mv], bias=eps)
        self.recip(mv[:, 2:3], mv[:, 2:3], [mv], [mv])
        self.stt("dve", mv[:, 3:4], mv[:, 0:1], -1.0, mv[:, 2:3], ALU.mult, ALU.mult, [mv], [mv])
        self.act(zn[:], z[:], AF.Identity, [z, mv], [zn], bias=mv[:, 3:4], scale=mv[:, 2:3])

    def resid_src(self, i, tt):
        if i == 0:
            return self.xsrc(tt)
        return self.x2, self.x2[tt * 128:(tt + 1) * 128, :]

    def post_mixer(self, i, w_out, tiles):
        k = self.k
        ps = ExitStack()
        wo = k.sbuf("wo", [128, 8, D], BF16, ps)
        self.dma(wo[:], w_out[:, :].rearrange("(k p) n -> p k n", p=128), wo, writes=[wo], eng="pool")
        rw = k.sbuf("rw", [128, 8, NE], F32, ps)
        self.dma(rw[:], self.r_w[i].rearrange("(k p) e -> p k e", p=128), rw, writes=[rw])
        rb = k.sbuf("rb", [1, NE], F32, ps)
        self.dma(rb[:], self.r_b[i], rb, writes=[rb])
        lnB = k.sbuf("lnB", [128, 2, D], F32, ps)
        for q in range(2):
            self.dma(lnB[:, q, :], self.ln_rows[i][q:q + 1, :].partition_broadcast(128), lnB, writes=[lnB])
        gB = k.sbuf("g2B", [128, 2, D], F32, ps)
        for s in range(2):
            self.dma(gB[:, s, :], self.gate_rows[i, 0, s:s + 1, :].partition_broadcast(128), gB, reads=[self.gate_rows], writes=[gB])
        ct = [k.sbuf("ct", [128, D], BF16, ps) for _ in range(2)]
        catT = k.sbuf("catT", [128, 8, 128], BF16, ps)
        xt = [k.sbuf("xtp", [128, D], F32, ps) for _ in range(2)]
        z = k.sbuf("z", [128, D], F32, ps)
        zn = k.sbuf("zn", [128, D], F32, ps)
        xl = [k.sbuf("xl", [128, D], F32, ps) for _ in range(2)]
        st = k.sbuf("st", [128, 12], F32, ps)
        mv = k.sbuf("mvp", [128, 4], F32, ps)
        h2f = k.sbuf("h2f", [128, 8, 128], F32, ps)
        h2b = [k.sbuf("h2b", [128, 8, 128], BF16, ps) for _ in range(2)]
        lgt = k.sbuf("lgt", [128, NE], F32, ps)
        m8 = k.sbuf("m8", [128, 8], F32, ps)
        msk = k.sbuf("msk", [128, NE], F32, ps)
        ex = k.sbuf("ex", [128, NE], F32, ps)
        den = k.sbuf("den", [128, 2], F32, ps)
        pTb = k.psum("pTbp", [128, 1024], BF16, ps)
        pY = k.psum("pYp", [128, 1024], F32, ps)
        pT = [k.psum("pTp", [128, 512], F32, ps) for _ in range(2)]
        pR = k.psum("pR", [128, 512], F32, ps)
        coef = self.coef[i]
        G = self.G[i]
        for it, tt in enumerate(tiles):
            i2 = it % 2
            s = 1 if tt < self.NTC else 0
            rows = slice(tt * 128, (tt + 1) * 128)
            c_ = ct[i2]
            self.dma(c_[:], self.cat[rows, :], c_, reads=[self.cat], writes=[c_])
            for c in range(8):
                self.tr(pTb[:, c * 128:(c + 1) * 128], c_[:, c * 128:(c + 1) * 128], self.identB[:], [c_, self.identB], [pTb])
            self.cp("act", catT[:], pTb[:].rearrange("p (c t) -> p c t", c=8), [pTb], [catT])
            for n in range(2):
                for c in range(8):
                    self.mm(pY[:, n * 512:(n + 1) * 512], catT[:, c, :], wo[:, c, n * 512:(n + 1) * 512], c == 0, c == 7, [catT, wo], [pY])
            src_t, src_ap = self.resid_src(i, tt)
            x_ = xt[i2]
            self.dma(x_[:], src_ap, x_, reads=[src_t], writes=[x_])
            self.tt("dve", z[:], pY[:], gB[:, s, :], ALU.mult, [pY, gB], [z])
            self.stt("dve", z[:], x_[:], DN_ALPHA, z[:], ALU.mult, ALU.add, [x_, z], [z])
            self.layernorm(zn, z, st, mv)
            x1_ = xl[i2]
            self.tt("pool", x1_[:], zn[:], lnB[:, 0, :], ALU.mult, [zn, lnB], [x1_])
            self.tt("pool", x1_[:], x1_[:], lnB[:, 1, :], ALU.add, [x1_, lnB], [x1_])
            self.dma(self.x1[rows, :], x1_[:], x1_, reads=[x1_], writes=[self.x1])
            for half in range(2):
                for j in range(4):
                    c = half * 4 + j
                    self.tr(pT[half][:, j * 128:(j + 1) * 128], zn[:, c * 128:(c + 1) * 128], self.identF[:], [zn, self.identF], [pT[half]])
                for j in range(4):
                    c = half * 4 + j
                    self.act(h2f[:, c, :], pT[half][:, j * 128:(j + 1) * 128], AF.Identity, [pT[half], coef], [h2f],
                             bias=coef[:, 3, s, c:c + 1], scale=coef[:, 2, s, c:c + 1])
            hb = h2b[i2]
            self.cp("pool", hb[:], h2f[:], [h2f], [hb])
            self.dma(self.h2T[tt], hb[:], hb, reads=[hb], writes=[self.h2T])
            for c in range(8):
                self.mm(pR[:, 0:NE], h2f[:, c, :], rw[:, c, :], c == 0, False, [h2f, rw], [pR])
            self.mm(pR[:, 0:NE], self.ones[0:1, 0:128], rb[:], False, True, [self.ones, rb], [pR])
            self.cp("act", lgt[:], pR[:, 0:NE], [pR], [lgt])
            k.op("dve", lambda e: e.max(out=m8[:], in_=lgt[:]), [lgt], [m8])
            self.ts("dve", msk[:], lgt[:], m8[:, 3:4], None, ALU.is_ge, None, [lgt, m8], [msk])
            self.ts("dve", den[:, 0:1], m8[:, 0:1], -1.0, None, ALU.mult, None, [m8], [den])
            self.act(ex[:], lgt[:], AF.Exp, [lgt, den], [ex], bias=den[:, 0:1])
            self.tt("dve", ex[:], ex[:], msk[:], ALU.mult, [ex, msk], [ex])
            k.op("dve", lambda e: e.reduce_sum(out=den[:, 1:2], in_=ex[:], axis=AX.X), [ex], [den])
            self.recip(den[:, 1:2], den[:, 1:2], [den], [den])
            self.ts("dve", G[:, tt, :], ex[:], den[:, 1:2], None, ALU.mult, None, [ex, den], [G])
        k.flush()
        ps.close()

    def moe(self, i, tiles, out_fn, GT=12, SC=4):
        k = self.k
        G = self.G[i]
        groups = [tiles[j:j + GT] for j in range(0, len(tiles), GT)]
        gps = ExitStack()
        bd = k.sbuf("bd", [NE, D], F32, gps)
        self.dma(bd[:], self.b_down[i], bd, writes=[bd])
        for grp in groups:
            ng = len(grp)
            subs = [list(range(j, min(ng, j + SC))) for j in range(0, ng, SC)]
            ps = ExitStack()
            facc = k.sbuf("facc", [128, GT, D], F32, ps)
            gT = k.sbuf("gT", [NE, 128], F32, ps)
            ins = ExitStack()
            Hb = [k.sbuf("H", [128, 8, SC * 128], BF16, ins) for _ in range(2)]
            wu = [k.sbuf("wu", [128, 8, 2 * D], BF16, ins) for _ in range(2)]
            wd = [k.sbuf("wd", [128, 8, D], BF16, ins) for _ in range(2)]
            bu = [k.sbuf("bu", [128, 16], F32, ins) for _ in range(2)]
            actT = [k.sbuf("actT", [128, 8, SC * 128], BF16, ins) for _ in range(2)]
            glu = [k.sbuf("glu", [128, SC * 128], F32, ins) for _ in range(2)]
            sig = [k.sbuf("sig", [128, SC * 128], F32, ins) for _ in range(2)]
            l1 = [k.sbuf("l1", [128, SC * 128], F32, ins) for _ in range(2)]
            pU = [k.psum("pU", [128, 2, 512], F32, ins) for _ in range(2)]
            pY = [k.psum("pYm", [128, 1024], F32, ins) for _ in range(2)]
            for j, tt in enumerate(grp):
                py = pY[j % 2]
                self.tr(pU[0][0:NE, 0, 0:128], G[:, tt, :], self.identF[:], [G, self.identF], [pU[0]])
                self.cp("act", gT[:], pU[0][0:NE, 0, 0:128], [pU[0]], [gT])
                for n in range(2):
                    self.mm(py[:, n * 512:(n + 1) * 512], gT[:], bd[:, n * 512:(n + 1) * 512], True, True, [gT, bd], [py])
                self.cp("act", facc[:, j, :], py[:], [py], [facc])
            hit = 0
            pending = None
            for e in range(NE):
                e2 = e % 2
                wu_, wd_, bu_ = wu[e2], wd[e2], bu[e2]
                for hf in range(2):
                    self.dma(wu_[:, :, hf * D:(hf + 1) * D], self.w_up[i, e][:, hf * D:(hf + 1) * D].rearrange("(k p) n -> p k n", p=128),
                             wu_, writes=[wu_], eng="pool")
                self.dma(wd_[:], self.w_down[i, e].rearrange("(k p) n -> p k n", p=128), wd_, writes=[wd_], eng="pool")
                self.dma(bu_[:], self.b_upF[i, e], bu_, writes=[bu_])
                self.ts("pool", bu_[:, 8:16], bu_[:, 8:16], 1.0, None, ALU.add, None, [bu_], [bu_])
                for sub in subs:
                    N = len(sub) * 128
                    H = Hb[hit % 2]
                    aT = actT[hit % 2]
                    hit += 1
                    for jj, j in enumerate(sub):
                        self.dma(H[:, :, jj * 128:(jj + 1) * 128], self.h2T[grp[j]], H, reads=[self.h2T], writes=[H])
                    for m in range(8):
                        m2 = m % 2
                        pu = pU[m2]
                        for c in range(8):
                            self.mm(pu[:, 0, 0:N], wu_[:, c, m * 128:(m + 1) * 128], H[:, c, 0:N], c == 0, c == 7, [wu_, H], [pu])
                        for c in range(8):
                            self.mm(pu[:, 1, 0:N], wu_[:, c, D + m * 128:D + (m + 1) * 128], H[:, c, 0:N], c == 0, c == 7, [wu_, H], [pu])
                        g_, s_, l_ = glu[m2], sig[m2], l1[m2]
                        self.ts("dve", g_[:, 0:N], pu[:, 0, 0:N], bu_[:, m:m + 1], LIMIT, ALU.add, ALU.min, [pu, bu_], [g_])
                        self.act(s_[:, 0:N], g_[:, 0:N], AF.Sigmoid, [g_], [s_], scale=SW_ALPHA)
                        self.ts("dve", l_[:, 0:N], pu[:, 1, 0:N], bu_[:, 8 + m:9 + m], LIMIT + 1.0, ALU.add, ALU.min, [pu, bu_], [l_])
                        self.tt("pool", s_[:, 0:N], s_[:, 0:N], g_[:, 0:N], ALU.mult, [s_, g_], [s_])
                        self.stt("dve", aT[:, m, 0:N], l_[:, 0:N], 1.0 - LIMIT, s_[:, 0:N], ALU.max, ALU.mult, [l_, s_], [aT])

                    def down(sub=sub, aT=aT, wd_=wd_, e=e):
                        for jj, j in enumerate(sub):
                            tt = grp[j]
                            py = pY[j % 2]
                            for n in range(2):
                                for m in range(8):
                                    self.mm(py[:, n * 512:(n + 1) * 512], aT[:, m, jj * 128:(jj + 1) * 128], wd_[:, m, n * 512:(n + 1) * 512], m == 0, m == 7,
                                            [aT, wd_], [py])
                            self.stt("dve", facc[:, j, :], py[:], G[:, tt, e:e + 1], facc[:, j, :], ALU.mult, ALU.add, [py, G, facc], [facc])

                    if pending is not None:
                        pending()
                    pending = down
            if pending is not None:
                pending()
                pending = None
            k.flush()
            ins.close()
            ln = ExitStack()
            lnB = k.sbuf("lnB2", [128, 2, D], F32, ln)
            for q in range(2):
                self.dma(lnB[:, q, :], self.ln_rows[i][2 + q:3 + q, :].partition_broadcast(128), lnB, writes=[lnB])
            gB = k.sbuf("g5B", [128, 2, D], F32, ln)
            for s in range(2):
                self.dma(gB[:, s, :], self.gate_rows[i, 1, s:s + 1, :].partition_broadcast(128), gB, reads=[self.gate_rows], writes=[gB])
            x1t = [k.sbuf("x1t", [128, D], F32, ln) for _ in range(2)]
            z = k.sbuf("z2", [128, D], F32, ln)
            zn = k.sbuf("zn2", [128, D], F32, ln)
            ot = [k.sbuf("ot", [128, D], F32, ln) for _ in range(2)]
            st = k.sbuf("st2", [128, 12], F32, ln)
            mv = k.sbuf("mv2", [128, 4], F32, ln)
            for j, tt in enumerate(grp):
                s = 1 if tt < self.NTC else 0
                x_ = x1t[j % 2]
                self.dma(x_[:], self.x1[tt * 128:(tt + 1) * 128, :], x_, reads=[self.x1], writes=[x_])
                self.tt("dve", z[:], facc[:, j, :], gB[:, s, :], ALU.mult, [facc, gB], [z])
                self.stt("dve", z[:], x_[:], DN_ALPHA, z[:], ALU.mult, ALU.add, [x_, z], [z])
                self.layernorm(zn, z, st, mv)
                o_ = ot[j % 2]
                self.tt("pool", o_[:], zn[:], lnB[:, 0, :], ALU.mult, [zn, lnB], [o_])
                self.tt("pool", o_[:], o_[:], lnB[:, 1, :], ALU.add, [o_, lnB], [o_])
                dst_t, dst_ap = out_fn(tt)
                self.dma(dst_ap, o_[:], o_, reads=[o_], writes=[dst_t])
            k.flush()
            ln.close()
            ps.close()
        gps.close()

    def l1_passM(self):
        k = self.k
        NTA, NTC, TA = self.NTA, self.NTC, self.TA
        self.stM = ExitStack()
        self.qlatT = k.sbuf("qlatT", [128, 3, TA], BF16, self.stM)
        self.ckvT = k.sbuf("ckvT", [128, 2, TA], BF16, self.stM)
        self.kpeT = k.sbuf("kpeT", [128, TA], BF16, self.stM)
        ps = ExitStack()
        wM = k.sbuf("wM", [128, 8, 704], BF16, ps)
        self.dma(wM[:], self.mla_w_in[:, :].rearrange("(k p) n -> p k n", p=128), wM, writes=[wM], eng="pool")
        gn = k.sbuf("gnM", [128, 640], F32, ps)
        self.dma(gn[:, 0:384], self.mla_qn[0:1, :].partition_broadcast(128), gn, writes=[gn])
        self.dma(gn[:, 384:640], self.mla_kvn[0:1, :].partition_broadcast(128), gn, writes=[gn])
        xt = [k.sbuf("xtM", [128, D], F32, ps) for _ in range(2)]
        hT = [k.sbuf("hTM", [128, 8, 128], BF16, ps) for _ in range(2)]
        tab = [k.sbuf("tabM", [128, 2, 64], F32, ps) for _ in range(2)]
        ss5 = k.sbuf("ss5", [128, 8], F32, ps)
        ssq = k.sbuf("ssq", [128, 2], F32, ps)
        rsd = k.sbuf("rsd", [128, 2], F32, ps)
        xnf = k.sbuf("xnf", [128, 640], F32, ps)
        junk = k.sbuf("junkM", [128, 128], F32, ps)
        xn = [k.sbuf("xnM", [128, 640], BF16, ps) for _ in range(2)]
        kp = k.sbuf("kpM", [128, 64], F32, ps)
        t1 = k.sbuf("t1M", [128, 64], F32, ps)
        t2 = k.sbuf("t2M", [128, 64], F32, ps)
        kb = [k.sbuf("kbM", [128, 128], BF16, ps) for _ in range(2)]
        for _kb in kb:
            self.memset("pool", _kb[:], 0.0, [_kb])
        pT = [k.psum("pTM", [128, 512], F32, ps) for _ in range(2)]
        pM = k.psum("pM", [128, 1024], F32, ps)
        pTb = k.psum("pTbM", [128, 1024], BF16, ps)
        coef = self.coef[1]
        for tt in range(NTA):
            i2 = tt % 2
            s = 1 if tt < NTC else 0
            rows = slice(tt * 128, (tt + 1) * 128)
            x_, h_, tb = xt[i2], hT[i2], tab[i2]
            self.load_hT(self.x2, self.x2[rows, :], x_, pT, h_, coef, s, self.identF)
            self.dma(tb[:], self.ropeM[rows, :, 0:64], tb, writes=[tb])
            for c in range(8):
                self.mm(pM[:, 0:512], h_[:, c, :], wM[:, c, 0:512], c == 0, c == 7, [h_, wM], [pM])
            for c in range(8):
                self.mm(pM[:, 512:704], h_[:, c, :], wM[:, c, 512:704], c == 0, c == 7, [h_, wM], [pM])
            for c in range(5):
                self.act(junk[:, 0:128], pM[:, c * 128:(c + 1) * 128], AF.Square, [pM], [junk, ss5], accum_out=ss5[:, c:c + 1])
            self.tt("dve", ssq[:, 0:1], ss5[:, 0:1], ss5[:, 1:2], ALU.add, [ss5], [ssq])
            self.tt("dve", ssq[:, 0:1], ssq[:, 0:1], ss5[:, 2:3], ALU.add, [ss5, ssq], [ssq])
            self.tt("dve", ssq[:, 1:2], ss5[:, 3:4], ss5[:, 4:5], ALU.add, [ss5], [ssq])
            self.act(rsd[:, 0:1], ssq[:, 0:1], AF.Sqrt, [ssq], [rsd], bias=1e-6, scale=1.0 / 384)
            self.act(rsd[:, 1:2], ssq[:, 1:2], AF.Sqrt, [ssq], [rsd], bias=1e-6, scale=1.0 / 256)
            self.recip(rsd[:, 0:2], rsd[:, 0:2], [rsd], [rsd])
            x2_ = xn[i2]
            self.stt("dve", xnf[:, 0:384], pM[:, 0:384], rsd[:, 0:1], gn[:, 0:384], ALU.mult, ALU.mult, [pM, rsd, gn], [xnf])
            self.stt("dve", xnf[:, 384:640], pM[:, 384:640], rsd[:, 1:2], gn[:, 384:640], ALU.mult, ALU.mult, [pM, rsd, gn], [xnf])
            self.cp("pool", x2_[:], xnf[:], [xnf], [x2_])
            self.cp("act", kp[:], pM[:, 640:704], [pM], [kp])
            self.rope(kb[i2][:, 0:64], kb[i2], kp[:], tb[:, 0, :], tb[:, 1, :], 2, 16, t1, t2, [kp, tb])
            for c in range(5):
                self.tr(pTb[:, c * 128:(c + 1) * 128], x2_[:, c * 128:(c + 1) * 128], self.identB[:], [x2_, self.identB], [pTb])
            self.tr(pTb[:, 640:768], kb[i2][:], self.identB[:], [kb[i2], self.identB], [pTb])
            self.cp("act", self.qlatT[:, :, rows], pTb[:, 0:384].rearrange("p (c t) -> p c t", c=3), [pTb], [self.qlatT])
            self.cp("act", self.ckvT[:, :, rows], pTb[:, 384:640].rearrange("p (c t) -> p c t", c=2), [pTb], [self.ckvT])
            self.cp("act", self.kpeT[:, rows], pTb[:, 640:768], [pTb], [self.kpeT])
        k.flush()
        ps.close()

    def l1_attn(self):
        k = self.k
        NTA, NTC, TA, TC, TL = self.NTA, self.NTC, self.TA, self.TC, self.TL
        ps = ExitStack()
        wuq = k.sbuf("wuq", [128, 3, 1536], BF16, ps)
        self.dma(wuq[:], self.mla_w_uq[:, :].rearrange("(k p) n -> p k n", p=128), wuq, writes=[wuq], eng="pool")
        wukv = k.sbuf("wukv", [128, 2, 2048], BF16, ps)
        self.dma(wukv[:], self.mla_w_ukv[:, :].rearrange("(k p) n -> p k n", p=128), wukv, writes=[wukv], eng="pool")
        wuqr = k.sbuf("wuqr", [128, 3, 8, 128], BF16, ps)
        self.memset("pool", wuqr[:], 0.0, [wuqr])
        for c in range(3):
            self.dma(wuqr[:, c, :, 0:64], self.mla_w_uq[c * 128:(c + 1) * 128, :].rearrange("p (h c) -> p h c", c=192)[:, :, 128:192],
                     wuqr, writes=[wuqr], eng="pool")
        pmat = k.sbuf("pmat", [128, 128], F32, ps)
        self.dma(pmat[:], self.pmat_in[:, :], pmat, writes=[pmat])
        CT = k.sbuf("CTm", [128, 2, TA], F32, ps)
        self.dma(CT[:], self.ropeMT[:, :, :], CT, writes=[CT])
        KnT = k.sbuf("KnT", [128, TA], BF16, ps)
        Vh = k.sbuf("Vh", [128, NTA, 130], BF16, ps)
        QnT = k.sbuf("QnT", [128, TA], BF16, ps)
        QrT = k.sbuf("QrT", [128, TA], BF16, ps)
        raw = k.sbuf("rawq", [128, 512], F32, ps)
        u1 = k.sbuf("u1", [128, 512], F32, ps)
        u2 = k.sbuf("u2", [128, 512], F32, ps)
        self.memset("pool", Vh[:], 1.0, [Vh])
        pP = [k.psum("pP", [128, 512], F32, ps) for _ in range(2)]
        bufs = self.attn_bufs(ps)
        qscale = 192.0 ** -0.5
        chunks = lambda a, b_: [(q, min(512, b_ - q)) for q in range(a, b_, 512)]
        for h in range(8):
            kc, vc, qc, rc = h * 256, h * 256 + 128, h * 192, h * 192 + 128
            for ci, (t0, n) in enumerate(chunks(0, TA)):
                p_ = pP[ci % 2]
                for c in range(2):
                    self.mm(p_[:, 0:n], wukv[:, c, kc:kc + 128], self.ckvT[:, c, t0:t0 + n], c == 0, c == 1, [wukv, self.ckvT], [p_])
                self.cp("act", KnT[:, t0:t0 + n], p_[:, 0:n], [p_], [KnT])
            for tt in range(NTA):
                p_ = pP[tt % 2]
                for c in range(2):
                    self.mm(p_[:, 0:128], self.ckvT[:, c, tt * 128:(tt + 1) * 128], wukv[:, c, vc:vc + 128], c == 0, c == 1, [wukv, self.ckvT], [p_])
                self.cp("dve", Vh[:, tt, 0:128], p_[:, 0:128], [p_], [Vh])
            for ci, (t0, n) in enumerate(chunks(TC, TA)):
                p_ = pP[ci % 2]
                for c in range(3):
                    self.mm(p_[:, 0:n], wuq[:, c, qc:qc + 128], self.qlatT[:, c, t0:t0 + n], c == 0, c == 2, [wuq, self.qlatT], [p_])
                self.act(QnT[:, t0:t0 + n], p_[:, 0:n], AF.Copy, [p_], [QnT], scale=qscale)
                p2 = pP[(ci + 1) % 2]
                for c in range(3):
                    self.mm(p2[:, 0:n], wuqr[:, c, h, :], self.qlatT[:, c, t0:t0 + n], c == 0, c == 2, [wuqr, self.qlatT], [p2])
                self.act(raw[:, 0:n], p2[:, 0:n], AF.Copy, [p2], [raw], scale=qscale)
                self.mm(p2[:, 0:n], pmat[:], raw[:, 0:n], True, True, [pmat, raw], [p2])
                self.tt("dve", u1[:, 0:n], raw[:, 0:n], CT[:, 0, t0:t0 + n], ALU.mult, [raw, CT], [u1])
                self.tt("dve", u2[:, 0:n], p2[:, 0:n], CT[:, 1, t0:t0 + n], ALU.mult, [p2, CT], [u2])
                self.tt("dve", QrT[:, t0:t0 + n], u1[:, 0:n], u2[:, 0:n], ALU.add, [u1, u2], [QrT])
            parts = [(lambda q0, n: QnT[:, q0:q0 + n], lambda kt: KnT[:, kt * 128:(kt + 1) * 128], QnT, KnT),
                     (lambda q0, n: QrT[:, q0:q0 + n], lambda kt: self.kpeT[:, kt * 128:(kt + 1) * 128], QrT, self.kpeT)]
            V = lambda kt: Vh[:, kt, 0:129]
            self.attention(parts, V, Vh, self.q_chunks(TC, TA, list(range(NTA))), h * 128, bufs)
        k.flush()
        ps.close()
        self.stM.close()

    def build(self):
        self.declare()
        self.pmat_in = self.inp("pmat", [128, 128])
        self.ropeMT = self.inp("ropeMT", [128, 2, self.TA])
        self.consts()
        self.phase0()
        allt = list(range(self.NTA))
        latt = list(range(self.NTC, self.NTA))
        self.l0_passA(); self.l0_attnA(); self.l0_passB(); self.l0_retention()
        self.post_mixer(0, self.ab_w_out, allt)
        self.moe(0, allt, lambda tt: (self.x2, self.x2[tt * 128:(tt + 1) * 128, :]), GT=self.GT, SC=self.SC)
        if self.stop == "l0":
            return self.k.emit()
        self.l1_passM(); self.l1_attn()
        self.post_mixer(1, self.mla_w_out, latt)
        self.moe(1, latt, lambda tt: (self.y_out, self.y_out[(tt - self.NTC) * 128:(tt - self.NTC + 1) * 128, :]), GT=self.GT, SC=self.SC)
        return self.k.emit()


_TL, _TC, _NB = 4096, 256, 8


def kernel(**inputs):
    maps = prep_inputs(inputs, _TL, _TC, _NB)
    b = B2(_TL, _TC)
    nc = b.build()
    res = run_bass_kernel_spmd(nc, maps, core_ids=list(range(_NB)))
    return np.stack([np.asarray(r["y"], dtype=np.float32) for r in res.results], 0)
```

```python
import numpy as np
import ml_dtypes
from contextlib import ExitStack
import concourse.bass as bass
import concourse.mybir as mybir
from concourse.bass_utils import run_bass_kernel_spmd

F32 = mybir.dt.float32
BF16 = mybir.dt.bfloat16
I32 = mybir.dt.int32
AF = mybir.ActivationFunctionType
ALU = mybir.AluOpType
AX = mybir.AxisListType

D = 1024
GRID_W = 64
THETA = 10000.0
NE = 32
LIMIT = 7.0
SW_ALPHA = 1.702
DN_ALPHA = 4.0 ** 0.25


class SemSlot:
    __slots__ = ("sem", "count")

    def __init__(self):
        self.sem = None
        self.count = 0


class Res:
    __slots__ = ("name", "slot", "lastw", "readers", "lastdma")

    def __init__(self, name):
        self.name = name
        self.slot = {}
        self.lastw = None
        self.readers = []
        self.lastdma = None


class Op:
    __slots__ = ("eng", "fn", "deps", "marked", "event", "is_dma", "ei", "done")

    def __init__(self, eng, fn):
        self.eng = eng
        self.fn = fn
        self.deps = []
        self.marked = False
        self.event = None
        self.is_dma = False
        self.done = False


class T:
    __slots__ = ("ap", "r")

    def __init__(self, ap, r):
        self.ap = ap
        self.r = r

    def __getitem__(self, idx):
        return self.ap[idx]


def _rs(xs):
    return [x.r if isinstance(x, T) else x for x in xs]


class K:
    ENGS = ("pe", "act", "dve", "pool", "sp")

    def __init__(self):
        self.nc = bass.Bass("TRN2", target_bir_lowering=False)
        self.ops = []
        self.stack = ExitStack()
        self.last_on = {e: None for e in self.ENGS}
        self.owners = []
        self.free_slots = {"hw": [], "sw": []}
        self.n = 0
        self.esem = {e: self.stack.enter_context(self.nc.semaphore(f"s_{e}")) for e in self.ENGS}
        self.cnt = {e: 0 for e in self.ENGS}
        self.waited = {e: {} for e in self.ENGS}
        self.stats = dict(nops=0, nwait=0, ndrain=0, nsem=5)

    def res(self, name=None):
        self.n += 1
        return Res(f"{name or 'r'}{self.n}")

    def sbuf(self, name, shape, dt, stack=None):
        self.n += 1
        ap = (stack or self.stack).enter_context(self.nc.sbuf_tensor(f"{name}_{self.n}", list(shape), dt))
        return T(ap, self.res(name))

    def psum(self, name, shape, dt, stack=None):
        self.n += 1
        ap = (stack or self.stack).enter_context(self.nc.psum_tensor(f"{name}_{self.n}", list(shape), dt))
        return T(ap, self.res(name))

    def dram(self, name, shape, dt, kind="Internal"):
        return T(self.nc.dram_tensor(name, list(shape), dt, kind=kind).ap(), self.res(name))

    def _track(self, op, reads, writes):
        deps = op.deps
        for r in reads:
            if r.lastw is not None:
                deps.append(r.lastw)
            r.readers.append(op)
        for w in writes:
            if w.lastw is not None:
                deps.append(w.lastw)
            deps.extend(w.readers)
            w.lastw = op
            w.readers = []
        seen = set()
        out = []
        for d in deps:
            if d is op or d.done or id(d) in seen:
                continue
            seen.add(id(d))
            if d.eng == "pe" and op.eng == "pe" and not d.is_dma and not op.is_dma:
                continue
            out.append(d)
            if d.eng != op.eng or d.is_dma:
                d.marked = True
        op.deps = out
        self.ops.append(op)
        self.last_on[op.eng] = op

    def op(self, eng, fn, reads=(), writes=()):
        o = Op(eng, fn)
        self._track(o, _rs(reads), _rs(writes))
        return o

    def dma(self, eng, out, in_, owner, reads=(), writes=(), **kw):
        o = Op(eng, lambda e: e.dma_start(out=out, in_=in_, **kw))
        self._dma_common(o, owner, reads, writes)
        return o

    def dma_fn(self, eng, fn, owner, reads=(), writes=()):
        o = Op(eng, fn)
        self._dma_common(o, owner, reads, writes)
        return o

    def _dma_common(self, o, owner, reads, writes):
        owner = owner.r if isinstance(owner, T) else owner
        o.is_dma = True
        o.marked = True
        kind = "sw" if o.eng == "pool" else "hw"
        if kind not in owner.slot:
            fl = self.free_slots[kind]
            owner.slot[kind] = fl.pop() if fl else SemSlot()
            if owner not in self.owners:
                self.owners.append(owner)
        if owner.lastdma is not None and not owner.lastdma.done:
            o.deps.append(owner.lastdma)
        owner.lastdma = o
        sl = owner.slot[kind]
        sl.count += 16
        o.event = (sl, sl.count)
        self._track(o, _rs(reads), _rs(writes))

    def barrier(self):
        lasts = [self.last_on[e] for e in self.ENGS if self.last_on[e] is not None and not self.last_on[e].done]
        lasts += [o.lastdma for o in self.owners if o.lastdma is not None and not o.lastdma.done]
        for e in self.ENGS:
            o = Op(e, None)
            for d in lasts:
                if d.eng == e and not d.is_dma:
                    continue
                if d.fn is None and not d.is_dma:
                    continue
                o.deps.append(d)
                d.marked = True
            self.ops.append(o)
            self.last_on[e] = o

    def flush(self):
        self.barrier()
        nc = self.nc
        esem = self.esem
        for r in self.owners:
            for sl in r.slot.values():
                if sl.sem is None:
                    self.n += 1
                    sl.sem = self.stack.enter_context(nc.semaphore(f"d{self.n}"))
                    self.stats["nsem"] += 1
        for o in self.ops:
            if o.is_dma:
                o.event = (o.event[0].sem, o.event[1])
            elif o.marked:
                self.cnt[o.eng] += 1
                o.event = (esem[o.eng], self.cnt[o.eng])
        by_eng = {e: [o for o in self.ops if o.eng == e] for e in self.ENGS}
        for e in self.ENGS:
            for i, o in enumerate(by_eng[e]):
                o.ei = i

        def run(ename, eng):
            w = self.waited[ename]
            last_drain = -1
            for o in by_eng[ename]:
                need = {}
                drain = False
                for d in o.deps:
                    if d.eng == ename and not d.is_dma:
                        if d.fn is not None and d.ei > last_drain and o.ei - d.ei <= 8:
                            drain = True
                        continue
                    sem, val = d.event
                    key = id(sem)
                    if w.get(key, 0) >= val:
                        continue
                    if key not in need or need[key][1] < val:
                        need[key] = (sem, val)
                for key, (sem, val) in need.items():
                    eng.wait_ge(sem, val)
                    w[key] = val
                    self.stats["nwait"] += 1
                if drain or (o.fn is None and ename != "pe"):
                    eng.drain()
                    last_drain = o.ei - 1
                    self.stats["ndrain"] += 1
                if o.fn is None:
                    continue
                ins = o.fn(eng)
                if o.is_dma:
                    ins.then_inc(o.event[0], 16)
                elif o.marked:
                    ins.then_inc(esem[o.eng], 1)

        with nc.Block() as block:
            block.tensor(lambda e: run("pe", e))
            block.scalar(lambda e: run("act", e))
            block.vector(lambda e: run("dve", e))
            block.gpsimd(lambda e: run("pool", e))
            block.sync(lambda e: run("sp", e))
        self.stats["nops"] += len(self.ops)
        for o in self.ops:
            o.done = True
        self.ops = []
        for r in self.owners:
            for kind, sl in r.slot.items():
                self.free_slots[kind].append(sl)
            r.slot = {}
        self.owners = []

    def emit(self):
        self.flush()
        self.stack.close()
        return self.nc


def _rope_tables(pos, d):
    half = d // 2
    inv = (THETA ** (-np.arange(half, dtype=np.float32) / np.float32(half))).astype(np.float32)
    ang = pos.astype(np.float32)[:, None] * inv[None, :]
    c, s = np.cos(ang).astype(np.float32), np.sin(ang).astype(np.float32)
    return np.concatenate([c, c], 1), np.concatenate([-s, s], 1)


def _axial_tables(n_tok, d):
    rows = np.repeat(np.arange(n_tok // GRID_W), GRID_W)
    cols = np.tile(np.arange(GRID_W), n_tok // GRID_W)
    c1, s1 = _rope_tables(rows, d // 2)
    c2, s2 = _rope_tables(cols, d // 2)
    return np.concatenate([c1, c2], 1), np.concatenate([s1, s2], 1)


def make_tables(TL, TC):
    TA = TL + TC
    ca, sa = _axial_tables(TL, 128)
    CA = np.concatenate([np.ones((TC, 128), np.float32), ca], 0)
    SA = np.concatenate([np.zeros((TC, 128), np.float32), sa], 0)
    cb, sb = _rope_tables(np.arange(TA), 128)
    cm, sm = _axial_tables(TL, 64)
    CM = np.concatenate([np.ones((TC, 64), np.float32), cm], 0)
    SM = np.concatenate([np.zeros((TC, 64), np.float32), sm], 0)
    pm = np.zeros((128, 128), np.float32)
    for m in range(64):
        src = m + 16 if (m % 32) < 16 else m - 16
        pm[src, m] = 1.0
    rmt = np.zeros((128, 2, TA), np.float32)
    rmt[:64, 0] = CM.T
    rmt[:64, 1] = SM.T
    return dict(pmat=pm, ropeMT=rmt,
                ropeA=np.stack([CA, SA], 1).astype(np.float32),
                ropeB=np.stack([np.tile(cb, (1, 4)), np.tile(sb, (1, 4))], 1).astype(np.float32),
                ropeM=np.stack([np.tile(CM, (1, 8)), np.tile(SM, (1, 8))], 1).astype(np.float32))


class B:
    def __init__(self, TL, TC, debug=False, stop=None):
        self.k = K()
        self.TL, self.TC, self.TA = TL, TC, TL + TC
        self.NTL, self.NTC, self.NTA = TL // 128, TC // 128, (TL + TC) // 128
        self.debug = debug
        self.stop = stop
        self.GT = 12
        self.SC = 4

    def mm(self, out, lhsT, rhs, start, stop, reads, writes):
        self.k.op("pe", lambda e: e.matmul(out=out, lhsT=lhsT, rhs=rhs, start=start, stop=stop), reads, writes)

    def tr(self, out, in_, ident, reads, writes):
        self.k.op("pe", lambda e: e.transpose(out=out, in_=in_, identity=ident), reads, writes)

    def act(self, out, in_, func, reads, writes, bias=0.0, scale=1.0, accum_out=None):
        if accum_out is None:
            self.k.op("act", lambda e: e.activation(out=out, in_=in_, func=func, bias=bias, scale=scale), reads, writes)
        else:
            self.k.op("act", lambda e: e.activation(out=out, in_=in_, func=func, bias=bias, scale=scale, accum_out=accum_out), reads, writes)

    def ts(self, eng, out, in0, s1, s2, op0, op1, reads, writes):
        if s2 is None:
            self.k.op(eng, lambda e: e.tensor_scalar(out=out, in0=in0, scalar1=s1, scalar2=None, op0=op0), reads, writes)
        else:
            self.k.op(eng, lambda e: e.tensor_scalar(out=out, in0=in0, scalar1=s1, scalar2=s2, op0=op0, op1=op1), reads, writes)

    def tt(self, eng, out, in0, in1, op, reads, writes):
        self.k.op(eng, lambda e: e.tensor_tensor(out=out, in0=in0, in1=in1, op=op), reads, writes)

    def stt(self, eng, out, in0, scalar, in1, op0, op1, reads, writes):
        self.k.op(eng, lambda e: e.scalar_tensor_tensor(out=out, in0=in0, scalar=scalar, in1=in1, op0=op0, op1=op1), reads, writes)

    def cp(self, eng, out, in_, reads, writes):
        if eng == "act":
            self.k.op("act", lambda e: e.copy(out=out, in_=in_), reads, writes)
        else:
            self.k.op(eng, lambda e: e.tensor_copy(out=out, in_=in_), reads, writes)

    def memset(self, eng, ap, val, writes):
        self.k.op(eng, lambda e: e.memset(ap, val), (), writes)

    def recip(self, out, in_, reads, writes):
        self.k.op("dve", lambda e: e.reciprocal(out=out, in_=in_), reads, writes)

    def dma(self, out, in_, owner, reads=(), writes=(), eng="sp", **kw):
        self.k.dma(eng, out, in_, owner, reads, writes, **kw)

    def scratch(self, name, shape, dt):
        return self.k.dram(name, shape, dt, kind="ExternalOutput" if self.debug else "Internal")

    def inp(self, name, shape, dt=F32):
        return self.k.dram(name, shape, dt, kind="ExternalInput")

    def declare(self):
        TL, TC, TA = self.TL, self.TC, self.TA
        i = self.inp
        self.x_in = i("x", [TL, D]); self.ctx_in = i("ctx", [TC, D])
        self.cv2 = i("cv2", [128, 8, 2])
        self.ada_w = i("ada_w", [2, D, 6 * D]); self.ada_bF = i("ada_bF", [2, 128, 48]); self.ada_b = i("ada_b", [2, 1, 6 * D])
        self.lnF = i("lnF", [2, 128, 4, 8]); self.ln_rows = i("ln_rows", [2, 4, D])
        self.ab_w_in = i("ab_w_in", [D, 3072]); self.ab_qk = i("ab_qk", [2, 128]); self.ab_ld = i("ab_ld", [1, 8])
        self.ab_gn = i("ab_gn", [1, 512]); self.ab_w_out = i("ab_w_out", [D, D])
        self.mla_w_in = i("mla_w_in", [D, 704]); self.mla_qn = i("mla_qn", [1, 384]); self.mla_kvn = i("mla_kvn", [1, 256])
        self.mla_w_uq = i("mla_w_uq", [384, 1536]); self.mla_w_ukv = i("mla_w_ukv", [256, 2048]); self.mla_w_out = i("mla_w_out", [D, D])
        self.r_w = i("r_w", [2, D, NE]); self.r_b = i("r_b", [2, 1, NE])
        ne = getattr(self, "ne_decl", NE)
        self.w_up = i("w_up", [2, ne, D, 2 * D]); self.b_upF = i("b_upF", [2, NE, 128, 16])
        self.w_down = i("w_down", [2, ne, D, D]); self.b_down = i("b_down", [2, NE, D])
        self.identF_in = i("identF", [128, 128])
        self.ropeA = i("ropeA", [TA, 2, 128]); self.ropeB = i("ropeB", [TA, 2, 512]); self.ropeM = i("ropeM", [TA, 2, 512])
        self.y_out = self.k.dram("y", [TL, D], F32, kind="ExternalOutput")
        s = self.scratch
        self.hT0 = s("hT0", [self.NTA, 128, 8, 128], BF16)
        self.cat = s("cat", [TA, D], BF16)
        self.x1 = s("x1", [TA, D], F32)
        self.h2T = s("h2T", [self.NTA, 128, 8, 128], BF16)
        self.x2 = s("x2", [TA, D], F32)
        self.qTB = s("qTB", [4, 128, TA], BF16); self.kTB = s("kTB", [4, 128, TA], BF16)
        self.kB = s("kB", [TA, 512], BF16); self.vB = s("vB", [TA, 512], BF16); self.gB = s("gB", [TA, 512], BF16)

    def consts(self):
        k = self.k
        self.identF = k.sbuf("identF", [128, 128], F32)
        self.dma(self.identF[:], self.identF_in[:, :], self.identF, writes=[self.identF])
        self.identB = k.sbuf("identB", [128, 128], BF16)
        self.cp("dve", self.identB[:], self.identF[:], [self.identF], [self.identB])
        self.ones = k.sbuf("ones", [128, 512], F32)
        self.memset("pool", self.ones[:], 1.0, [self.ones])

    def xsrc(self, tt):
        if tt < self.NTC:
            return self.ctx_in, self.ctx_in[tt * 128:(tt + 1) * 128, :]
        t = tt - self.NTC
        return self.x_in, self.x_in[t * 128:(t + 1) * 128, :]

    def phase0(self):
        k = self.k
        P_modF = [k.sbuf("modF", [128, 48, 2], F32) for _ in range(2)]
        self.G = [k.sbuf("G", [128, self.NTA, NE], F32) for _ in range(2)]
        self.gate_rows = self.scratch("gate_rows", [2, 2, 2, D], F32)
        P_coef = [k.sbuf("coef", [128, 4, 2, 8], F32) for _ in range(2)]
        P_lnF = [k.sbuf("lnF", [128, 4, 8], F32) for _ in range(2)]
        ps = ExitStack()
        cv = k.sbuf("cv", [128, 8, 2], F32, ps)
        self.dma(cv[:], self.cv2[:, :, :], cv, writes=[cv])
        sc2 = k.sbuf("sc2", [128, 8, 2], F32, ps)
        self.act(sc2[:], cv[:], AF.Silu, [cv], [sc2])
        scB = k.sbuf("scB", [128, 8, 2, 128], F32, ps)
        for kk in range(8):
            for s in range(2):
                self.act(scB[:, kk, s, :], self.ones[:, 0:128], AF.Copy, [self.ones, sc2], [scB], scale=sc2[:, kk, s:s + 1])
        wch = [k.sbuf("wch", [128, 8, 512], F32, ps) for _ in range(2)]
        brow = [k.sbuf("brow", [1, 512], F32, ps) for _ in range(2)]
        grow = [k.sbuf("grow", [1, 512], F32, ps) for _ in range(2)]
        abFs = [k.sbuf("abF", [128, 48], F32, ps) for _ in range(2)]
        tmp = k.sbuf("ctmp", [128, 2, 8], F32, ps)
        psm = k.psum("psm", [128, 512], F32, ps)
        psg = [k.psum("psg", [128, 512], F32, ps) for _ in range(2)]
        self.modF, self.coef = [], []
        for i in range(2):
            modF, coef, lnF = P_modF[i], P_coef[i], P_lnF[i]
            abF = abFs[i]
            self.dma(abF[:], self.ada_bF[i], abF, writes=[abF])
            for n in range(12):
                w = wch[n % 2]
                self.dma(w[:], self.ada_w[i][:, n * 512:(n + 1) * 512].rearrange("(k p) n -> p k n", p=128), w, writes=[w])
                for m in range(4):
                    mc = n * 4 + m
                    for kk in range(8):
                        self.mm(psm[:, mc * 2:mc * 2 + 2], w[:, kk, m * 128:(m + 1) * 128], sc2[:, kk, :], kk == 0, kk == 7, [w, sc2], [psm])
                j = n // 2
                if j in (2, 5):
                    br = brow[n % 2]
                    self.dma(br[:], self.ada_b[i][:, n * 512:(n + 1) * 512], br, writes=[br])
                    for s in range(2):
                        for kk in range(8):
                            self.mm(psg[s][:], scB[:, kk, s, :], w[:, kk, :], kk == 0, False, [w, scB], [psg[s]])
                        self.mm(psg[s][:], self.ones[0:1, 0:128], br[:], False, True, [self.ones, br], [psg[s]])
                        gr = grow[(n + s) % 2]
                        self.cp("act", gr[:], psg[s][0:1, :], [psg[s]], [gr])
                        self.dma(self.gate_rows[i, 0 if j == 2 else 1, s:s + 1, (n % 2) * 512:(n % 2 + 1) * 512], gr[:], gr, reads=[gr], writes=[self.gate_rows])
            for s in range(2):
                self.tt("dve", modF[:, :, s], psm[:, s:96:2], abF[:], ALU.add, [psm, abF], [modF])
            self.dma(lnF[:], self.lnF[i], lnF, writes=[lnF])
            for s in range(2):
                self.ts("dve", coef[:, 0, s, :], modF[:, 8:16, s], 1.0, None, ALU.add, None, [modF], [coef])
                self.cp("dve", coef[:, 1, s, :], modF[:, 0:8, s], [modF], [coef])
                self.ts("dve", tmp[:, s, :], modF[:, 32:40, s], 1.0, None, ALU.add, None, [modF], [tmp])
                self.tt("dve", coef[:, 2, s, :], tmp[:, s, :], lnF[:, 0, :], ALU.mult, [tmp, lnF], [coef])
                self.tt("dve", coef[:, 3, s, :], tmp[:, s, :], lnF[:, 1, :], ALU.mult, [tmp, lnF], [coef])
                self.tt("dve", coef[:, 3, s, :], coef[:, 3, s, :], modF[:, 24:32, s], ALU.add, [coef, modF], [coef])
            self.modF.append(modF); self.coef.append(coef)
        k.flush()
        ps.close()

    def dbg(self, name, t, shape, dt=F32):
        if not self.debug:
            return
        d = self.k.dram("dbg_" + name, shape, dt, kind="ExternalOutput")
        self.dma(d.ap, t[:], t, reads=[t], writes=[d])


def _fm(v):
    return np.ascontiguousarray(np.swapaxes(v.reshape(v.shape[:-1] + (8, 128)), -1, -2))


def prep_inputs(inp, TL, TC, nb):
    f = lambda a: np.ascontiguousarray(np.asarray(a, dtype=np.float32))
    tabs = make_tables(TL, TC)
    shared = dict(
        ada_w=f(inp["ada_w"]),
        ada_bF=np.ascontiguousarray(np.transpose(f(inp["ada_b"]).reshape(2, 48, 128), (0, 2, 1))),
        ada_b=f(inp["ada_b"]).reshape(2, 1, 6 * D),
        lnF=np.ascontiguousarray(np.stack([_fm(f(inp[n])) for n in ("ln1_g", "ln1_b", "ln2_g", "ln2_b")], 2)),
        ln_rows=np.ascontiguousarray(np.stack([f(inp[n]) for n in ("ln1_g", "ln1_b", "ln2_g", "ln2_b")], 1)),
        ab_w_in=f(inp["ab_w_in"])[0], ab_qk=np.concatenate([f(inp["ab_q_norm"]), f(inp["ab_k_norm"])], 0),
        ab_ld=f(inp["ab_log_decay"]).reshape(1, 8), ab_gn=f(inp["ab_gn_g"]).reshape(1, 512), ab_w_out=f(inp["ab_w_out"])[0],
        mla_w_in=f(inp["mla_w_in"])[0], mla_qn=f(inp["mla_q_norm"]).reshape(1, 384), mla_kvn=f(inp["mla_kv_norm"]).reshape(1, 256),
        mla_w_uq=f(inp["mla_w_uq"])[0], mla_w_ukv=f(inp["mla_w_ukv"])[0], mla_w_out=f(inp["mla_w_out"])[0],
        r_w=f(inp["moe_router_w"]), r_b=f(inp["moe_router_b"]).reshape(2, 1, NE),
        w_up=f(inp["moe_w_up"]),
        b_upF=np.ascontiguousarray(np.transpose(f(inp["moe_b_up"]).reshape(2, NE, 16, 128), (0, 1, 3, 2))),
        w_down=f(inp["moe_w_down"]), b_down=f(inp["moe_b_down"]),
        identF=np.eye(128, dtype=np.float32), **tabs)
    x, c, ctx, c_ctx = f(inp["x"]), f(inp["c"]), f(inp["ctx"]), f(inp["c_ctx"])
    maps = []
    for b in range(nb):
        cv2 = np.ascontiguousarray(np.stack([_fm(c[b]), _fm(c_ctx)], -1))
        maps.append(dict(x=x[b], ctx=ctx[b], cv2=cv2, **shared))
    return maps


class B2(B):
    def load_hT(self, src_t, src_ap, xt, pT, hT, coef, s, ident, a_idx=0):
        self.dma(xt[:], src_ap, xt, reads=[src_t], writes=[xt])
        for half in range(2):
            for j in range(4):
                c = half * 4 + j
                self.tr(pT[half][:, j * 128:(j + 1) * 128], xt[:, c * 128:(c + 1) * 128], ident[:], [xt, ident], [pT[half]])
            for j in range(4):
                c = half * 4 + j
                self.act(hT[:, c, :], pT[half][:, j * 128:(j + 1) * 128], AF.Identity, [pT[half], coef], [hT],
                         bias=coef[:, a_idx + 1, s, c:c + 1], scale=coef[:, a_idx, s, c:c + 1])

    def rope(self, out, out_t, xin, C, S, nblk, h, tmp1, tmp2, reads):
        n = nblk * 2 * h
        v = lambda ap: ap.rearrange("p (b two h) -> p b two h", two=2, h=h)
        self.tt("dve", tmp1[:, 0:n], xin, C, ALU.mult, reads, [tmp1])
        self.tt("pool", v(tmp2[:, 0:n])[:, :, 0, :], v(xin)[:, :, 1, :], v(S)[:, :, 0, :], ALU.mult, reads, [tmp2])
        self.tt("pool", v(tmp2[:, 0:n])[:, :, 1, :], v(xin)[:, :, 0, :], v(S)[:, :, 1, :], ALU.mult, reads, [tmp2])
        self.tt("dve", out, tmp1[:, 0:n], tmp2[:, 0:n], ALU.add, [tmp1, tmp2], [out_t])

    def l0_passA(self):
        k = self.k
        NTA, TA = self.NTA, self.TA
        self.stA = ExitStack()
        self.QT = k.sbuf("QT", [128, 4, TA], BF16, self.stA)
        self.KT = k.sbuf("KT", [128, 2, TA], BF16, self.stA)
        self.VA = k.sbuf("VA", [128, NTA, 2, 130], BF16, self.stA)
        ps = ExitStack()
        wA = k.sbuf("wA", [128, 8, 1024], BF16, ps)
        self.dma(wA[:], self.ab_w_in[:, 0:1024].rearrange("(k p) n -> p k n", p=128), wA, writes=[wA], eng="pool")
        gq = k.sbuf("gq", [128, 2, 128], F32, ps)
        self.dma(gq[:, 0, :], self.ab_qk[0:1, :].partition_broadcast(128), gq, writes=[gq])
        self.dma(gq[:, 1, :], self.ab_qk[1:2, :].partition_broadcast(128), gq, writes=[gq])
        self.k.op("act", lambda e: e.mul(out=gq[:, 0, :], in_=gq[:, 0, :], mul=128.0 ** -0.5), [gq], [gq])
        self.memset("pool", self.VA[:], 1.0, [self.VA])
        xt = [k.sbuf("xt", [128, D], F32, ps) for _ in range(2)]
        hT = [k.sbuf("hT", [128, 8, 128], BF16, ps) for _ in range(2)]
        tab = [k.sbuf("tabA", [128, 2, 128], F32, ps) for _ in range(2)]
        ss = k.sbuf("ss", [128, 8], F32, ps)
        rstd = k.sbuf("rstd", [128, 8], F32, ps)
        junk = k.sbuf("junk", [128, 128], F32, ps)
        xn = [k.sbuf("xn", [128, 128], F32, ps) for _ in range(2)]
        t1 = [k.sbuf("t1", [128, 128], F32, ps) for _ in range(2)]
        t2 = [k.sbuf("t2", [128, 128], F32, ps) for _ in range(2)]
        ob = [k.sbuf("ob", [128, 128], BF16, ps) for _ in range(2)]
        pT = [k.psum("pT", [128, 512], F32, ps) for _ in range(2)]
        pA = [k.psum("pA", [128, 1024], F32, ps) for _ in range(2)]
        pTb = k.psum("pTb", [128, 1024], BF16, ps)
        coef = self.coef[0]
        for tt in range(NTA):
            s = 1 if tt < self.NTC else 0
            src_t, src_ap = self.xsrc(tt)
            x_, h_, tb, pa = xt[tt % 2], hT[tt % 2], tab[tt % 2], pA[tt % 2]
            self.load_hT(src_t, src_ap, x_, pT, h_, coef, s, self.identF)
            self.dma(self.hT0[tt], h_[:], h_, reads=[h_], writes=[self.hT0])
            self.dma(tb[:], self.ropeA[tt * 128:(tt + 1) * 128], tb, writes=[tb])
            for n in range(2):
                for c in range(8):
                    self.mm(pa[:, n * 512:(n + 1) * 512], h_[:, c, :], wA[:, c, n * 512:(n + 1) * 512], c == 0, c == 7, [h_, wA], [pa])
            for hh in range(6):
                self.act(junk[:], pa[:, hh * 128:(hh + 1) * 128], AF.Square, [pa], [junk, ss], accum_out=ss[:, hh:hh + 1])
            self.act(rstd[:, 0:6], ss[:, 0:6], AF.Sqrt, [ss], [rstd], bias=1e-6, scale=1.0 / 128)
            self.recip(rstd[:, 0:6], rstd[:, 0:6], [rstd], [rstd])
            for hh in range(6):
                i2 = hh % 2
                g = gq[:, 0, :] if hh < 4 else gq[:, 1, :]
                self.stt("dve", xn[i2][:], pa[:, hh * 128:(hh + 1) * 128], rstd[:, hh:hh + 1], g, ALU.mult, ALU.mult, [pa, rstd, gq], [xn[i2]])
                self.rope(ob[i2][:], ob[i2], xn[i2][:], tb[:, 0, :], tb[:, 1, :], 2, 32, t1[i2], t2[i2], [xn[i2], tb])
                self.tr(pTb[:, hh * 128:(hh + 1) * 128], ob[i2][:], self.identB[:], [ob[i2], self.identB], [pTb])
            self.cp("act", self.QT[:, :, tt * 128:(tt + 1) * 128], pTb[:, 0:512].rearrange("p (h t) -> p h t", h=4), [pTb], [self.QT])
            self.cp("act", self.KT[:, :, tt * 128:(tt + 1) * 128], pTb[:, 512:768].rearrange("p (h t) -> p h t", h=2), [pTb], [self.KT])
            self.cp("dve", self.VA[:, tt, :, 0:128], pa[:, 768:1024].rearrange("p (h d) -> p h d", h=2), [pa], [self.VA])
        k.flush()
        ps.close()

    def attention(self, parts, V, vres, q_ranges, cat_col, bufs, exp_bias=None):
        pS, pO, PT, osb, rec = bufs
        np_ = len(parts)
        it = 0
        for (q0, nq, ktiles) in q_ranges:
            nqb = nq // 128
            nk = len(ktiles)
            pts = []

            def emit_S(ki, it_):
                sp = pS[it_ % 2]
                pt = PT[it_ % 2]
                kt = ktiles[ki]
                for pi, (Qap, Kap, qres, kres) in enumerate(parts):
                    self.mm(sp[:, 0:nq], Kap(kt), Qap(q0, nq), pi == 0, pi == np_ - 1, [qres, kres], [sp])
                if exp_bias is None:
                    self.act(pt[:, 0:nq], sp[:, 0:nq], AF.Exp, [sp], [pt])
                else:
                    self.act(pt[:, 0:nq], sp[:, 0:nq], AF.Exp, [sp, exp_bias[1]], [pt], bias=exp_bias[0])
                return pt

            pts.append(emit_S(0, it))
            for ki in range(nk):
                if ki + 1 < nk:
                    pts.append(emit_S(ki + 1, it + ki + 1))
                pt = pts[ki]
                kt = ktiles[ki]
                for qb in range(nqb):
                    self.mm(pO[qb][:, 0:129], pt[:, qb * 128:(qb + 1) * 128], V(kt), ki == 0, ki == nk - 1, [pt, vres], [pO[qb]])
            it += nk
            ob = osb[(q0 // 512) % 2]
            for qb in range(nqb):
                self.recip(rec[:, qb:qb + 1], pO[qb][:, 128:129], [pO[qb]], [rec])
                self.act(ob[:, qb, :], pO[qb][:, 0:128], AF.Copy, [pO[qb], rec], [ob], scale=rec[:, qb:qb + 1])
            self.dma(self.cat[q0:q0 + nq, cat_col:cat_col + 128].rearrange("(qb p) c -> p qb c", p=128), ob[:, 0:nqb, :], ob,
                     reads=[ob], writes=[self.cat])

    def attn_bufs(self, ps):
        k = self.k
        pS = [k.psum("pS", [128, 512], F32, ps) for _ in range(2)]
        pO = [k.psum("pO", [128, 512], F32, ps) for _ in range(4)]
        PT = [k.sbuf("PT", [128, 512], BF16, ps) for _ in range(2)]
        osb = [k.sbuf("osb", [128, 4, 128], BF16, ps) for _ in range(2)]
        rec = k.sbuf("rec", [128, 4], F32, ps)
        return pS, pO, PT, osb, rec

    def q_chunks(self, q0, q1, ktiles):
        out = []
        q = q0
        while q < q1:
            n = min(512, q1 - q)
            out.append((q, n, ktiles))
            q += n
        return out

    def l0_attnA(self):
        ps = ExitStack()
        bufs = self.attn_bufs(ps)
        TC, TA, NTC, NTA = self.TC, self.TA, self.NTC, self.NTA
        for h in range(4):
            kvh = h // 2
            parts = [(lambda q0, n, h=h: self.QT[:, h, q0:q0 + n], lambda kt, kvh=kvh: self.KT[:, kvh, kt * 128:(kt + 1) * 128], self.QT, self.KT)]
            V = lambda kt, kvh=kvh: self.VA[:, kt, kvh, 0:129]
            ranges = self.q_chunks(0, TC, list(range(NTC))) + self.q_chunks(TC, TA, list(range(NTA)))
            self.attention(parts, V, self.VA, ranges, h * 128, bufs)
        self.k.flush()
        ps.close()
        self.stA.close()

    def l0_passB(self):
        k = self.k
        NTA = self.NTA
        ps = ExitStack()
        wB = k.sbuf("wB", [128, 8, 2048], BF16, ps)
        for hf in range(2):
            self.dma(wB[:, :, hf * 1024:(hf + 1) * 1024], self.ab_w_in[:, 1024 + hf * 1024:2048 + hf * 1024].rearrange("(k p) n -> p k n", p=128),
                     wB, writes=[wB], eng="pool")
        gnB = k.sbuf("gnB", [128, 512], F32, ps)
        self.dma(gnB[:], self.ab_gn[0:1, :].partition_broadcast(128), gnB, writes=[gnB])
        hT = [k.sbuf("hTb", [128, 8, 128], BF16, ps) for _ in range(2)]
        tab = [k.sbuf("tabB", [128, 2, 512], F32, ps) for _ in range(2)]
        xs = [k.sbuf("xsB", [128, 512], F32, ps) for _ in range(2)]
        t1 = k.sbuf("t1B", [128, 512], F32, ps)
        t2 = k.sbuf("t2B", [128, 512], F32, ps)
        qr = [k.sbuf("qrB", [128, 512], BF16, ps) for _ in range(2)]
        kr = [k.sbuf("krB", [128, 512], BF16, ps) for _ in range(2)]
        vb = [k.sbuf("vbB", [128, 512], BF16, ps) for _ in range(2)]
        gb = [k.sbuf("gbB", [128, 512], BF16, ps) for _ in range(2)]
        gs = k.sbuf("gsB", [128, 512], F32, ps)
        stg = [k.sbuf("stgB", [128, 8, 128], BF16, ps) for _ in range(2)]
        pB = k.psum("pB", [128, 2048], F32, ps)
        pTb = k.psum("pTbB", [128, 1024], BF16, ps)
        for tt in range(NTA):
            i2 = tt % 2
            h_, tb = hT[i2], tab[i2]
            rows = slice(tt * 128, (tt + 1) * 128)
            self.dma(h_[:], self.hT0[tt], h_, reads=[self.hT0], writes=[h_])
            self.dma(tb[:], self.ropeB[rows], tb, writes=[tb])
            for n in range(4):
                for c in range(8):
                    self.mm(pB[:, n * 512:(n + 1) * 512], h_[:, c, :], wB[:, c, n * 512:(n + 1) * 512], c == 0, c == 7, [h_, wB], [pB])
            self.act(xs[0][:], pB[:, 0:512], AF.Copy, [pB], [xs[0]], scale=128.0 ** -0.5)
            self.rope(qr[i2][:], qr[i2], xs[0][:], tb[:, 0, :], tb[:, 1, :], 4, 64, t1, t2, [xs[0], tb])
            self.act(xs[1][:], pB[:, 512:1024], AF.Copy, [pB], [xs[1]])
            self.rope(kr[i2][:], kr[i2], xs[1][:], tb[:, 0, :], tb[:, 1, :], 4, 64, t1, t2, [xs[1], tb])
            self.dma(self.kB[rows, :], kr[i2][:], kr[i2], reads=[kr[i2]], writes=[self.kB])
            for hh in range(4):
                self.tr(pTb[:, hh * 128:(hh + 1) * 128], qr[i2][:, hh * 128:(hh + 1) * 128], self.identB[:], [qr[i2], self.identB], [pTb])
                self.tr(pTb[:, 512 + hh * 128:512 + (hh + 1) * 128], kr[i2][:, hh * 128:(hh + 1) * 128], self.identB[:], [kr[i2], self.identB], [pTb])
            self.cp("act", stg[i2][:], pTb[:].rearrange("p (h t) -> p h t", h=8), [pTb], [stg[i2]])
            self.dma(self.qTB[:, :, rows].rearrange("h d t -> d h t"), stg[i2][:, 0:4, :], stg[i2], reads=[stg[i2]], writes=[self.qTB])
            self.dma(self.kTB[:, :, rows].rearrange("h d t -> d h t"), stg[i2][:, 4:8, :], stg[i2], reads=[stg[i2]], writes=[self.kTB])
            self.cp("dve", vb[i2][:], pB[:, 1024:1536], [pB], [vb[i2]])
            self.dma(self.vB[rows, :], vb[i2][:], vb[i2], reads=[vb[i2]], writes=[self.vB])
            self.act(gs[:], pB[:, 1536:2048], AF.Silu, [pB], [gs])
            self.tt("dve", gb[i2][:], gs[:], gnB[:], ALU.mult, [gs, gnB], [gb[i2]])
            self.dma(self.gB[rows, :], gb[i2][:], gb[i2], reads=[gb[i2]], writes=[self.gB])
        k.flush()
        ps.close()

    def l0_retention(self):
        k = self.k
        NTA, NTC, TA = self.NTA, self.NTC, self.TA
        ps = ExitStack()
        ld = k.sbuf("ld", [128, 8], F32, ps)
        self.dma(ld[:], self.ab_ld[0:1, :].partition_broadcast(128), ld, writes=[ld])
        lg = k.sbuf("lg", [128, 8], F32, ps)
        nlg = k.sbuf("nlg", [128, 8], F32, ps)
        self.act(lg[:], ld[:], AF.Exp, [ld], [lg])
        self.ts("dve", lg[:], lg[:], -1.0, 1.0, ALU.mult, ALU.add, [lg], [lg])
        self.act(lg[:], lg[:], AF.Ln, [lg], [lg])
        self.ts("dve", nlg[:], lg[:], -1.0, None, ALU.mult, None, [lg], [nlg])
        ii = k.sbuf("ii", [128, 128], I32, ps)
        dif = k.sbuf("dif", [128, 128], F32, ps)
        c1 = k.sbuf("c1", [128, 128], F32, ps)
        c128 = k.sbuf("c128", [128, 128], F32, ps)
        pcol = k.sbuf("pcol", [128, 4], F32, ps)
        k.op("pool", lambda e: e.iota(ii[:], [[1, 128]], base=0, channel_multiplier=-1), (), [ii])
        self.cp("dve", dif[:], ii[:], [ii], [dif])
        k.op("pool", lambda e: e.iota(ii[:], [[1, 128]], base=1, channel_multiplier=0), [dif], [ii])
        self.cp("dve", c1[:], ii[:], [ii], [c1])
        self.ts("dve", c128[:], c1[:], -1.0, 129.0, ALU.mult, ALU.add, [c1], [c128])
        k.op("pool", lambda e: e.iota(ii[:, 0:1], [[1, 1]], base=0, channel_multiplier=1), [c1], [ii])
        self.cp("dve", pcol[:, 0:1], ii[:, 0:1], [ii], [pcol])
        self.ts("dve", pcol[:, 1:2], pcol[:, 0:1], -1.0, 127.0, ALU.mult, ALU.add, [pcol], [pcol])
        self.memset("dve", pcol[:, 2:3], 128.0, [pcol])
        mf = k.sbuf("mf", [128, 128], F32, ps)
        mb = k.sbuf("mb", [128, 128], F32, ps)
        self.ts("dve", mf[:], dif[:], 0.0, None, ALU.is_ge, None, [dif], [mf])
        self.ts("dve", mb[:], dif[:], 0.0, None, ALU.is_lt, None, [dif], [mb])
        DT = k.sbuf("DT", [128, 128], F32, ps)
        DT2 = k.sbuf("DT2", [128, 128], F32, ps)
        xiF = k.sbuf("xiF", [128, 128], F32, ps)
        xiB = k.sbuf("xiB", [128, 128], F32, ps)
        zc = k.sbuf("zc", [128, 4], F32, ps)
        qT = k.sbuf("qTr", [128, TA], BF16, ps)
        kT = k.sbuf("kTr", [128, TA], BF16, ps)
        kk = k.sbuf("kkr", [128, NTA, 128], BF16, ps)
        vv = k.sbuf("vvr", [128, NTA, 128], BF16, ps)
        gg = k.sbuf("ggr", [128, NTA, 128], BF16, ps)
        Kzf = k.sbuf("Kzf", [128, NTA, 128], BF16, ps)
        Kzb = k.sbuf("Kzb", [128, NTA, 128], BF16, ps)
        SfP = k.sbuf("SfP", [128, NTA, 128], BF16, ps)
        SbP = k.sbuf("SbP", [128, NTA, 128], BF16, ps)
        Sf = k.sbuf("Sf", [128, 128], F32, ps)
        Sb = k.sbuf("Sb", [128, 128], F32, ps)
        Pm = [k.sbuf("Pm", [128, 128], BF16, ps) for _ in range(2)]
        qxf = [k.sbuf("qxf", [128, 128], BF16, ps) for _ in range(2)]
        qxb = [k.sbuf("qxb", [128, 128], BF16, ps) for _ in range(2)]
        st6 = k.sbuf("st6", [128, 6], F32, ps)
        mv = k.sbuf("mv", [128, 4], F32, ps)
        on = [k.sbuf("on", [128, 128], F32, ps) for _ in range(2)]
        ystg = k.sbuf("ystg", [128, NTA, 128], BF16, ps)
        pkv = [k.psum("pkv", [128, 512], F32, ps) for _ in range(2)]
        psc = [k.psum("psc", [128, 512], F32, ps) for _ in range(2)]
        pO = [k.psum("pOr", [128, 512], F32, ps) for _ in range(2)]
        order_b = list(range(NTC - 1, -1, -1)) + list(range(NTA - 1, NTC - 1, -1))
        for h in range(4):
            hs = slice(h * 128, (h + 1) * 128)
            self.dma(qT[:], self.qTB[h], qT, reads=[self.qTB], writes=[qT])
            self.dma(kT[:], self.kTB[h], kT, reads=[self.kTB], writes=[kT])
            for n0 in range(0, NTA, 8):
                n1 = min(NTA, n0 + 8)
                rs = slice(n0 * 128, n1 * 128)
                self.dma(kk[:, n0:n1, :], self.kB[rs, hs].rearrange("(n p) d -> p n d", p=128), kk, reads=[self.kB], writes=[kk])
                self.dma(vv[:, n0:n1, :], self.vB[rs, hs].rearrange("(n p) d -> p n d", p=128), vv, reads=[self.vB], writes=[vv])
                self.dma(gg[:, n0:n1, :], self.gB[rs, hs].rearrange("(n p) d -> p n d", p=128), gg, reads=[self.gB], writes=[gg])
            lf, lb, nlb = lg[:, h:h + 1], lg[:, 4 + h:5 + h], nlg[:, 4 + h:5 + h]
            self.act(DT[:], dif[:], AF.Exp, [dif, lg], [DT], scale=lf)
            self.tt("dve", DT[:], DT[:], mf[:], ALU.mult, [DT, mf], [DT])
            self.act(DT2[:], dif[:], AF.Exp, [dif, nlg], [DT2], scale=nlb)
            self.tt("dve", DT2[:], DT2[:], mb[:], ALU.mult, [DT2, mb], [DT2])
            self.tt("dve", DT[:], DT[:], DT2[:], ALU.add, [DT, DT2], [DT])
            self.act(xiF[:], c1[:], AF.Exp, [c1, lg], [xiF], scale=lf)
            self.act(xiB[:], c128[:], AF.Exp, [c128, lg], [xiB], scale=lb)
            self.act(zc[:, 0:1], pcol[:, 1:2], AF.Exp, [pcol, lg], [zc], scale=lf)
            self.act(zc[:, 1:2], pcol[:, 0:1], AF.Exp, [pcol, lg], [zc], scale=lb)
            self.act(zc[:, 2:3], pcol[:, 2:3], AF.Exp, [pcol, lg], [zc], scale=lf)
            self.act(zc[:, 3:4], pcol[:, 2:3], AF.Exp, [pcol, lg], [zc], scale=lb)
            self.ts("dve", Kzf[:], kk[:], zc[:, 0:1], None, ALU.mult, None, [kk, zc], [Kzf])
            self.ts("pool", Kzb[:], kk[:], zc[:, 1:2], None, ALU.mult, None, [kk, zc], [Kzb])
            self.memset("dve", Sf[:], 0.0, [Sf])
            self.memset("pool", Sb[:], 0.0, [Sb])
            for it, n in enumerate(range(NTA)):
                p_ = pkv[it % 2]
                self.cp("act", SfP[:, n, :], Sf[:], [Sf], [SfP])
                self.mm(p_[:, 0:128], Kzf[:, n, :], vv[:, n, :], True, True, [Kzf, vv], [p_])
                self.stt("dve", Sf[:], Sf[:], zc[:, 2:3], p_[:, 0:128], ALU.mult, ALU.add, [Sf, zc, p_], [Sf])
            for it, n in enumerate(order_b):
                p_ = pkv[it % 2]
                self.cp("act", SbP[:, n, :], Sb[:], [Sb], [SbP])
                self.mm(p_[:, 0:128], Kzb[:, n, :], vv[:, n, :], True, True, [Kzb, vv], [p_])
                self.stt("dve", Sb[:], Sb[:], zc[:, 3:4], p_[:, 0:128], ALU.mult, ALU.add, [Sb, zc, p_], [Sb])
            for n in range(NTA):
                i2 = n % 2
                cs = slice(n * 128, (n + 1) * 128)
                self.mm(psc[i2][:, 0:128], kT[:, cs], qT[:, cs], True, True, [kT, qT], [psc[i2]])
                self.tt("dve", Pm[i2][:], psc[i2][:, 0:128], DT[:], ALU.mult, [psc[i2], DT], [Pm[i2]])
                self.tt("pool", qxf[i2][:], qT[:, cs], xiF[:], ALU.mult, [qT, xiF], [qxf[i2]])
                self.tt("pool", qxb[i2][:], qT[:, cs], xiB[:], ALU.mult, [qT, xiB], [qxb[i2]])
                o_ = pO[i2]
                self.mm(o_[:, 0:128], Pm[i2][:], vv[:, n, :], True, False, [Pm[i2], vv], [o_])
                self.mm(o_[:, 0:128], qxf[i2][:], SfP[:, n, :], False, False, [qxf[i2], SfP], [o_])
                self.mm(o_[:, 0:128], qxb[i2][:], SbP[:, n, :], False, True, [qxb[i2], SbP], [o_])
                k.op("dve", lambda e, o_=o_: e.bn_stats(out=st6[:], in_=o_[:, 0:128]), [o_], [st6])
                k.op("dve", lambda e: e.bn_aggr(out=mv[:, 0:2], in_=st6[:]), [st6], [mv])
                self.act(mv[:, 2:3], mv[:, 1:2], AF.Sqrt, [mv], [mv], bias=1e-5)
                self.recip(mv[:, 2:3], mv[:, 2:3], [mv], [mv])
                self.stt("dve", mv[:, 3:4], mv[:, 0:1], -1.0, mv[:, 2:3], ALU.mult, ALU.mult, [mv], [mv])
                self.act(on[i2][:], o_[:, 0:128], AF.Identity, [o_, mv], [on[i2]], bias=mv[:, 3:4], scale=mv[:, 2:3])
                self.tt("pool", ystg[:, n, :], on[i2][:], gg[:, n, :], ALU.mult, [on[i2], gg], [ystg])
            for n0 in range(0, NTA, 8):
                n1 = min(NTA, n0 + 8)
                self.dma(self.cat[n0 * 128:n1 * 128, 512 + h * 128:512 + (h + 1) * 128].rearrange("(n p) c -> p n c", p=128), ystg[:, n0:n1, :], ystg,
                         reads=[ystg], writes=[self.cat])
        k.flush()
        ps.close()

    def layernorm(self, zn, z, st, mv, eps=1e-5):
        k = self.k
        for hf in range(2):
            k.op("dve", lambda e, hf=hf: e.bn_stats(out=st[:, hf * 6:(hf + 1) * 6], in_=z[:, hf * 512:(hf + 1) * 512]), [z], [st])
        k.op("dve", lambda e: e.bn_aggr(out=mv[:, 0:2], in_=st[:, 0:12]), [st], [mv])
        self.act(mv[:, 2:3], mv[:, 1:2], AF.Sqrt, [mv], [mv], bias=eps)
        self.recip(mv[:, 2:3], mv[:, 2:3], [mv], [mv])
        self.stt("dve", mv[:, 3:4], mv[:, 0:1], -1.0, mv[:, 2:3], ALU.mult, ALU.mult, [mv], [mv])
        self.act(zn[:], z[:], AF.Identity, [z, mv], [zn], bias=mv[:, 3:4], scale=mv[:, 2:3])

    def resid_src(self, i, tt):
        if i == 0:
            return self.xsrc(tt)
        return self.x2, self.x2[tt * 128:(tt + 1) * 128, :]

    def post_mixer(self, i, w_out, tiles):
        k = self.k
        ps = ExitStack()
        wo = k.sbuf("wo", [128, 8, D], BF16, ps)
        self.dma(wo[:], w_out[:, :].rearrange("(k p) n -> p k n", p=128), wo, writes=[wo], eng="pool")
        rw = k.sbuf("rw", [128, 8, NE], F32, ps)
        self.dma(rw[:], self.r_w[i].rearrange("(k p) e -> p k e", p=128), rw, writes=[rw])
        rb = k.sbuf("rb", [1, NE], F32, ps)
        self.dma(rb[:], self.r_b[i], rb, writes=[rb])
        lnB = k.sbuf("lnB", [128, 2, D], F32, ps)
        for q in range(2):
            self.dma(lnB[:, q, :], self.ln_rows[i][q:q + 1, :].partition_broadcast(128), lnB, writes=[lnB])
        gB = k.sbuf("g2B", [128, 2, D], F32, ps)
        for s in range(2):
            self.dma(gB[:, s, :], self.gate_rows[i, 0, s:s + 1, :].partition_broadcast(128), gB, reads=[self.gate_rows], writes=[gB])
        ct = [k.sbuf("ct", [128, D], BF16, ps) for _ in range(2)]
        catT_2 = [k.sbuf("catT", [128, 8, 128], BF16, ps) for _ in range(2)]
        xt = [k.sbuf("xtp", [128, D], F32, ps) for _ in range(2)]
        z_2 = [k.sbuf("z", [128, D], F32, ps) for _ in range(2)]
        zn_2 = [k.sbuf("zn", [128, D], F32, ps) for _ in range(2)]
        xl = [k.sbuf("xl", [128, D], F32, ps) for _ in range(2)]
        st_2 = [k.sbuf("st", [128, 12], F32, ps) for _ in range(2)]
        mv_2 = [k.sbuf("mvp", [128, 4], F32, ps) for _ in range(2)]
        h2f_2 = [k.sbuf("h2f", [128, 8, 128], F32, ps) for _ in range(2)]
        h2b = [k.sbuf("h2b", [128, 8, 128], BF16, ps) for _ in range(2)]
        lgt_2 = [k.sbuf("lgt", [128, NE], F32, ps) for _ in range(2)]
        m8_2 = [k.sbuf("m8", [128, 8], F32, ps) for _ in range(2)]
        msk_2 = [k.sbuf("msk", [128, NE], F32, ps) for _ in range(2)]
        ex_2 = [k.sbuf("ex", [128, NE], F32, ps) for _ in range(2)]
        den_2 = [k.sbuf("den", [128, 2], F32, ps) for _ in range(2)]
        pTb = k.psum("pTbp", [128, 1024], BF16, ps)
        pY = k.psum("pYp", [128, 1024], F32, ps)
        pT = [k.psum("pTp", [128, 512], F32, ps) for _ in range(2)]
        pR = k.psum("pR", [128, 512], F32, ps)
        coef = self.coef[i]
        G = self.G[i]
        for it, tt in enumerate(tiles):
            i2 = it % 2
            s = 1 if tt < self.NTC else 0
            rows = slice(tt * 128, (tt + 1) * 128)
            c_ = ct[i2]
            catT, z, zn, st, mv, h2f, lgt, m8, msk, ex, den = (catT_2[i2], z_2[i2], zn_2[i2], st_2[i2], mv_2[i2], h2f_2[i2], lgt_2[i2], m8_2[i2], msk_2[i2], ex_2[i2], den_2[i2])
            self.dma(c_[:], self.cat[rows, :], c_, reads=[self.cat], writes=[c_])
            for c in range(8):
                self.tr(pTb[:, c * 128:(c + 1) * 128], c_[:, c * 128:(c + 1) * 128], self.identB[:], [c_, self.identB], [pTb])
            self.cp("act", catT[:], pTb[:].rearrange("p (c t) -> p c t", c=8), [pTb], [catT])
            for n in range(2):
                for c in range(8):
                    self.mm(pY[:, n * 512:(n + 1) * 512], catT[:, c, :], wo[:, c, n * 512:(n + 1) * 512], c == 0, c == 7, [catT, wo], [pY])
            src_t, src_ap = self.resid_src(i, tt)
            x_ = xt[i2]
            self.dma(x_[:], src_ap, x_, reads=[src_t], writes=[x_])
            self.tt("dve", z[:], pY[:], gB[:, s, :], ALU.mult, [pY, gB], [z])
            self.stt("dve", z[:], x_[:], DN_ALPHA, z[:], ALU.mult, ALU.add, [x_, z], [z])
            self.layernorm(zn, z, st, mv)
            x1_ = xl[i2]
            self.tt("pool", x1_[:], zn[:], lnB[:, 0, :], ALU.mult, [zn, lnB], [x1_])
            self.tt("pool", x1_[:], x1_[:], lnB[:, 1, :], ALU.add, [x1_, lnB], [x1_])
            self.dma(self.x1[rows, :], x1_[:], x1_, reads=[x1_], writes=[self.x1])
            for half in range(2):
                for j in range(4):
                    c = half * 4 + j
                    self.tr(pT[half][:, j * 128:(j + 1) * 128], zn[:, c * 128:(c + 1) * 128], self.identF[:], [zn, self.identF], [pT[half]])
                for j in range(4):
                    c = half * 4 + j
                    self.act(h2f[:, c, :], pT[half][:, j * 128:(j + 1) * 128], AF.Identity, [pT[half], coef], [h2f],
                             bias=coef[:, 3, s, c:c + 1], scale=coef[:, 2, s, c:c + 1])
            hb = h2b[i2]
            self.cp("pool", hb[:], h2f[:], [h2f], [hb])
            self.dma(self.h2T[tt], hb[:], hb, reads=[hb], writes=[self.h2T])
            for c in range(8):
                self.mm(pR[:, 0:NE], h2f[:, c, :], rw[:, c, :], c == 0, False, [h2f, rw], [pR])
            self.mm(pR[:, 0:NE], self.ones[0:1, 0:128], rb[:], False, True, [self.ones, rb], [pR])
            self.cp("act", lgt[:], pR[:, 0:NE], [pR], [lgt])
            k.op("dve", lambda e, m8=m8, lgt=lgt: e.max(out=m8[:], in_=lgt[:]), [lgt], [m8])
            self.ts("dve", msk[:], lgt[:], m8[:, 3:4], None, ALU.is_ge, None, [lgt, m8], [msk])
            self.ts("dve", den[:, 0:1], m8[:, 0:1], -1.0, None, ALU.mult, None, [m8], [den])
            self.act(ex[:], lgt[:], AF.Exp, [lgt, den], [ex], bias=den[:, 0:1])
            self.tt("dve", ex[:], ex[:], msk[:], ALU.mult, [ex, msk], [ex])
            k.op("dve", lambda e, den=den, ex=ex: e.reduce_sum(out=den[:, 1:2], in_=ex[:], axis=AX.X), [ex], [den])
            self.recip(den[:, 1:2], den[:, 1:2], [den], [den])
            self.ts("dve", G[:, tt, :], ex[:], den[:, 1:2], None, ALU.mult, None, [ex, den], [G])
        k.flush()
        ps.close()

    def moe(self, i, tiles, out_fn, GT=12, SC=4):
        k = self.k
        G = self.G[i]
        groups = [tiles[j:j + GT] for j in range(0, len(tiles), GT)]
        gps = ExitStack()
        bd = k.sbuf("bd", [NE, D], F32, gps)
        self.dma(bd[:], self.b_down[i], bd, writes=[bd])
        for grp in groups:
            ng = len(grp)
            subs = [list(range(j, min(ng, j + SC))) for j in range(0, ng, SC)]
            ps = ExitStack()
            facc = k.sbuf("facc", [128, GT, D], F32, ps)
            gT = k.sbuf("gT", [NE, 128], F32, ps)
            ins = ExitStack()
            Hb = [k.sbuf("H", [128, 8, SC * 128], BF16, ins) for _ in range(2)]
            wu = [k.sbuf("wu", [128, 8, 2 * D], BF16, ins) for _ in range(2)]
            wd = [k.sbuf("wd", [128, 8, D], BF16, ins) for _ in range(2)]
            bu = [k.sbuf("bu", [128, 16], F32, ins) for _ in range(2)]
            actT = [k.sbuf("actT", [128, 8, SC * 128], BF16, ins) for _ in range(2)]
            glu = [k.sbuf("glu", [128, SC * 128], F32, ins) for _ in range(2)]
            sig = [k.sbuf("sig", [128, SC * 128], F32, ins) for _ in range(2)]
            l1 = [k.sbuf("l1", [128, SC * 128], F32, ins) for _ in range(2)]
            pU = [k.psum("pU", [128, 2, 512], F32, ins) for _ in range(2)]
            pY = [k.psum("pYm", [128, 1024], F32, ins) for _ in range(2)]
            for j, tt in enumerate(grp):
                py = pY[j % 2]
                self.tr(pU[0][0:NE, 0, 0:128], G[:, tt, :], self.identF[:], [G, self.identF], [pU[0]])
                self.cp("act", gT[:], pU[0][0:NE, 0, 0:128], [pU[0]], [gT])
                for n in range(2):
                    self.mm(py[:, n * 512:(n + 1) * 512], gT[:], bd[:, n * 512:(n + 1) * 512], True, True, [gT, bd], [py])
                self.cp("act", facc[:, j, :], py[:], [py], [facc])
            hit = 0
            pending = None
            for e in range(NE):
                e2 = e % 2
                wu_, wd_, bu_ = wu[e2], wd[e2], bu[e2]
                for hf in range(2):
                    self.dma(wu_[:, :, hf * D:(hf + 1) * D], self.w_up[i, e][:, hf * D:(hf + 1) * D].rearrange("(k p) n -> p k n", p=128),
                             wu_, writes=[wu_], eng="pool")
                self.dma(wd_[:], self.w_down[i, e].rearrange("(k p) n -> p k n", p=128), wd_, writes=[wd_], eng="pool")
                self.dma(bu_[:], self.b_upF[i, e], bu_, writes=[bu_])
                self.ts("pool", bu_[:, 8:16], bu_[:, 8:16], 1.0, None, ALU.add, None, [bu_], [bu_])
                for sub in subs:
                    N = len(sub) * 128
                    H = Hb[hit % 2]
                    aT = actT[hit % 2]
                    hit += 1
                    for jj, j in enumerate(sub):
                        self.dma(H[:, :, jj * 128:(jj + 1) * 128], self.h2T[grp[j]], H, reads=[self.h2T], writes=[H])
                    for m in range(8):
                        m2 = m % 2
                        pu = pU[m2]
                        for c in range(8):
                            self.mm(pu[:, 0, 0:N], wu_[:, c, m * 128:(m + 1) * 128], H[:, c, 0:N], c == 0, c == 7, [wu_, H], [pu])
                        for c in range(8):
                            self.mm(pu[:, 1, 0:N], wu_[:, c, D + m * 128:D + (m + 1) * 128], H[:, c, 0:N], c == 0, c == 7, [wu_, H], [pu])
                        g_, s_, l_ = glu[m2], sig[m2], l1[m2]
                        self.ts("dve", g_[:, 0:N], pu[:, 0, 0:N], bu_[:, m:m + 1], LIMIT, ALU.add, ALU.min, [pu, bu_], [g_])
                        self.act(s_[:, 0:N], g_[:, 0:N], AF.Sigmoid, [g_], [s_], scale=SW_ALPHA)
                        self.ts("dve", l_[:, 0:N], pu[:, 1, 0:N], bu_[:, 8 + m:9 + m], LIMIT + 1.0, ALU.add, ALU.min, [pu, bu_], [l_])
                        self.tt("pool", s_[:, 0:N], s_[:, 0:N], g_[:, 0:N], ALU.mult, [s_, g_], [s_])
                        self.stt("dve", aT[:, m, 0:N], l_[:, 0:N], 1.0 - LIMIT, s_[:, 0:N], ALU.max, ALU.mult, [l_, s_], [aT])

                    def down(sub=sub, aT=aT, wd_=wd_, e=e):
                        for jj, j in enumerate(sub):
                            tt = grp[j]
                            py = pY[j % 2]
                            for n in range(2):
                                for m in range(8):
                                    self.mm(py[:, n * 512:(n + 1) * 512], aT[:, m, jj * 128:(jj + 1) * 128], wd_[:, m, n * 512:(n + 1) * 512], m == 0, m == 7,
                                            [aT, wd_], [py])
                            self.stt("dve", facc[:, j, :], py[:], G[:, tt, e:e + 1], facc[:, j, :], ALU.mult, ALU.add, [py, G, facc], [facc])

                    if pending is not None:
                        pending()
                    pending = down
            if pending is not None:
                pending()
                pending = None
            k.flush()
            ins.close()
            ln = ExitStack()
            lnB = k.sbuf("lnB2", [128, 2, D], F32, ln)
            for q in range(2):
                self.dma(lnB[:, q, :], self.ln_rows[i][2 + q:3 + q, :].partition_broadcast(128), lnB, writes=[lnB])
            gB = k.sbuf("g5B", [128, 2, D], F32, ln)
            for s in range(2):
                self.dma(gB[:, s, :], self.gate_rows[i, 1, s:s + 1, :].partition_broadcast(128), gB, reads=[self.gate_rows], writes=[gB])
            x1t = [k.sbuf("x1t", [128, D], F32, ln) for _ in range(2)]
            z = k.sbuf("z2", [128, D], F32, ln)
            zn = k.sbuf("zn2", [128, D], F32, ln)
            ot = [k.sbuf("ot", [128, D], F32, ln) for _ in range(2)]
            st = k.sbuf("st2", [128, 12], F32, ln)
            mv = k.sbuf("mv2", [128, 4], F32, ln)
            for j, tt in enumerate(grp):
                s = 1 if tt < self.NTC else 0
                x_ = x1t[j % 2]
                self.dma(x_[:], self.x1[tt * 128:(tt + 1) * 128, :], x_, reads=[self.x1], writes=[x_])
                self.tt("dve", z[:], facc[:, j, :], gB[:, s, :], ALU.mult, [facc, gB], [z])
                self.stt("dve", z[:], x_[:], DN_ALPHA, z[:], ALU.mult, ALU.add, [x_, z], [z])
                self.layernorm(zn, z, st, mv)
                o_ = ot[j % 2]
                self.tt("pool", o_[:], zn[:], lnB[:, 0, :], ALU.mult, [zn, lnB], [o_])
                self.tt("pool", o_[:], o_[:], lnB[:, 1, :], ALU.add, [o_, lnB], [o_])
                dst_t, dst_ap = out_fn(tt)
                self.dma(dst_ap, o_[:], o_, reads=[o_], writes=[dst_t])
            k.flush()
            ln.close()
            ps.close()
        gps.close()

    def l1_passM(self):
        k = self.k
        NTA, NTC, TA = self.NTA, self.NTC, self.TA
        self.stM = ExitStack()
        self.qlatT = k.sbuf("qlatT", [128, 3, TA], BF16, self.stM)
        self.ckvT = k.sbuf("ckvT", [128, 2, TA], BF16, self.stM)
        self.kpeT = k.sbuf("kpeT", [128, TA], BF16, self.stM)
        ps = ExitStack()
        wM = k.sbuf("wM", [128, 8, 704], BF16, ps)
        self.dma(wM[:], self.mla_w_in[:, :].rearrange("(k p) n -> p k n", p=128), wM, writes=[wM], eng="pool")
        gn = k.sbuf("gnM", [128, 640], F32, ps)
        self.dma(gn[:, 0:384], self.mla_qn[0:1, :].partition_broadcast(128), gn, writes=[gn])
        self.dma(gn[:, 384:640], self.mla_kvn[0:1, :].partition_broadcast(128), gn, writes=[gn])
        xt = [k.sbuf("xtM", [128, D], F32, ps) for _ in range(2)]
        hT = [k.sbuf("hTM", [128, 8, 128], BF16, ps) for _ in range(2)]
        tab = [k.sbuf("tabM", [128, 2, 64], F32, ps) for _ in range(2)]
        ss5 = k.sbuf("ss5", [128, 8], F32, ps)
        ssq = k.sbuf("ssq", [128, 2], F32, ps)
        rsd = k.sbuf("rsd", [128, 2], F32, ps)
        xnf = k.sbuf("xnf", [128, 640], F32, ps)
        junk = k.sbuf("junkM", [128, 128], F32, ps)
        xn = [k.sbuf("xnM", [128, 640], BF16, ps) for _ in range(2)]
        kp = k.sbuf("kpM", [128, 64], F32, ps)
        t1 = k.sbuf("t1M", [128, 64], F32, ps)
        t2 = k.sbuf("t2M", [128, 64], F32, ps)
        kb = [k.sbuf("kbM", [128, 128], BF16, ps) for _ in range(2)]
        for _kb in kb:
            self.memset("pool", _kb[:], 0.0, [_kb])
        pT = [k.psum("pTM", [128, 512], F32, ps) for _ in range(2)]
        pM = k.psum("pM", [128, 1024], F32, ps)
        pTb = k.psum("pTbM", [128, 1024], BF16, ps)
        coef = self.coef[1]
        for tt in range(NTA):
            i2 = tt % 2
            s = 1 if tt < NTC else 0
            rows = slice(tt * 128, (tt + 1) * 128)
            x_, h_, tb = xt[i2], hT[i2], tab[i2]
            self.load_hT(self.x2, self.x2[rows, :], x_, pT, h_, coef, s, self.identF)
            self.dma(tb[:], self.ropeM[rows, :, 0:64], tb, writes=[tb])
            for c in range(8):
                self.mm(pM[:, 0:512], h_[:, c, :], wM[:, c, 0:512], c == 0, c == 7, [h_, wM], [pM])
            for c in range(8):
                self.mm(pM[:, 512:704], h_[:, c, :], wM[:, c, 512:704], c == 0, c == 7, [h_, wM], [pM])
            for c in range(5):
                self.act(junk[:, 0:128], pM[:, c * 128:(c + 1) * 128], AF.Square, [pM], [junk, ss5], accum_out=ss5[:, c:c + 1])
            self.tt("dve", ssq[:, 0:1], ss5[:, 0:1], ss5[:, 1:2], ALU.add, [ss5], [ssq])
            self.tt("dve", ssq[:, 0:1], ssq[:, 0:1], ss5[:, 2:3], ALU.add, [ss5, ssq], [ssq])
            self.tt("dve", ssq[:, 1:2], ss5[:, 3:4], ss5[:, 4:5], ALU.add, [ss5], [ssq])
            self.act(rsd[:, 0:1], ssq[:, 0:1], AF.Sqrt, [ssq], [rsd], bias=1e-6, scale=1.0 / 384)
            self.act(rsd[:, 1:2], ssq[:, 1:2], AF.Sqrt, [ssq], [rsd], bias=1e-6, scale=1.0 / 256)
            self.recip(rsd[:, 0:2], rsd[:, 0:2], [rsd], [rsd])
            x2_ = xn[i2]
            self.stt("dve", xnf[:, 0:384], pM[:, 0:384], rsd[:, 0:1], gn[:, 0:384], ALU.mult, ALU.mult, [pM, rsd, gn], [xnf])
            self.stt("dve", xnf[:, 384:640], pM[:, 384:640], rsd[:, 1:2], gn[:, 384:640], ALU.mult, ALU.mult, [pM, rsd, gn], [xnf])
            self.cp("pool", x2_[:], xnf[:], [xnf], [x2_])
            self.cp("act", kp[:], pM[:, 640:704], [pM], [kp])
            self.rope(kb[i2][:, 0:64], kb[i2], kp[:], tb[:, 0, :], tb[:, 1, :], 2, 16, t1, t2, [kp, tb])
            for c in range(5):
                self.tr(pTb[:, c * 128:(c + 1) * 128], x2_[:, c * 128:(c + 1) * 128], self.identB[:], [x2_, self.identB], [pTb])
            self.tr(pTb[:, 640:768], kb[i2][:], self.identB[:], [kb[i2], self.identB], [pTb])
            self.cp("act", self.qlatT[:, :, rows], pTb[:, 0:384].rearrange("p (c t) -> p c t", c=3), [pTb], [self.qlatT])
            self.cp("act", self.ckvT[:, :, rows], pTb[:, 384:640].rearrange("p (c t) -> p c t", c=2), [pTb], [self.ckvT])
            self.cp("act", self.kpeT[:, rows], pTb[:, 640:768], [pTb], [self.kpeT])
        k.flush()
        ps.close()

    def l1_attn(self):
        k = self.k
        NTA, NTC, TA, TC, TL = self.NTA, self.NTC, self.TA, self.TC, self.TL
        ps = ExitStack()
        wuq = k.sbuf("wuq", [128, 3, 1536], BF16, ps)
        self.dma(wuq[:], self.mla_w_uq[:, :].rearrange("(k p) n -> p k n", p=128), wuq, writes=[wuq], eng="pool")
        wukv = k.sbuf("wukv", [128, 2, 2048], BF16, ps)
        self.dma(wukv[:], self.mla_w_ukv[:, :].rearrange("(k p) n -> p k n", p=128), wukv, writes=[wukv], eng="pool")
        wuqr = k.sbuf("wuqr", [128, 3, 8, 128], BF16, ps)
        self.memset("pool", wuqr[:], 0.0, [wuqr])
        for c in range(3):
            self.dma(wuqr[:, c, :, 0:64], self.mla_w_uq[c * 128:(c + 1) * 128, :].rearrange("p (h c) -> p h c", c=192)[:, :, 128:192],
                     wuqr, writes=[wuqr], eng="pool")
        pmat = k.sbuf("pmat", [128, 128], F32, ps)
        self.dma(pmat[:], self.pmat_in[:, :], pmat, writes=[pmat])
        CT = k.sbuf("CTm", [128, 2, TA], F32, ps)
        self.dma(CT[:], self.ropeMT[:, :, :], CT, writes=[CT])
        KnT = k.sbuf("KnT", [128, TA], BF16, ps)
        Vh = k.sbuf("Vh", [128, NTA, 130], BF16, ps)
        QnT = k.sbuf("QnT", [128, TA], BF16, ps)
        QrT = k.sbuf("QrT", [128, TA], BF16, ps)
        raw = k.sbuf("rawq", [128, 512], F32, ps)
        u1 = k.sbuf("u1", [128, 512], F32, ps)
        u2 = k.sbuf("u2", [128, 512], F32, ps)
        self.memset("pool", Vh[:], 1.0, [Vh])
        pP = [k.psum("pP", [128, 512], F32, ps) for _ in range(2)]
        bufs = self.attn_bufs(ps)
        qscale = 192.0 ** -0.5
        chunks = lambda a, b_: [(q, min(512, b_ - q)) for q in range(a, b_, 512)]
        for h in range(8):
            kc, vc, qc, rc = h * 256, h * 256 + 128, h * 192, h * 192 + 128
            for ci, (t0, n) in enumerate(chunks(0, TA)):
                p_ = pP[ci % 2]
                for c in range(2):
                    self.mm(p_[:, 0:n], wukv[:, c, kc:kc + 128], self.ckvT[:, c, t0:t0 + n], c == 0, c == 1, [wukv, self.ckvT], [p_])
                self.cp("act", KnT[:, t0:t0 + n], p_[:, 0:n], [p_], [KnT])
            for tt in range(NTA):
                p_ = pP[tt % 2]
                for c in range(2):
                    self.mm(p_[:, 0:128], self.ckvT[:, c, tt * 128:(tt + 1) * 128], wukv[:, c, vc:vc + 128], c == 0, c == 1, [wukv, self.ckvT], [p_])
                self.cp("dve", Vh[:, tt, 0:128], p_[:, 0:128], [p_], [Vh])
            for ci, (t0, n) in enumerate(chunks(TC, TA)):
                p_ = pP[ci % 2]
                for c in range(3):
                    self.mm(p_[:, 0:n], wuq[:, c, qc:qc + 128], self.qlatT[:, c, t0:t0 + n], c == 0, c == 2, [wuq, self.qlatT], [p_])
                self.act(QnT[:, t0:t0 + n], p_[:, 0:n], AF.Copy, [p_], [QnT], scale=qscale)
                p2 = pP[(ci + 1) % 2]
                for c in range(3):
                    self.mm(p2[:, 0:n], wuqr[:, c, h, :], self.qlatT[:, c, t0:t0 + n], c == 0, c == 2, [wuqr, self.qlatT], [p2])
                self.act(raw[:, 0:n], p2[:, 0:n], AF.Copy, [p2], [raw], scale=qscale)
                self.mm(p2[:, 0:n], pmat[:], raw[:, 0:n], True, True, [pmat, raw], [p2])
                self.tt("dve", u1[:, 0:n], raw[:, 0:n], CT[:, 0, t0:t0 + n], ALU.mult, [raw, CT], [u1])
                self.tt("dve", u2[:, 0:n], p2[:, 0:n], CT[:, 1, t0:t0 + n], ALU.mult, [p2, CT], [u2])
                self.tt("dve", QrT[:, t0:t0 + n], u1[:, 0:n], u2[:, 0:n], ALU.add, [u1, u2], [QrT])
            parts = [(lambda q0, n: QnT[:, q0:q0 + n], lambda kt: KnT[:, kt * 128:(kt + 1) * 128], QnT, KnT),
                     (lambda q0, n: QrT[:, q0:q0 + n], lambda kt: self.kpeT[:, kt * 128:(kt + 1) * 128], QrT, self.kpeT)]
            V = lambda kt: Vh[:, kt, 0:129]
            self.attention(parts, V, Vh, self.q_chunks(TC, TA, list(range(NTA))), h * 128, bufs)
        k.flush()
        ps.close()
        self.stM.close()

    def build(self):
        self.declare()
        self.pmat_in = self.inp("pmat", [128, 128])
        self.ropeMT = self.inp("ropeMT", [128, 2, self.TA])
        self.consts()
        self.phase0()
        allt = list(range(self.NTA))
        latt = list(range(self.NTC, self.NTA))
        self.l0_passA(); self.l0_attnA(); self.l0_passB(); self.l0_retention()
        self.post_mixer(0, self.ab_w_out, allt)
        self.moe(0, allt, lambda tt: (self.x2, self.x2[tt * 128:(tt + 1) * 128, :]), GT=self.GT, SC=self.SC)
        if self.stop == "l0":
            return self.k.emit()
        self.l1_passM(); self.l1_attn()
        self.post_mixer(1, self.mla_w_out, latt)
        self.moe(1, latt, lambda tt: (self.y_out, self.y_out[(tt - self.NTC) * 128:(tt - self.NTC + 1) * 128, :]), GT=self.GT, SC=self.SC)
        return self.k.emit()


_TL, _TC, _NB = 4096, 256, 8


def kernel(**inputs):
    maps = prep_inputs(inputs, _TL, _TC, _NB)
    b = B2(_TL, _TC)
    nc = b.build()
    res = run_bass_kernel_spmd(nc, maps, core_ids=list(range(_NB)))
    return np.stack([np.asarray(r["y"], dtype=np.float32) for r in res.results], 0)
```
